# Optimizing a Trainium2 kernel written in Bass

```python
import math
import jax, jax.numpy as jnp
from jax import lax
import numpy as np

D_MODEL = 2048
BATCH = 8
SEQ = 2048
DEPTH = 1

DN_HEADS = 8
DN_HEAD_DIM = 128
DN_WIDTH = DN_HEADS * DN_HEAD_DIM
DN_CONV = 4
DN_CHUNK = 64
MLA_HEADS = 8
MLA_Q_RANK = 512
MLA_KV_RANK = 512
MLA_NOPE = 128
MLA_ROPE = 64
MLA_V = 128
MLA_WIDTH = MLA_HEADS * MLA_V
ROPE_THETA = 10000.0
ATTN_BLOCK = 128
MIX_WIDTH = DN_WIDTH + MLA_WIDTH
IN_SPLITS = (3 * DN_WIDTH, 4 * DN_WIDTH, 4 * DN_WIDTH + DN_HEADS, 4 * DN_WIDTH + 2 * DN_HEADS, 4 * DN_WIDTH + 2 * DN_HEADS + MLA_Q_RANK, 4 * DN_WIDTH + 2 * DN_HEADS + MLA_Q_RANK + MLA_KV_RANK)
IN_WIDTH = IN_SPLITS[-1] + MLA_ROPE
N_EXPERTS = 64
TOP_K = 6
N_GROUPS = 8
TOPK_GROUPS = 4
EXPERT_FF = 512
SHARED_FF = 512
ROUTED_SCALE = 2.5
EXPERT_BLOCK = 256
RMS_EPS = 1e-6
POS_OFFSET_MAX = 4096

kernel_name = "hybrid_deltanet_mla_moe_adaln"


def rms_norm(x, gain):
    xf = x.astype(jnp.float32)
    y = xf * lax.rsqrt(jnp.mean(xf * xf, axis=-1, keepdims=True) + RMS_EPS)
    return (y * gain.astype(jnp.float32)).astype(x.dtype)


def modulate(xn, shift, scale):
    return xn * (1.0 + scale[:, None, :]) + shift[:, None, :]


def l2_normalize(x):
    return x * lax.rsqrt(jnp.sum(x * x, axis=-1, keepdims=True) + 1e-6)


def causal_depthwise_conv(x, w):
    k = w.shape[-1]
    return lax.conv_general_dilated(
        x, w.T[:, None, :].astype(x.dtype), window_strides=(1,), padding=((k - 1, 0),),
        dimension_numbers=('NWC', 'WIO', 'NWC'), feature_group_count=x.shape[-1])


def rotary(x, positions):
    half = x.shape[-1] // 2
    inv_freq = ROPE_THETA ** (-jnp.arange(half, dtype=jnp.float32) / half)
    ang = positions.astype(jnp.float32)[..., None] * inv_freq
    cos = jnp.cos(ang)[:, :, None, :]
    sin = jnp.sin(ang)[:, :, None, :]
    xf = x.astype(jnp.float32)
    x1, x2 = xf[..., :half], xf[..., half:]
    return jnp.concatenate([x1 * cos - x2 * sin, x2 * cos + x1 * sin], axis=-1).astype(x.dtype)


def gated_delta_rule_chunked(q, k, v, g, beta):
    f32 = jnp.float32
    bsz, seq, nh, dk = q.shape
    dv = v.shape[-1]
    n_chunks = seq // DN_CHUNK
    q = l2_normalize(q.astype(f32)) * (dk ** -0.5)
    k = l2_normalize(k.astype(f32))

    def to_chunks(t):
        t = t.reshape((bsz, n_chunks, DN_CHUNK, nh) + t.shape[3:])
        return jnp.moveaxis(t, (1, 3), (0, 2))

    qc, kc, vc = to_chunks(q), to_chunks(k), to_chunks(v.astype(f32))
    gc = jnp.cumsum(to_chunks(g.astype(f32)), axis=-1)
    bc = to_chunks(beta.astype(f32))
    idx = jnp.arange(DN_CHUNK)
    causal = idx[:, None] >= idx[None, :]
    strict = idx[:, None] > idx[None, :]
    decay = jnp.exp(jnp.where(causal, gc[..., :, None] - gc[..., None, :], -jnp.inf))
    kb = kc * bc[..., None]
    m = jnp.where(strict, jnp.einsum('nbhid,nbhjd->nbhij', kb, kc) * decay, 0.0)
    rhs = jnp.concatenate([vc * bc[..., None], kb * jnp.exp(gc)[..., None]], axis=-1)
    sol = lax.linalg.triangular_solve(m + jnp.eye(DN_CHUNK, dtype=f32), rhs, left_side=True, lower=True, unit_diagonal=True)
    u, w = sol[..., :dv], sol[..., dv:]
    a_intra = jnp.where(causal, jnp.einsum('nbhid,nbhjd->nbhij', qc, kc) * decay, 0.0)

    def chunk_step(state, inp):
        q_i, k_i, u_i, w_i, g_i, a_i = inp
        v_new = u_i - jnp.einsum('bhck,bhkv->bhcv', w_i, state)
        o_i = (jnp.einsum('bhck,bhkv->bhcv', q_i * jnp.exp(g_i)[..., None], state)
               + jnp.einsum('bhij,bhjv->bhiv', a_i, v_new))
        g_last = g_i[..., -1:]
        state = (state * jnp.exp(g_last)[..., None]
                 + jnp.einsum('bhck,bhcv->bhkv', k_i * jnp.exp(g_last - g_i)[..., None], v_new))
        return state, o_i

    state0 = jnp.zeros((bsz, nh, dk, dv), f32)
    _, o = lax.scan(chunk_step, state0, (qc, kc, u, w, gc, a_intra))
    return jnp.moveaxis(o, (0, 2), (1, 3)).reshape(bsz, seq, nh, dv)


def causal_block_attention(q, k, v):
    seq = q.shape[1]
    scale = q.shape[-1] ** -0.5
    outs = []
    for blk in range(seq // ATTN_BLOCK):
        q0 = blk * ATTN_BLOCK
        kend = q0 + ATTN_BLOCK
        s = jnp.einsum('bqhd,bkhd->bhqk', q[:, q0:kend], k[:, :kend], preferred_element_type=jnp.float32) * scale
        qpos = q0 + jnp.arange(ATTN_BLOCK)
        kpos = jnp.arange(kend)
        s = jnp.where(kpos[None, :] <= qpos[:, None], s, -jnp.inf)
        p = jax.nn.softmax(s, axis=-1)
        outs.append(jnp.einsum('bhqk,bkhd->bqhd', p.astype(v.dtype), v[:, :kend]))
    return jnp.concatenate(outs, axis=1)


def hybrid_mixer(h, positions, w_in, dn_conv_w, dn_a_log, dn_dt_bias, dn_norm_gain,
                 mla_q_norm_gain, w_q_up, mla_kv_norm_gain, w_kv_up, w_out):
    f32 = jnp.float32
    bsz, seq, _ = h.shape
    dn_qkv, dn_z, dn_b, dn_a, mla_cq, mla_ckv, mla_kpe = jnp.split(h @ w_in, IN_SPLITS, axis=-1)
    hs = (bsz, seq, DN_HEADS, DN_HEAD_DIM)
    qkv = jax.nn.silu(causal_depthwise_conv(dn_qkv, dn_conv_w)).astype(f32)
    q, k, v = [t.reshape(hs) for t in jnp.split(qkv, 3, axis=-1)]
    beta = jax.nn.sigmoid(dn_b.astype(f32))
    g = -jnp.exp(dn_a_log.astype(f32)) * jax.nn.softplus(dn_a.astype(f32) + dn_dt_bias.astype(f32))
    o = gated_delta_rule_chunked(q, k, v, g, beta)
    o = rms_norm(o, dn_norm_gain) * jax.nn.silu(dn_z.astype(f32).reshape(hs))
    dn_out = o.reshape(bsz, seq, DN_WIDTH).astype(h.dtype)
    q_lat = (rms_norm(mla_cq, mla_q_norm_gain) @ w_q_up).reshape(bsz, seq, MLA_HEADS, MLA_NOPE + MLA_ROPE)
    kv = (rms_norm(mla_ckv, mla_kv_norm_gain) @ w_kv_up).reshape(bsz, seq, MLA_HEADS, MLA_NOPE + MLA_V)
    k_nope, v_mla = kv[..., :MLA_NOPE], kv[..., MLA_NOPE:]
    k_pe = jnp.broadcast_to(rotary(mla_kpe[:, :, None, :], positions), (bsz, seq, MLA_HEADS, MLA_ROPE))
    q_mla = jnp.concatenate([q_lat[..., :MLA_NOPE], rotary(q_lat[..., MLA_NOPE:], positions)], axis=-1)
    k_mla = jnp.concatenate([k_nope, k_pe.astype(k_nope.dtype)], axis=-1)
    mla_out = causal_block_attention(q_mla, k_mla, v_mla).reshape(bsz, seq, MLA_WIDTH)
    return jnp.concatenate([dn_out, mla_out.astype(h.dtype)], axis=-1) @ w_out


def moe_ffn(h, w_router, router_bias, w_exp_gate_up, w_exp_down, w_sh_gate_up, w_sh_down):
    f32 = jnp.float32
    bsz, seq, d = h.shape
    n_tok = bsz * seq
    hf = h.reshape(n_tok, d)
    scores = jax.nn.sigmoid(jnp.dot(hf, w_router, preferred_element_type=f32))
    biased = scores + router_bias.astype(f32)
    group_score = lax.top_k(biased.reshape(n_tok, N_GROUPS, N_EXPERTS // N_GROUPS), 2)[0].sum(-1)
    _, top_groups = lax.top_k(group_score, TOPK_GROUPS)
    group_mask = jnp.any(top_groups[:, :, None] == jnp.arange(N_GROUPS)[None, None, :], axis=1)
    expert_mask = jnp.repeat(group_mask, N_EXPERTS // N_GROUPS, axis=-1)
    _, top_idx = lax.top_k(jnp.where(expert_mask, biased, -jnp.inf), TOP_K)
    top_w = jnp.take_along_axis(scores, top_idx, axis=-1)
    top_w = top_w / jnp.sum(top_w, axis=-1, keepdims=True) * ROUTED_SCALE
    n_assign = n_tok * TOP_K
    flat_e = top_idx.reshape(n_assign)
    order = jnp.argsort(flat_e)
    sorted_e = flat_e[order]
    counts = jnp.bincount(flat_e, length=N_EXPERTS)
    padded = (counts + EXPERT_BLOCK - 1) // EXPERT_BLOCK * EXPERT_BLOCK
    pad_end = jnp.cumsum(padded)
    start = jnp.cumsum(counts) - counts
    dest = (pad_end - padded)[sorted_e] + jnp.arange(n_assign) - start[sorted_e]
    n_rows = -(-(n_assign + N_EXPERTS * (EXPERT_BLOCK - 1)) // EXPERT_BLOCK) * EXPERT_BLOCK
    n_blocks = n_rows // EXPERT_BLOCK
    row_tok = jnp.full((n_rows,), n_tok, jnp.int32).at[dest].set((order // TOP_K).astype(jnp.int32))
    row_w = jnp.zeros((n_rows,), f32).at[dest].set(top_w.reshape(n_assign)[order])
    block_e = jnp.minimum(jnp.searchsorted(pad_end, jnp.arange(n_blocks) * EXPERT_BLOCK, side='right'), N_EXPERTS - 1)
    h_pad = jnp.concatenate([hf, jnp.zeros((1, d), hf.dtype)], axis=0)

    def expert_block(acc, blk):
        tok, wts, e = blk
        gate, up = jnp.split(h_pad[tok] @ w_exp_gate_up[e], 2, axis=-1)
        y = (jax.nn.silu(gate) * up) @ w_exp_down[e]
        return acc.at[tok].add(y.astype(f32) * wts[:, None]), None

    acc, _ = lax.scan(expert_block, jnp.zeros((n_tok + 1, d), f32),
                      (row_tok.reshape(n_blocks, EXPERT_BLOCK), row_w.reshape(n_blocks, EXPERT_BLOCK), block_e))
    sg, su = jnp.split(hf @ w_sh_gate_up, 2, axis=-1)
    shared = (jax.nn.silu(sg) * su) @ w_sh_down
    return (acc[:n_tok] + shared.astype(f32)).astype(h.dtype).reshape(bsz, seq, d)


def setup_inputs(seed: int = 0) -> dict:
    key = jax.random.key(seed)
    ks = jax.random.split(key, 24)
    f32 = jnp.float32
    L = DEPTH

    def dense(k, shape, fan_in, gain=1.0):
        return jax.random.normal(k, shape, f32) * (gain * fan_in ** -0.5)

    def norm_gain(k, shape):
        return 1.0 + 0.02 * jax.random.normal(k, shape, f32)

    x = jax.random.normal(ks[0], (BATCH, SEQ, D_MODEL), f32)
    c = jax.random.normal(ks[1], (BATCH, D_MODEL), f32)
    positions = (jax.random.randint(ks[2], (BATCH, 1), 0, POS_OFFSET_MAX, dtype=jnp.int32)
                 + jnp.arange(SEQ, dtype=jnp.int32)[None, :]).astype(jnp.int32)
    w_ada = dense(ks[3], (L, D_MODEL, 6 * D_MODEL), D_MODEL, 0.5)
    b_ada = 0.02 * jax.random.normal(ks[4], (L, 6 * D_MODEL), f32)
    norm1_gain = norm_gain(ks[5], (L, D_MODEL))
    w_in = dense(ks[6], (L, D_MODEL, IN_WIDTH), D_MODEL)
    dn_conv_w = dense(ks[7], (L, 3 * DN_WIDTH, DN_CONV), DN_CONV)
    dn_a_log = jnp.log(jax.random.uniform(ks[8], (L, DN_HEADS), f32, 1.0, 16.0))
    dt = jnp.exp(jax.random.uniform(ks[9], (L, DN_HEADS), f32, math.log(1e-3), math.log(1e-1)))
    dn_dt_bias = dt + jnp.log(-jnp.expm1(-dt))
    dn_norm_gain = norm_gain(ks[10], (L, DN_HEAD_DIM))
    mla_q_norm_gain = norm_gain(ks[11], (L, MLA_Q_RANK))
    w_q_up = dense(ks[12], (L, MLA_Q_RANK, MLA_HEADS * (MLA_NOPE + MLA_ROPE)), MLA_Q_RANK)
    mla_kv_norm_gain = norm_gain(ks[13], (L, MLA_KV_RANK))
    w_kv_up = dense(ks[14], (L, MLA_KV_RANK, MLA_HEADS * (MLA_NOPE + MLA_V)), MLA_KV_RANK)
    w_out = dense(ks[15], (L, MIX_WIDTH, D_MODEL), MIX_WIDTH)
    norm2_gain = norm_gain(ks[16], (L, D_MODEL))
    w_router = dense(ks[17], (L, D_MODEL, N_EXPERTS), D_MODEL)
    router_bias = 0.01 * jax.random.normal(ks[18], (L, N_EXPERTS), f32)
    w_exp_gate_up = dense(ks[19], (L, N_EXPERTS, D_MODEL, 2 * EXPERT_FF), D_MODEL)
    w_exp_down = dense(ks[20], (L, N_EXPERTS, EXPERT_FF, D_MODEL), EXPERT_FF)
    w_sh_gate_up = dense(ks[21], (L, D_MODEL, 2 * SHARED_FF), D_MODEL)
    w_sh_down = dense(ks[22], (L, SHARED_FF, D_MODEL), SHARED_FF)
    final_norm_gain = norm_gain(ks[23], (D_MODEL,))
    return {"x": x, "c": c, "positions": positions, "w_ada": w_ada, "b_ada": b_ada,
            "norm1_gain": norm1_gain, "w_in": w_in, "dn_conv_w": dn_conv_w, "dn_a_log": dn_a_log,
            "dn_dt_bias": dn_dt_bias, "dn_norm_gain": dn_norm_gain, "mla_q_norm_gain": mla_q_norm_gain,
            "w_q_up": w_q_up, "mla_kv_norm_gain": mla_kv_norm_gain, "w_kv_up": w_kv_up, "w_out": w_out,
            "norm2_gain": norm2_gain, "w_router": w_router, "router_bias": router_bias,
            "w_exp_gate_up": w_exp_gate_up, "w_exp_down": w_exp_down, "w_sh_gate_up": w_sh_gate_up,
            "w_sh_down": w_sh_down, "final_norm_gain": final_norm_gain}


def reference(x, c, positions, w_ada, b_ada, norm1_gain, w_in, dn_conv_w, dn_a_log, dn_dt_bias,
              dn_norm_gain, mla_q_norm_gain, w_q_up, mla_kv_norm_gain, w_kv_up, w_out, norm2_gain,
              w_router, router_bias, w_exp_gate_up, w_exp_down, w_sh_gate_up, w_sh_down, final_norm_gain):
    cond = jax.nn.silu(c)
    for layer in range(DEPTH):
        ada = cond @ w_ada[layer] + b_ada[layer]
        shift1, scale1, gate1, shift2, scale2, gate2 = jnp.split(ada, 6, axis=-1)
        h = modulate(rms_norm(x, norm1_gain[layer]), shift1, scale1)
        mix = hybrid_mixer(h, positions, w_in[layer], dn_conv_w[layer], dn_a_log[layer], dn_dt_bias[layer],
                           dn_norm_gain[layer], mla_q_norm_gain[layer], w_q_up[layer], mla_kv_norm_gain[layer],
                           w_kv_up[layer], w_out[layer])
        x = x + gate1[:, None, :] * mix
        h = modulate(rms_norm(x, norm2_gain[layer]), shift2, scale2)
        ffn = moe_ffn(h, w_router[layer], router_bias[layer], w_exp_gate_up[layer], w_exp_down[layer],
                      w_sh_gate_up[layer], w_sh_down[layer])
        x = x + gate2[:, None, :] * ffn
    return rms_norm(x, final_norm_gain)
```

```python
import numpy as np
from contextlib import ExitStack
import concourse.bass as bass
import concourse.mybir as mybir
from concourse.bass_utils import run_bass_kernel_spmd

F32 = mybir.dt.float32
BF16 = mybir.dt.bfloat16
I32 = mybir.dt.int32
ALU = mybir.AluOpType
AF = mybir.ActivationFunctionType
AX = mybir.AxisListType

ENGS = ("pe", "act", "dve", "pool", "sp")
S = 2048
D = 2048
NT = 16
BS = 256
NBLK = 112
NROWS = NBLK * BS
EPS = 1e-6
TWO_PI = 6.283185307179586
PI = 3.141592653589793


class MK:
    NDMA = 20
    LIMIT = 30000

    def __init__(self, nc, es):
        self.nc = nc
        self.es = es
        self.q = {e: [] for e in ENGS}
        self.epoch = {e: 0 for e in ENGS}
        self.sem = {}
        self.cnt = {e: 0 for e in ENGS}
        for e in ENGS:
            self.sem[(e, 0)] = es.enter_context(nc.semaphore(f"s_{e}0"))
        self.dsem = {}
        self.dcnt = {}
        self.dnext = {}
        for e in ("sp", "pool", "act"):
            self.dsem[e] = [es.enter_context(nc.semaphore(f"d_{e}{i}")) for i in range(self.NDMA)]
            self.dcnt[e] = [0] * self.NDMA
            self.dnext[e] = 0
        self.seen = {e: {} for e in ENGS}
        self.last_w = {}
        self.readers = {}
        self.n_inst = 0
        self.pes = None
        self.uid = 0

    def sb(self, name, shape, dt):
        self.uid += 1
        return self.pes.enter_context(self.nc.sbuf_tensor(f"sb{self.uid}_{name}", list(shape), dt))

    def ps(self, name, shape, dt=F32):
        self.uid += 1
        return self.pes.enter_context(self.nc.psum_tensor(f"ps{self.uid}_{name}", list(shape), dt))

    def _semobj(self, key):
        if key[0] == "c":
            return self.sem[(key[1], key[2])]
        return self.dsem[key[1]][key[2]]

    def _deps(self, reads, writes):
        deps = set()
        for k in reads:
            t = self.last_w.get(k)
            if t is not None:
                deps.add(t)
        for k in writes:
            t = self.last_w.get(k)
            if t is not None:
                deps.add(t)
            for t in self.readers.get(k, ()):
                deps.add(t)
        return deps

    def _emit_waits(self, eng, deps):
        seen = self.seen[eng]
        best = {}
        for (key, val) in deps:
            if eng == "pe" and key[0] == "c" and key[1] == "pe":
                continue
            if seen.get(key, 0) >= val:
                continue
            if best.get(key, 0) < val:
                best[key] = val
        for key, val in best.items():
            seen[key] = val
            so = self._semobj(key)
            self.q[eng].append(lambda e, so=so, val=val: e.wait_ge(so, val))
            self.n_inst += 1

    def _commit(self, tok, reads, writes):
        for k in writes:
            self.last_w[k] = tok
            self.readers[k] = []
        for k in reads:
            self.readers.setdefault(k, []).append(tok)

    def op(self, eng, fn, reads=(), writes=()):
        deps = self._deps(reads, writes)
        self._emit_waits(eng, deps)
        if self.cnt[eng] >= self.LIMIT:
            self.epoch[eng] += 1
            self.cnt[eng] = 0
            self.sem[(eng, self.epoch[eng])] = self.es.enter_context(self.nc.semaphore(f"s_{eng}{self.epoch[eng]}"))
        self.cnt[eng] += 1
        so = self.sem[(eng, self.epoch[eng])]
        self.q[eng].append(lambda e, fn=fn, so=so: fn(e).then_inc(so, 1))
        self.n_inst += 1
        tok = (("c", eng, self.epoch[eng]), self.cnt[eng])
        self._commit(tok, reads, writes)
        return tok

    def dma(self, eng, fn, reads=(), writes=()):
        deps = self._deps(reads, writes)
        i = self.dnext[eng]
        self.dnext[eng] = (i + 1) % self.NDMA
        key = ("d", eng, i)
        if self.dcnt[eng][i] > 0:
            deps.add((key, self.dcnt[eng][i]))
        self._emit_waits(eng, deps)
        self.dcnt[eng][i] += 16
        so = self.dsem[eng][i]
        self.q[eng].append(lambda e, fn=fn, so=so: fn(e).then_inc(so, 16))
        self.n_inst += 1
        tok = (key, self.dcnt[eng][i])
        self._commit(tok, reads, writes)
        return tok

    def barrier(self):
        toks = set()
        for e in ENGS:
            if self.cnt[e] > 0:
                toks.add((("c", e, self.epoch[e]), self.cnt[e]))
        for e in self.dsem:
            for i in range(self.NDMA):
                if self.dcnt[e][i] > 0:
                    toks.add((("d", e, i), self.dcnt[e][i]))
        for e in ENGS:
            self._emit_waits(e, toks)

    def flush(self):
        self.barrier()
        nc = self.nc
        q = self.q
        with nc.Block() as block:
            @block.tensor
            def _(e):
                for f in q["pe"]:
                    f(e)

            @block.scalar
            def _(e):
                for f in q["act"]:
                    f(e)

            @block.vector
            def _(e):
                for f in q["dve"]:
                    f(e)

            @block.gpsimd
            def _(e):
                for f in q["pool"]:
                    f(e)

            @block.sync
            def _(e):
                for f in q["sp"]:
                    f(e)
        self.q = {e: [] for e in ENGS}


def AP(t, F, off, dims, npart=128):
    return bass.AP(t, off, [[F, npart]] + [list(d) for d in dims])


def build(upto=99, dbg=False, with_experts=True):
    nc = bass.Bass("TRN2", target_bir_lowering=False)
    dt = nc.dram_tensor
    x_d = dt("x", [S, D], F32, kind="ExternalInput")
    c_d = dt("c", [128, 16], F32, kind="ExternalInput")
    pos_d = dt("positions", [1, S], I32, kind="ExternalInput")
    w_ada_d = dt("w_ada", [D, 6 * D], F32, kind="ExternalInput")
    b_ada_d = dt("b_ada", [1, 6 * D], F32, kind="ExternalInput")
    n1g_d = dt("norm1_gain", [1, D], F32, kind="ExternalInput")
    w_in_d = dt("w_in", [D, 5200], F32, kind="ExternalInput")
    convw_d = dt("dn_conv_w", [128, 24, 4], F32, kind="ExternalInput")
    alog_d = dt("dn_a_log", [1, 8], F32, kind="ExternalInput")
    dtb_d = dt("dn_dt_bias", [1, 8], F32, kind="ExternalInput")
    dng_d = dt("dn_norm_gain", [1, 128], F32, kind="ExternalInput")
    qg_d = dt("mla_q_norm_gain", [128, 4], F32, kind="ExternalInput")
    wqu_d = dt("w_q_up", [512, 1536], F32, kind="ExternalInput")
    kvg_d = dt("mla_kv_norm_gain", [128, 4], F32, kind="ExternalInput")
    wkvu_d = dt("w_kv_up", [512, 2048], F32, kind="ExternalInput")
    wout_d = dt("w_out", [D, D], F32, kind="ExternalInput")
    n2g_d = dt("norm2_gain", [1, D], F32, kind="ExternalInput")
    wr_d = dt("w_router", [D, 64], F32, kind="ExternalInput")
    rb_d = dt("router_bias", [1, 64], F32, kind="ExternalInput")
    if with_experts:
        wegu_d = dt("w_exp_gate_up", [64 * 128 * 4, 4096], F32, kind="ExternalInput")
        wed_d = dt("w_exp_down", [64 * 128 * 2, 4096], F32, kind="ExternalInput")
        wsgu_d = dt("w_sh_gate_up", [128, 16 * 1024], F32, kind="ExternalInput")
        wsd_d = dt("w_sh_down", [128, 4 * 2048], F32, kind="ExternalInput")
    fng_d = dt("final_norm_gain", [1, D], F32, kind="ExternalInput")
    invf_d = dt("invf", [64, 2], F32, kind="ExternalInput")
    out_d = dt("out", [S, D], F32, kind="ExternalOutput")
    ada_d = dt("ada_s", [6, D], F32, kind="Internal")
    qT_d = dt("qT_s", [8, 128, S], F32, kind="Internal")
    kT_d = dt("kT_s", [8, 128, S], F32, kind="Internal")
    k_d = dt("k_s", [S, 8, 128], F32, kind="Internal")
    v_d = dt("v_s", [S, 8, 128], F32, kind="Internal")
    z_d = dt("z_s", [S, 1024], F32, kind="Internal")
    mix_d = dt("mix_s", [S, D], BF16, kind="Internal")
    x1_d = dt("x1_s", [S, D], F32, kind="Internal")
    h2_d = dt("h2_s", [S + 128, D], BF16, kind="Internal")
    rm_d = dt("rm_s", [NROWS, 2], F32, kind="Internal")
    Y_d = dt("Y_s", [NROWS + S, D], BF16, kind="Internal")
    dbg_d = {}

    def dbgout(name, shape, dtype=F32):
        dbg_d[name] = dt("dbg_" + name, list(shape), dtype, kind="ExternalOutput")
        return dbg_d[name]

    es = ExitStack()
    with es:
        mk = MK(nc, es)
        V = lambda fn, r=(), w=(): mk.op("dve", fn, r, w)
        A = lambda fn, r=(), w=(): mk.op("act", fn, r, w)
        P = lambda fn, r=(), w=(): mk.op("pe", fn, r, w)
        G = lambda fn, r=(), w=(): mk.op("pool", fn, r, w)
        DM = lambda eng, fn, r=(), w=(): mk.dma(eng, fn, r, w)

        pers = ExitStack()
        es.enter_context(pers)
        mk.pes = pers
        ident_i = mk.sb("ident_i", [128, 128], I32)
        identf = mk.sb("identf", [128, 128], F32)
        identb = mk.sb("identb", [128, 128], BF16)
        onesf = mk.sb("onesf", [128, 128], F32)
        dif = mk.sb("dif", [128, 128], F32)
        bg_sb = mk.sb("bg_sb", [128, NT, 16], F32)
        G(lambda e: e.iota(ident_i[:], [[1, 128]], base=0, channel_multiplier=-1), w=["ident_i"])
        V(lambda e: e.tensor_copy(out=dif[:], in_=ident_i[:]), ["ident_i"], ["dif"])
        V(lambda e: e.tensor_single_scalar(out=identf[:], in_=dif[:], scalar=0.0, op=ALU.is_equal), ["dif"], ["identf"])
        V(lambda e: e.tensor_copy(out=identb[:], in_=identf[:]), ["identf"], ["identb"])
        G(lambda e: e.memset(onesf[:], 1.0), w=["onesf"])

        if upto >= 0:
            with ExitStack() as pes:
                mk.pes = pes
                c_sb = mk.sb("c_sb", [128, 16], F32)
                sc = mk.sb("sc", [128, 16], F32)
                wa = [mk.sb(f"wa{i}", [128, 16, 512], F32) for i in range(2)]
                arow = mk.sb("arow", [1, 6 * D], F32)
                brow = mk.sb("brow", [1, 6 * D], F32)
                grow = mk.sb("grow", [1, 2 * D], F32)
                psa = [mk.ps(f"psa{i}", [128, 512], F32) for i in range(2)]
                DM("sp", lambda e: e.dma_start(out=c_sb[:], in_=c_d.ap()), w=["c_sb"])
                DM("sp", lambda e: e.dma_start(out=brow[:], in_=b_ada_d.ap()), w=["brow"])
                DM("sp", lambda e: e.dma_start(out=grow[:, 0:D], in_=n1g_d.ap()), w=["grow0"])
                DM("sp", lambda e: e.dma_start(out=grow[:, D:2 * D], in_=n2g_d.ap()), w=["grow1"])
                A(lambda e: e.activation(out=sc[:], in_=c_sb[:], func=AF.Silu), ["c_sb"], ["sc"])
                wsrc = w_ada_d.ap().rearrange("(k p) n -> p k n", p=128)
                for j in range(24):
                    b = j % 2
                    DM("sp", lambda e, j=j, b=b: e.dma_start(out=wa[b][:], in_=wsrc[:, :, j * 512:(j + 1) * 512]), w=[f"wa{b}"])

                    def mm(e, b=b):
                        for k in range(16):
                            r = e.matmul(psa[b][0:1, :], lhsT=sc[:, k:k + 1], rhs=wa[b][:, k, :], start=(k == 0), stop=(k == 15))
                        return r
                    P(mm, ["sc", f"wa{b}"], [f"psa{b}"])
                    V(lambda e, j=j, b=b: e.tensor_tensor(out=arow[:, j * 512:(j + 1) * 512], in0=psa[b][0:1, :], in1=brow[:, j * 512:(j + 1) * 512], op=ALU.add),
                      ["brow"], [f"psa{b}", f"arow{j}"])
                akeys = [f"arow{j}" for j in range(24)]
                V(lambda e: e.scalar_tensor_tensor(out=arow[:, D:2 * D], in0=arow[:, D:2 * D], scalar=1.0, in1=grow[:, 0:D], op0=ALU.add, op1=ALU.mult), akeys + ["grow0"], ["arowG1"])
                V(lambda e: e.scalar_tensor_tensor(out=arow[:, 4 * D:5 * D], in0=arow[:, 4 * D:5 * D], scalar=1.0, in1=grow[:, D:2 * D], op0=ALU.add, op1=ALU.mult), akeys + ["grow1"], ["arowG2"])
                DM("sp", lambda e: e.dma_start(out=ada_d.ap().rearrange("(o r) n -> o (r n)", o=1), in_=arow[:]), akeys + ["arowG1", "arowG2"], ["ada_d"])
                mk.flush()

        p13 = ExitStack()
        es.enter_context(p13)
        if upto >= 1:
            mk.pes = p13
            cqn = mk.sb("cqn", [128, 4, S], BF16)
            ckvn = mk.sb("ckvn", [128, 4, S], BF16)
            KTpe = mk.sb("KTpe", [64, S], BF16)
            CS = mk.sb("CS", [64, S], F32)
            SN = mk.sb("SN", [64, S], F32)
            p12 = ExitStack()
            p13.enter_context(p12)
            mk.pes = p12
            hT = mk.sb("hT", [128, 16, S], BF16)
            with ExitStack() as pes:
                mk.pes = pes
                G1b = mk.sb("G1b", [128, D], F32)
                S1b = mk.sb("S1b", [128, D], F32)
                xt = [mk.sb(f"xt{i}", [128, D], F32) for i in range(2)]
                junk = mk.sb("junk", [128, D], BF16)
                tmpf = mk.sb("tmpf", [128, D], F32)
                hbf = [mk.sb(f"hbf{i}", [128, D], BF16) for i in range(2)]
                st = mk.sb("st", [128, NT, 2], F32)
                pst = [mk.ps(f"pst{i}", [128, 1024], BF16) for i in range(2)]
                DM("sp", lambda e: e.dma_start(out=S1b[:], in_=ada_d.ap()[0:1, :].partition_broadcast(128)), ["ada_d"], ["S1b"])
                DM("sp", lambda e: e.dma_start(out=G1b[:], in_=ada_d.ap()[1:2, :].partition_broadcast(128)), ["ada_d"], ["G1b"])
                for t in range(NT):
                    b = t % 2
                    DM("sp", lambda e, t=t, b=b: e.dma_start(out=xt[b][:], in_=x_d.ap()[t * 128:(t + 1) * 128, :]), w=[f"xt{b}"])
                    A(lambda e, t=t, b=b: e.activation(out=junk[:], in_=xt[b][:], func=AF.Square, accum_out=st[:, t, 0:1]), [f"xt{b}"], ["junk", f"st{t}"])
                    A(lambda e, t=t: e.activation(out=st[:, t, 1:2], in_=st[:, t, 0:1], func=AF.Sqrt, bias=EPS, scale=1.0 / D), [f"st{t}"], [f"st{t}"])
                    V(lambda e, t=t: e.reciprocal(out=st[:, t, 1:2], in_=st[:, t, 1:2]), [f"st{t}"], [f"st{t}"])
                    V(lambda e, t=t, b=b: e.scalar_tensor_tensor(out=tmpf[:], in0=xt[b][:], scalar=st[:, t, 1:2], in1=G1b[:], op0=ALU.mult, op1=ALU.mult), [f"xt{b}", f"st{t}", "G1b"], ["tmpf"])
                    V(lambda e, b=b: e.tensor_tensor(out=hbf[b][:], in0=tmpf[:], in1=S1b[:], op=ALU.add), ["tmpf", "S1b"], [f"hbf{b}"])
                    for hh in range(2):
                        def tr(e, b=b, hh=hh):
                            for k in range(8):
                                kk = hh * 8 + k
                                r = e.transpose(pst[hh][:, k * 128:(k + 1) * 128], hbf[b][:, kk * 128:(kk + 1) * 128], identb[:])
                            return r
                        P(tr, [f"hbf{b}", "identb"], [f"pst{hh}"])
                        eng = V if hh == 0 else A
                        if hh == 0:
                            V(lambda e, t=t, hh=hh: e.tensor_copy(out=hT[:, hh * 8:(hh + 1) * 8, t * 128:(t + 1) * 128], in_=pst[hh][:].rearrange("p (k c) -> p k c", k=8)), [], [f"pst{hh}", f"hT{t}"])
                        else:
                            A(lambda e, t=t, hh=hh: e.copy(out=hT[:, hh * 8:(hh + 1) * 8, t * 128:(t + 1) * 128], in_=pst[hh][:].rearrange("p (k c) -> p k c", k=8)), [], [f"pst{hh}", f"hT{t}b"])
                if dbg and upto == 1:
                    dd = dbgout("hT", [128, 16, S], BF16)
                    DM("sp", lambda e: e.dma_start(out=dd.ap(), in_=hT[:]), [f"hT{t}" for t in range(NT)] + [f"hT{t}b" for t in range(NT)], ["dbg"])
                mk.flush()
        hTkeys = [f"hT{t}" for t in range(NT)] + [f"hT{t}b" for t in range(NT)]

        if upto >= 2:
            wsrc = w_in_d.ap().rearrange("(k p) n -> p k n", p=128)
            with ExitStack() as pes:
                mk.pes = pes
                wb = [mk.sb(f"wb{i}", [128, 16, 512], BF16) for i in range(2)]
                cw = mk.sb("cw", [128, 24, 4], F32)
                raws = [mk.sb(f"raw{i}", [128, 3 + S], F32) for i in range(2)]
                acc = mk.sb("acc", [128, S], F32)
                cs = mk.sb("cs", [128, S], F32)
                sq = mk.sb("sq", [128, S], F32)
                nrm = acc
                tok = mk.sb("tok", [128, NT, 128], F32)
                PQ = mk.ps("PQ", [128, S], F32)
                PN = mk.ps("PN", [128, 1024], F32)
                PT = mk.ps("PT", [128, 1024], F32)
                DM("sp", lambda e: e.dma_start(out=cw[:], in_=convw_d.ap()), w=["cw"])
                for i in range(2):
                    G(lambda e, i=i: e.memset(raws[i][:, 0:3], 0.0), w=[f"rawpad{i}"])

                def wload(blk):
                    b = blk % 2
                    DM("pool", lambda e: e.dma_start(out=wb[b][:], in_=wsrc[:, :, blk * 512:(blk + 1) * 512]), w=[f"wb{b}"])

                def stageA(cc):
                    blk, ci = cc // 4, cc % 4
                    b = blk % 2
                    rb = cc % 2
                    raw = raws[rb]
                    if ci == 0 and blk + 1 < 6:
                        wload(blk + 1)
                    for qd in range(4):
                        def mm(e, qd=qd):
                            for k in range(16):
                                r = e.matmul(PQ[:, qd * 512:(qd + 1) * 512], lhsT=wb[b][:, k, ci * 128:(ci + 1) * 128], rhs=hT[:, k, qd * 512:(qd + 1) * 512], start=(k == 0), stop=(k == 15))
                            return r
                        P(mm, [f"wb{b}"] + hTkeys, [f"PQ{qd}"])
                        A(lambda e, qd=qd: e.copy(out=raw[:, 3 + qd * 512:3 + (qd + 1) * 512], in_=PQ[:, qd * 512:(qd + 1) * 512]), [], [f"PQ{qd}", f"raw{rb}_{qd}"])
                        yield

                def stageB(cc):
                    rb = cc % 2
                    raw = raws[rb]
                    rk = [f"raw{rb}_{q}" for q in range(4)] + [f"rawpad{rb}"]
                    V(lambda e: e.tensor_scalar(out=acc[:], in0=raw[:, 3:3 + S], scalar1=cw[:, cc, 3:4], scalar2=None, op0=ALU.mult), rk + ["cw"], ["acc"])
                    for j in (2, 1, 0):
                        V(lambda e, j=j: e.scalar_tensor_tensor(out=acc[:], in0=raw[:, j:j + S], scalar=cw[:, cc, j:j + 1], in1=acc[:], op0=ALU.mult, op1=ALU.add), rk + ["cw", "acc"], ["acc"])
                    yield
                    A(lambda e: e.activation(out=cs[:], in_=acc[:], func=AF.Silu), ["acc"], ["cs"])
                    yield
                    if cc < 16:
                        head = cc % 8
                        isq = cc < 8
                        A(lambda e: e.activation(out=sq[:], in_=cs[:], func=AF.Square), ["cs"], ["sq"])
                        for hf in range(2):
                            def mm2(e, hf=hf):
                                for q2 in range(2):
                                    r = e.matmul(PN[:, q2 * 512:(q2 + 1) * 512], lhsT=onesf[:], rhs=sq[:, hf * 1024 + q2 * 512: hf * 1024 + (q2 + 1) * 512], start=True, stop=True)
                                return r
                            P(mm2, ["sq", "onesf"], ["PN"])
                            scl = 128.0 if isq else 1.0
                            A(lambda e, hf=hf, scl=scl: e.activation(out=nrm[:, hf * 1024:(hf + 1) * 1024], in_=PN[:], func=AF.Sqrt, bias=EPS * scl, scale=scl), [], ["PN", "acc"])
                            yield
                        V(lambda e: e.reciprocal(out=nrm[:], in_=nrm[:]), ["acc"], ["acc"])
                        V(lambda e: e.tensor_tensor(out=cs[:], in0=cs[:], in1=nrm[:], op=ALU.mult), ["cs", "acc"], ["cs"])
                        dst = qT_d if isq else kT_d
                        DM("sp", lambda e, dst=dst, head=head: e.dma_start(out=dst.ap()[head], in_=cs[:]), ["cs"], [f"qk_d{cc}"])
                        yield
                    if cc >= 8:
                        head = cc % 8
                        dst = k_d if cc < 16 else v_d
                        for g4 in range(4):
                            def tr(e, g4=g4):
                                for i in range(4):
                                    t = g4 * 4 + i
                                    r = e.transpose(PT[:, ((g4 % 2) * 4 + i) * 128:((g4 % 2) * 4 + i + 1) * 128], cs[:, t * 128:(t + 1) * 128], identf[:])
                                return r
                            P(tr, ["cs", "identf"], [f"PT{g4 % 2}"])
                            V(lambda e, g4=g4: e.tensor_copy(out=tok[:, g4 * 4:(g4 + 1) * 4, :], in_=PT[:, (g4 % 2) * 512:((g4 % 2) + 1) * 512].rearrange("p (t c) -> p t c", t=4)), [], [f"PT{g4 % 2}", f"tok{g4}"])
                            yield
                        DM("sp", lambda e, dst=dst, head=head: e.dma_start(out=dst.ap().rearrange("(t p) h c -> p t h c", p=128)[:, :, head, :], in_=tok[:]), [f"tok{g}" for g in range(4)], [f"kv_d{cc}"])
                    yield

                def interleave2(gens):
                    gens = [g for g in gens if g is not None]
                    while gens:
                        for g in list(gens):
                            try:
                                next(g)
                            except StopIteration:
                                gens.remove(g)

                wload(0)
                interleave2([stageA(0)])
                for cc in range(24):
                    interleave2([stageA(cc + 1) if cc + 1 < 24 else None, stageB(cc)])
                mk.flush()
            with ExitStack() as pes:
                mk.pes = pes
                wb = [mk.sb(f"wz{i}", [128, 16, 512], BF16) for i in range(2)]
                wsm = mk.sb("wsm", [128, 16, 16], BF16)
                zs = [mk.sb(f"zs{i}", [128, 512], F32) for i in range(2)]
                dtb = mk.sb("dtb", [128, 8], F32)
                nA = mk.sb("nA", [128, 8], F32)
                tmp8 = mk.sb("tmp8", [128, NT, 8], F32)
                PZ = [mk.ps(f"PZ{i}", [128, 512], F32) for i in range(2)]
                PB = mk.ps("PB", [128, NT * 16], F32)
                for blk in range(2):
                    DM("pool", lambda e, blk=blk: e.dma_start(out=wb[blk][:], in_=wsrc[:, :, 3072 + blk * 512:3072 + (blk + 1) * 512]), w=[f"wz{blk}"])
                DM("pool", lambda e: e.dma_start(out=wsm[:], in_=wsrc[:, :, 4096:4112]), w=["wsm"])
                DM("sp", lambda e: e.dma_start(out=dtb[:], in_=dtb_d.ap().partition_broadcast(128)), w=["dtb"])
                DM("sp", lambda e: e.dma_start(out=nA[:], in_=alog_d.ap().partition_broadcast(128)), w=["nA"])
                i = 0
                for blk in range(2):
                    for t in range(NT):
                        b = i % 2
                        i += 1

                        def mm(e, blk=blk, t=t, b=b):
                            for k in range(16):
                                r = e.matmul(PZ[b][:], lhsT=hT[:, k, t * 128:(t + 1) * 128], rhs=wb[blk][:, k, :], start=(k == 0), stop=(k == 15))
                            return r
                        P(mm, [f"wz{blk}"] + hTkeys, [f"PZ{b}"])
                        A(lambda e, b=b: e.activation(out=zs[b][:], in_=PZ[b][:], func=AF.Silu), [], [f"PZ{b}", f"zs{b}"])
                        DM("sp", lambda e, blk=blk, t=t, b=b: e.dma_start(out=z_d.ap()[t * 128:(t + 1) * 128, blk * 512:(blk + 1) * 512], in_=zs[b][:]), [f"zs{b}"], [f"z_d{blk}_{t}"])
                for t in range(NT):
                    def mm(e, t=t):
                        for k in range(16):
                            r = e.matmul(PB[:, t * 16:(t + 1) * 16], lhsT=hT[:, k, t * 128:(t + 1) * 128], rhs=wsm[:, k, :], start=(k == 0), stop=(k == 15))
                        return r
                    P(mm, ["wsm"] + hTkeys, ["PB"])
                PB3 = PB[:].rearrange("p (t c) -> p t c", t=NT)
                A(lambda e: e.activation(out=bg_sb[:, :, 0:8], in_=PB3[:, :, 0:8], func=AF.Sigmoid), [], ["PB", "bg_b"])
                V(lambda e: e.tensor_tensor(out=tmp8[:], in0=PB3[:, :, 8:16], in1=AP(dtb, 8, 0, [[0, NT], [1, 8]]), op=ALU.add), ["dtb"], ["PB", "tmp8"])
                A(lambda e: e.activation(out=tmp8[:], in_=tmp8[:], func=AF.Exp), ["tmp8"], ["tmp8"])
                A(lambda e: e.activation(out=tmp8[:], in_=tmp8[:], func=AF.Ln, bias=1.0, scale=1.0), ["tmp8"], ["tmp8"])
                A(lambda e: e.activation(out=nA[:], in_=nA[:], func=AF.Exp), ["nA"], ["nA"])
                V(lambda e: e.scalar_tensor_tensor(out=bg_sb[:, :, 8:16], in0=tmp8[:], scalar=-1.0, in1=AP(nA, 8, 0, [[0, NT], [1, 8]]), op0=ALU.mult, op1=ALU.mult), ["tmp8", "nA"], ["bg_g"])
                mk.flush()
            with ExitStack() as pes:
                mk.pes = pes
                wb = [mk.sb(f"wl{i}", [128, 16, 512], BF16) for i in range(2)]
                lat = mk.sb("lat", [128, 4, S], BF16)
                sq = mk.sb("sq2", [128, S], F32)
                rs = mk.sb("rs", [128, S], F32)
                gq = mk.sb("gq", [128, 8], F32)
                PQ = mk.ps("PQ2", [128, S], F32)
                PN = mk.ps("PN2", [128, S], F32)
                DM("sp", lambda e: e.dma_start(out=gq[:, 0:4], in_=qg_d.ap()), w=["gq0"])
                DM("sp", lambda e: e.dma_start(out=gq[:, 4:8], in_=kvg_d.ap()), w=["gq1"])
                for blk in range(2):
                    DM("pool", lambda e, blk=blk: e.dma_start(out=wb[blk][:], in_=wsrc[:, :, 4112 + blk * 512:4112 + (blk + 1) * 512]), w=[f"wl{blk}"])
                    for ci in range(4):
                        for qd in range(4):
                            def mm(e, blk=blk, ci=ci, qd=qd):
                                for k in range(16):
                                    r = e.matmul(PQ[:, qd * 512:(qd + 1) * 512], lhsT=wb[blk][:, k, ci * 128:(ci + 1) * 128], rhs=hT[:, k, qd * 512:(qd + 1) * 512], start=(k == 0), stop=(k == 15))
                                return r
                            P(mm, [f"wl{blk}"] + hTkeys, [f"PQ{qd}"])
                            V(lambda e, ci=ci, qd=qd: e.tensor_copy(out=lat[:, ci, qd * 512:(qd + 1) * 512], in_=PQ[:, qd * 512:(qd + 1) * 512]), [], [f"PQ{qd}", f"lat{ci}_{qd}"])
                            A(lambda e, qd=qd: e.activation(out=sq[:, qd * 512:(qd + 1) * 512], in_=PQ[:, qd * 512:(qd + 1) * 512], func=AF.Square), [], [f"PQ{qd}", f"sq{qd}"])
                            P(lambda e, ci=ci, qd=qd: e.matmul(PN[:, qd * 512:(qd + 1) * 512], lhsT=onesf[:], rhs=sq[:, qd * 512:(qd + 1) * 512], start=(ci == 0), stop=(ci == 3)), [f"sq{qd}", "onesf"], [f"PN{qd}"])
                    for qd in range(4):
                        A(lambda e, qd=qd: e.activation(out=rs[:, qd * 512:(qd + 1) * 512], in_=PN[:, qd * 512:(qd + 1) * 512], func=AF.Sqrt, bias=EPS, scale=1.0 / 512), [], [f"PN{qd}", f"rs{qd}"])
                        V(lambda e, qd=qd: e.reciprocal(out=rs[:, qd * 512:(qd + 1) * 512], in_=rs[:, qd * 512:(qd + 1) * 512]), [f"rs{qd}"], [f"rs{qd}"])
                    dstn = cqn if blk == 0 else ckvn
                    for ci in range(4):
                        V(lambda e, blk=blk, ci=ci, dstn=dstn: e.scalar_tensor_tensor(out=dstn[:, ci, :], in0=lat[:, ci, :], scalar=gq[:, blk * 4 + ci:blk * 4 + ci + 1], in1=rs[:], op0=ALU.mult, op1=ALU.mult),
                          [f"lat{ci}_{q}" for q in range(4)] + [f"rs{q}" for q in range(4)] + ["gq0", "gq1"], [f"latn{blk}_{ci}"])
                mk.flush()
            with ExitStack() as pes:
                mk.pes = pes
                wk = mk.sb("wk", [128, 16, 64], BF16)
                wks = mk.sb("wks", [128, 16, 64], BF16)
                posi = mk.sb("posi", [64, S], I32)
                ang = mk.sb("ang", [64, S], F32)
                kf = mk.sb("kf", [64, S], F32)
                ki = mk.sb("ki", [64, S], I32)
                invf = mk.sb("invf", [64, 2], F32)
                t1 = mk.sb("t1", [64, S], F32)
                PA = mk.ps("PA", [128, S], F32)
                PBm = mk.ps("PBm", [128, S], F32)
                DM("pool", lambda e: e.dma_start(out=wk[:], in_=wsrc[:, :, 5136:5200]), w=["wk"])
                DM("pool", lambda e: e.dma_start(out=wks[:, :, 0:32], in_=wsrc[:, :, 5168:5200]), w=["wks0"])
                DM("pool", lambda e: e.dma_start(out=wks[:, :, 32:64], in_=wsrc[:, :, 5136:5168]), w=["wks1"])
                DM("sp", lambda e: e.dma_start(out=invf[:], in_=invf_d.ap()), w=["invf"])
                DM("sp", lambda e: e.dma_start(out=posi[:], in_=pos_d.ap().partition_broadcast(64)), w=["posi"])
                V(lambda e: e.tensor_copy(out=ang[:], in_=posi[:]), ["posi"], ["ang"])
                V(lambda e: e.tensor_scalar(out=ang[:], in0=ang[:], scalar1=invf[:, 0:1], scalar2=None, op0=ALU.mult), ["ang", "invf"], ["ang"])
                for which, tab, shift in (("sin", SN, 0.0), ("cos", CS, PI / 2)):
                    V(lambda e, shift=shift: e.tensor_scalar(out=kf[:], in0=ang[:], scalar1=shift, scalar2=1.0 / TWO_PI, op0=ALU.add, op1=ALU.mult), ["ang"], ["kf"])
                    V(lambda e: e.tensor_copy(out=ki[:], in_=kf[:]), ["kf"], ["ki"])
                    V(lambda e: e.tensor_copy(out=kf[:], in_=ki[:]), ["ki"], ["kf"])
                    V(lambda e, shift=shift: e.scalar_tensor_tensor(out=kf[:], in0=kf[:], scalar=-TWO_PI, in1=ang[:], op0=ALU.mult, op1=ALU.add), ["kf", "ang"], ["kf"])
                    V(lambda e, shift=shift: e.tensor_scalar(out=kf[:], in0=kf[:], scalar1=shift, scalar2=PI, op0=ALU.add, op1=ALU.min), ["kf"], ["kf"])
                    V(lambda e: e.tensor_scalar(out=kf[:], in0=kf[:], scalar1=-PI, scalar2=None, op0=ALU.max), ["kf"], ["kf"])
                    A(lambda e, tab=tab: e.activation(out=tab[:], in_=kf[:], func=AF.Sin), ["kf"], [which])
                V(lambda e: e.tensor_scalar(out=SN[:], in0=SN[:], scalar1=invf[:, 1:2], scalar2=None, op0=ALU.mult), ["sin", "invf"], ["sin"])
                for qd in range(4):
                    def mm(e, qd=qd):
                        for k in range(16):
                            r = e.matmul(PA[0:64, qd * 512:(qd + 1) * 512], lhsT=wk[:, k, :], rhs=hT[:, k, qd * 512:(qd + 1) * 512], start=(k == 0), stop=(k == 15))
                        return r
                    P(mm, ["wk"] + hTkeys, [f"PA{qd}"])

                    def mm2(e, qd=qd):
                        for k in range(16):
                            r = e.matmul(PBm[0:64, qd * 512:(qd + 1) * 512], lhsT=wks[:, k, :], rhs=hT[:, k, qd * 512:(qd + 1) * 512], start=(k == 0), stop=(k == 15))
                        return r
                    P(mm2, ["wks0", "wks1"] + hTkeys, [f"PB{qd}"])
                    sl = slice(qd * 512, (qd + 1) * 512)
                    V(lambda e, sl=sl: e.tensor_tensor(out=t1[:, sl], in0=PA[0:64, sl], in1=CS[:, sl], op=ALU.mult), ["cos"], [f"PA{qd}", f"t1{qd}"])
                    V(lambda e, sl=sl: e.tensor_tensor(out=kf[:, sl], in0=PBm[0:64, sl], in1=SN[:, sl], op=ALU.mult), ["sin"], [f"PB{qd}", "kf"])
                    V(lambda e, sl=sl: e.tensor_tensor(out=KTpe[:, sl], in0=t1[:, sl], in1=kf[:, sl], op=ALU.add), [f"t1{qd}", "kf"], [f"KTpe{qd}"])
                if dbg and upto == 2:
                    d1 = dbgout("cqn", [128, 4, S], BF16)
                    d2 = dbgout("KTpe", [64, S], BF16)
                    d3 = dbgout("bg", [128, NT, 16], F32)
                    d4 = dbgout("ckvn", [128, 4, S], BF16)
                    mk.barrier()
                    DM("sp", lambda e: e.dma_start(out=d4.ap(), in_=ckvn[:]), [], ["dbg4"])
                    for nm, src, shp in (("qT", qT_d, [8, 128, S]), ("kT", kT_d, [8, 128, S]), ("k", k_d, [S, 8, 128]), ("v", v_d, [S, 8, 128]), ("z", z_d, [S, 1024]), ("ada", ada_d, [6, D])):
                        dd = dbgout(nm, shp, F32)
                        DM("sp", lambda e, dd=dd, src=src: e.dma_start(out=dd.ap(), in_=src.ap()), [], ["dbg_" + nm])
                    DM("sp", lambda e: e.dma_start(out=d1.ap(), in_=cqn[:]), [], ["dbg1"])
                    DM("sp", lambda e: e.dma_start(out=d2.ap(), in_=KTpe[:]), [], ["dbg2"])
                    DM("sp", lambda e: e.dma_start(out=d3.ap(), in_=bg_sb[:]), [], ["dbg3"])
                mk.flush()
            p12.close()

        if upto >= 3:
            with ExitStack() as pes:
                mk.pes = pes
                SC = 192.0 ** -0.5
                wq = mk.sb("wq", [128, 4, 1536], BF16)
                wqs = mk.sb("wqs", [128, 4, 8, 64], BF16)
                wkv = mk.sb("wkv", [128, 4, 2048], BF16)
                QTn = mk.sb("QTn", [128, S], BF16)
                QTp = mk.sb("QTp", [64, S], BF16)
                KTn = mk.sb("KTn", [128, S], BF16)
                Vaug = mk.sb("Vaug", [128, NT, 130], BF16)
                PTs = [mk.sb(f"PTs{i}", [128, 512], BF16) for i in range(4)]
                it0 = [0]
                mla_tok = mk.sb("mla_tok", [128, NT, 1024], BF16)
                t1 = mk.sb("t1m", [64, S], F32)
                t2 = mk.sb("t2m", [64, S], F32)
                triu = mk.sb("triu", [128, 128], BF16)
                rec = mk.sb("rec", [128, 4], F32)
                P0 = mk.ps("P0", [128, S], F32)
                P1 = mk.ps("P1", [128, S], F32)
                DM("pool", lambda e: e.dma_start(out=wq[:], in_=wqu_d.ap().rearrange("(k p) n -> p k n", p=128)), w=["wq"])
                DM("pool", lambda e: e.dma_start(out=wkv[:], in_=wkvu_d.ap().rearrange("(k p) n -> p k n", p=128)), w=["wkv"])
                src4 = wqu_d.ap().rearrange("(k p) (h c) -> p k h c", p=128, c=192)
                for k4 in range(4):
                    DM("pool", lambda e, k4=k4: e.dma_start(out=wqs[:, k4, :, 0:32], in_=src4[:, k4, :, 160:192]), w=[f"wqs0_{k4}"])
                    DM("pool", lambda e, k4=k4: e.dma_start(out=wqs[:, k4, :, 32:64], in_=src4[:, k4, :, 128:160]), w=[f"wqs1_{k4}"])
                V(lambda e: e.tensor_single_scalar(out=triu[:], in_=dif[:], scalar=0.0, op=ALU.is_ge), ["dif"], ["triu"])
                G(lambda e: e.memset(Vaug[:, :, 128:130], 1.0), w=["Vaug1"])
                latq = [f"latn0_{c}" for c in range(4)]
                latkv = [f"latn1_{c}" for c in range(4)]
                it = 0
                for h in range(8):
                    for qd in range(4):
                        sl = slice(qd * 512, (qd + 1) * 512)

                        def mm(e, h=h, sl=sl):
                            for k in range(4):
                                r = e.matmul(P0[:, sl], lhsT=wq[:, k, h * 192:h * 192 + 128], rhs=cqn[:, k, sl], start=(k == 0), stop=(k == 3))
                            return r
                        P(mm, ["wq"] + latq, [f"P0_{qd}"])
                        A(lambda e, sl=sl: e.mul(out=QTn[:, sl], in_=P0[:, sl], mul=SC), [], [f"P0_{qd}", f"QTn{qd}"])
                    for qd in range(4):
                        sl = slice(qd * 512, (qd + 1) * 512)

                        def mma(e, h=h, sl=sl):
                            for k in range(4):
                                r = e.matmul(P0[0:64, sl], lhsT=wq[:, k, h * 192 + 128:h * 192 + 192], rhs=cqn[:, k, sl], start=(k == 0), stop=(k == 3))
                            return r
                        P(mma, ["wq"] + latq, [f"P0_{qd}"])

                        def mmb(e, h=h, sl=sl):
                            for k in range(4):
                                r = e.matmul(P1[0:64, sl], lhsT=wqs[:, k, h, :], rhs=cqn[:, k, sl], start=(k == 0), stop=(k == 3))
                            return r
                        P(mmb, [f"wqs{a}_{b}" for a in range(2) for b in range(4)] + latq, [f"P1_{qd}"])
                        V(lambda e, sl=sl: e.scalar_tensor_tensor(out=t1[:, sl], in0=P0[0:64, sl], scalar=SC, in1=CS[:, sl], op0=ALU.mult, op1=ALU.mult), ["cos"], [f"P0_{qd}", f"t1m{qd}"])
                        V(lambda e, sl=sl: e.scalar_tensor_tensor(out=t2[:, sl], in0=P1[0:64, sl], scalar=SC, in1=SN[:, sl], op0=ALU.mult, op1=ALU.mult), ["sin"], [f"P1_{qd}", f"t2m{qd}"])
                        V(lambda e, sl=sl: e.tensor_tensor(out=QTp[:, sl], in0=t1[:, sl], in1=t2[:, sl], op=ALU.add), [f"t1m{qd}", f"t2m{qd}"], [f"QTp{qd}"])
                    for qd in range(4):
                        sl = slice(qd * 512, (qd + 1) * 512)

                        def mmk(e, h=h, sl=sl):
                            for k in range(4):
                                r = e.matmul(P0[:, sl], lhsT=wkv[:, k, h * 256:h * 256 + 128], rhs=ckvn[:, k, sl], start=(k == 0), stop=(k == 3))
                            return r
                        P(mmk, ["wkv"] + latkv, [f"P0_{qd}"])
                        V(lambda e, sl=sl: e.tensor_copy(out=KTn[:, sl], in_=P0[:, sl]), [], [f"P0_{qd}", f"KTn{qd}"])
                    for g4 in range(4):
                        def mmv(e, h=h, g4=g4):
                            for i in range(4):
                                t = g4 * 4 + i
                                for k in range(4):
                                    r = e.matmul(P1[:, t * 128:(t + 1) * 128], lhsT=ckvn[:, k, t * 128:(t + 1) * 128], rhs=wkv[:, k, h * 256 + 128:h * 256 + 256], start=(k == 0), stop=(k == 3))
                            return r
                        P(mmv, ["wkv"] + latkv, [f"P1_{g4}"])
                        A(lambda e, g4=g4: e.copy(out=Vaug[:, g4 * 4:(g4 + 1) * 4, 0:128], in_=P1[:, g4 * 512:(g4 + 1) * 512].rearrange("p (t c) -> p t c", t=4)), [], [f"P1_{g4}", f"Vaug_{g4}"])
                    qk = [f"QTn{q}" for q in range(4)] + [f"QTp{q}" for q in range(4)] + [f"KTn{q}" for q in range(4)] + [f"KTpe{q}" for q in range(4)]
                    vk = [f"Vaug_{g}" for g in range(4)] + ["Vaug1"]
                    for Gq in range(4):
                        its = []
                        for j in range(4 * Gq + 4):
                            qlo = max(Gq * 512, j * 128)
                            its.append((j, qlo, (Gq + 1) * 512 - qlo))
                        LA = 3

                        def emit_s(ii, its=its):
                            j, qlo, width = its[ii]
                            sbk = (ii + it0[0]) % 4

                            def mms(e):
                                e.matmul(P0[:, sbk * 512:sbk * 512 + width], lhsT=KTn[:, j * 128:(j + 1) * 128], rhs=QTn[:, qlo:qlo + width], start=True, stop=False)
                                return e.matmul(P0[:, sbk * 512:sbk * 512 + width], lhsT=KTpe[:, j * 128:(j + 1) * 128], rhs=QTp[:, qlo:qlo + width], start=False, stop=True)
                            P(mms, qk, [f"P0_{sbk}"])
                            A(lambda e: e.activation(out=PTs[sbk][:, 0:width], in_=P0[:, sbk * 512:sbk * 512 + width], func=AF.Exp), [], [f"P0_{sbk}", f"PTs{sbk}"])
                            if j >= 4 * Gq:
                                V(lambda e: e.tensor_tensor(out=PTs[sbk][:, 0:128], in0=PTs[sbk][:, 0:128], in1=triu[:], op=ALU.mult), ["triu", f"PTs{sbk}"], [f"PTs{sbk}"])

                        def emit_pv(ii, its=its, Gq=Gq):
                            j, qlo, width = its[ii]
                            sbk = (ii + it0[0]) % 4
                            qbs = list(range(qlo // 128, (Gq + 1) * 4))

                            def mmpv(e):
                                for qb in qbs:
                                    a = qb - 4 * Gq
                                    off = qb * 128 - qlo
                                    r = e.matmul(P1[:, a * 512:a * 512 + 129], lhsT=PTs[sbk][:, off:off + 128], rhs=Vaug[:, j, 0:129], start=(j == 0), stop=(j == qb))
                                return r
                            P(mmpv, [f"PTs{sbk}"] + vk, [f"P1_{qb - 4 * Gq}" for qb in qbs])

                        n_it = len(its)
                        for ii in range(min(LA, n_it)):
                            emit_s(ii)
                        for ii in range(n_it):
                            if ii + LA < n_it:
                                emit_s(ii + LA)
                            emit_pv(ii)
                        it0[0] += n_it
                        for a in range(4):
                            qb = 4 * Gq + a
                            V(lambda e, a=a: e.reciprocal(out=rec[:, a:a + 1], in_=P1[:, a * 512 + 128:a * 512 + 129]), [], [f"P1_{a}", f"rec{a}"])
                            V(lambda e, a=a, qb=qb, h=h: e.tensor_scalar(out=mla_tok[:, qb, h * 128:(h + 1) * 128], in0=P1[:, a * 512:a * 512 + 128], scalar1=rec[:, a:a + 1], scalar2=None, op0=ALU.mult), [f"rec{a}"], [f"P1_{a}", f"mla_tok{h}_{qb}"])
                mk.barrier()
                DM("sp", lambda e: e.dma_start(out=mix_d.ap().rearrange("(t p) c -> p t c", p=128)[:, :, 1024:2048], in_=mla_tok[:]), [], ["mix_mla"])
                if dbg and upto == 3:
                    dd = dbgout("mla", [128, NT, 1024], BF16)
                    DM("sp", lambda e: e.dma_start(out=dd.ap(), in_=mla_tok[:]), [], ["dbgm"])
                mk.flush()
        p13.close()

        if upto >= 4:
            with ExitStack() as pes:
                mk.pes = pes
                H8 = 8
                Uincl = mk.sb("Uincl", [128, 128], F32)
                offd = mk.sb("offd", [128, 128], F32)
                M2rep = mk.sb("M2rep", [128, 4, 128], F32)
                gdn = mk.sb("gdn", [128, 128], F32)
                Sst = mk.sb("Sst", [128, H8, 128], F32)
                ld = [[mk.sb(f"ld{n}{i}", [128, H8, 128], F32) for n in range(5)] for i in range(2)]
                gsmq = [mk.sb(f"gsm{i}", [128, 16], F32) for i in range(2)]
                smq = [mk.sb(f"sm{i}", [128, 6, 8], F32) for i in range(2)]
                rhsD = mk.sb("rhsD", [128, H8, 128], F32)
                E2 = mk.sb("E2", [128, H8, 128], F32)
                Amat = mk.sb("Amat", [128, H8, 128], F32)
                Atq = [mk.sb(f"At{i}", [128, H8, 128], F32) for i in range(2)]
                Pq = [mk.sb(f"Pq{i}", [128, H8, 128], F32) for i in range(2)]
                Ptq = [mk.sb(f"Ptq{i}", [128, H8, 128], F32) for i in range(2)]
                Xq0 = [mk.sb(f"Xq0{i}", [128, H8, 128], F32) for i in range(2)]
                Xq1 = mk.sb("Xq1", [128, H8, 128], F32)
                vbq = [mk.sb(f"vb{i}", [128, H8, 128], F32) for i in range(2)]
                kbgq = [mk.sb(f"kbg{i}", [128, H8, 128], F32) for i in range(2)]
                kdecq = [mk.sb(f"kdec{i}", [128, H8, 128], F32) for i in range(2)]
                nwT = mk.sb("nwT", [128, H8, 128], F32)
                vn = mk.sb("vn", [128, H8, 128], F32)
                osb = mk.sb("osb", [128, H8, 128], F32)
                sqo = mk.sb("sqo", [128, H8, 128], F32)
                mixdn = [mk.sb(f"mixdn{i}", [128, 1024], BF16) for i in range(2)]
                PA_ = mk.ps("PA_", [128, 1024], F32)
                PB_ = mk.ps("PB_", [128, 1024], F32)
                PC_ = mk.ps("PC_", [128, 1024], F32)
                PD_ = mk.ps("PD_", [128, 1024], F32)
                V(lambda e: e.tensor_single_scalar(out=Uincl[:], in_=dif[:], scalar=0.0, op=ALU.is_ge), ["dif"], ["Uincl"])
                V(lambda e: e.tensor_single_scalar(out=offd[:], in_=dif[:], scalar=0.0, op=ALU.not_equal), ["dif"], ["offd"])
                for a in range(4):
                    V(lambda e, a=a: e.tensor_single_scalar(out=M2rep[:, a, :], in_=dif[:], scalar=0.0, op=ALU.is_gt), ["dif"], [f"M2rep{a}"])
                    V(lambda e, a=a: e.tensor_single_scalar(out=M2rep[:, a, :], in_=M2rep[:, a, :], scalar=1.0e4, op=ALU.mult), [f"M2rep{a}"], [f"M2rep{a}"])
                M2k = [f"M2rep{a}" for a in range(4)]
                DM("sp", lambda e: e.dma_start(out=gdn[:], in_=dng_d.ap().partition_broadcast(128)), w=["gdn"])
                G(lambda e: e.memset(Sst[:], 0.0), w=["Sst"])
                F = H8 * 128

                def bc_col(t, Ft, col0):
                    return AP(t, Ft, col0, [[1, H8], [0, 128]])

                def bc_mat(t):
                    return AP(t, 128, 0, [[0, H8], [1, 128]])

                def flat(t):
                    return t[:].rearrange("p h c -> p (h c)")

                def ph(ps, h):
                    return ps[:, h * 128:(h + 1) * 128]

                def ps3(ps):
                    return ps[:].rearrange("p (h c) -> p h c", h=H8)

                def pk(name):
                    return [name + "0", name + "1"]

                def solve(c):
                    b = c % 2
                    qTc, kTc, ktok, vtok, zc = ld[b]
                    sm = smq[b]
                    gsm = gsmq[b]
                    At = Atq[b]
                    kbg = kbgq[b]
                    vb = vbq[b]
                    kdec = kdecq[b]
                    Xq = [Xq0[b], Xq1]
                    SM = f"sm{b}_"
                    sl = slice(c * 128, (c + 1) * 128)
                    DM("sp", lambda e: e.dma_start(out=qTc[:], in_=qT_d.ap().rearrange("h d s -> d h s")[:, :, sl]), [], [f"qTc{b}"])
                    DM("sp", lambda e: e.dma_start(out=kTc[:], in_=kT_d.ap().rearrange("h d s -> d h s")[:, :, sl]), [], [f"kTc{b}"])
                    DM("sp", lambda e: e.dma_start(out=ktok[:], in_=k_d.ap()[sl]), [], [f"ktok{b}"])
                    DM("sp", lambda e: e.dma_start(out=vtok[:], in_=v_d.ap()[sl]), [], [f"vtok{b}"])
                    DM("sp", lambda e: e.dma_start(out=flat(zc), in_=z_d.ap()[sl, :]), [], [f"zc{b}"])

                    def mm1(e):
                        e.matmul(PC_[:, 0:8], lhsT=Uincl[:], rhs=bg_sb[:, c, 8:16], start=True, stop=True)
                        return e.matmul(PC_[:, 8:16], lhsT=onesf[:], rhs=bg_sb[:, c, 8:16], start=True, stop=True)
                    P(mm1, ["Uincl", "onesf", "bg_g"], ["PC_0"])
                    V(lambda e: e.tensor_copy(out=gsm[:], in_=PC_[:, 0:16]), [], ["PC_0", f"gsm{b}"])
                    A(lambda e: e.activation(out=sm[:, 0, :], in_=gsm[:, 0:8], func=AF.Exp), [f"gsm{b}"], [SM + "0"])
                    A(lambda e: e.activation(out=sm[:, 1, :], in_=gsm[:, 8:16], func=AF.Exp), [f"gsm{b}"], [SM + "1"])
                    V(lambda e: e.tensor_tensor(out=sm[:, 2, :], in0=gsm[:, 8:16], in1=gsm[:, 0:8], op=ALU.subtract), [f"gsm{b}"], [SM + "2"])
                    A(lambda e: e.activation(out=sm[:, 2, :], in_=sm[:, 2, :], func=AF.Exp), [SM + "2"], [SM + "2"])
                    V(lambda e: e.tensor_tensor(out=sm[:, 3, :], in0=bg_sb[:, c, 0:8], in1=sm[:, 0, :], op=ALU.mult), ["bg_b", SM + "0"], [SM + "3"])
                    V(lambda e: e.tensor_single_scalar(out=sm[:, 4, :], in_=bg_sb[:, c, 0:8], scalar=-1.0, op=ALU.mult), ["bg_b"], [SM + "4"])
                    V(lambda e: e.tensor_tensor(out=rhsD[:], in0=bc_mat(identf), in1=bc_col(gsm, 16, 0), op=ALU.mult), ["identf", f"gsm{b}"], ["rhsD"])
                    yield
                    for hf in range(2):
                        def mm3(e, hf=hf):
                            e.matmul(PA_[:, hf * 512:(hf + 1) * 512], lhsT=onesf[:], rhs=flat(rhsD)[:, hf * 512:(hf + 1) * 512], start=True, stop=False)
                            return e.matmul(PA_[:, hf * 512:(hf + 1) * 512], lhsT=identf[:], rhs=M2rep[:].rearrange("p a c -> p (a c)"), start=False, stop=True)
                        P(mm3, ["onesf", "identf", "rhsD"] + M2k, [f"PA_{hf}"])
                    for h in range(H8):
                        A(lambda e, h=h: e.activation(out=E2[:, h, :], in_=ph(PA_, h), func=AF.Exp, bias=gsm[:, h:h + 1], scale=-1.0), [f"gsm{b}"], [f"PA_{h // 4}", f"E2_{h}"])
                    E2k = [f"E2_{h}" for h in range(H8)]
                    for hf in range(2):
                        def mm4(e, hf=hf):
                            for h in range(hf * 4, hf * 4 + 4):
                                r = e.matmul(ph(PB_, h), lhsT=qTc[:, h, :], rhs=kTc[:, h, :], start=True, stop=True)
                            return r
                        P(mm4, [f"qTc{b}", f"kTc{b}"], [f"PB_{hf}"])

                        def mm5(e, hf=hf):
                            for h in range(hf * 4, hf * 4 + 4):
                                r = e.matmul(ph(PC_, h), lhsT=kTc[:, h, :], rhs=kTc[:, h, :], start=True, stop=True)
                            return r
                        P(mm5, [f"kTc{b}"], [f"PC_{hf}"])
                    yield
                    V(lambda e: e.tensor_tensor(out=Amat[:], in0=ps3(PB_), in1=E2[:], op=ALU.mult), E2k, pk("PB_") + ["Amat"])
                    V(lambda e: e.tensor_tensor(out=Pq[0][:], in0=ps3(PC_), in1=bc_mat(offd), op=ALU.mult), ["offd"], pk("PC_") + ["Pq0"])
                    V(lambda e: e.tensor_tensor(out=Pq[0][:], in0=Pq[0][:], in1=E2[:], op=ALU.mult), E2k + ["Pq0"], ["Pq0"])
                    V(lambda e: e.tensor_tensor(out=Pq[0][:], in0=Pq[0][:], in1=AP(sm, 48, 32, [[1, H8], [0, 128]]), op=ALU.mult), [SM + "4", "Pq0"], ["Pq0"])
                    for hf in range(2):
                        def tr1(e, hf=hf):
                            for h in range(hf * 4, hf * 4 + 4):
                                r = e.transpose(ph(PA_, h), Amat[:, h, :], identf[:])
                            return r
                        P(tr1, ["Amat", "identf"], [f"PA_{hf}"])

                        def tr2(e, hf=hf):
                            for h in range(hf * 4, hf * 4 + 4):
                                r = e.transpose(ph(PB_, h), Pq[0][:, h, :], identf[:])
                            return r
                        P(tr2, ["Pq0", "identf"], [f"PB_{hf}"])
                    yield
                    A(lambda e: e.copy(out=flat(At), in_=PA_[:]), [], pk("PA_") + [f"At{b}"])
                    V(lambda e: e.tensor_copy(out=flat(Ptq[0]), in_=PB_[:]), [], pk("PB_") + ["Ptq0"])
                    V(lambda e: e.tensor_tensor(out=Xq[0][:], in0=Ptq[0][:], in1=bc_mat(identf), op=ALU.add), ["Ptq0", "identf"], [f"Xq0{b}"])
                    xkey = [f"Xq0{b}", "Xq1"]
                    G(lambda e: e.tensor_tensor(out=vb[:], in0=vtok[:], in1=AP(bg_sb, NT * 16, c * 16, [[1, H8], [0, 128]]), op=ALU.mult), [f"vtok{b}", "bg_b"], [f"vb{b}"])
                    G(lambda e: e.tensor_tensor(out=kbg[:], in0=ktok[:], in1=AP(sm, 48, 24, [[1, H8], [0, 128]]), op=ALU.mult), [f"ktok{b}", SM + "3"], [f"kbg{b}"])
                    G(lambda e: e.tensor_tensor(out=kdec[:], in0=ktok[:], in1=AP(sm, 48, 16, [[1, H8], [0, 128]]), op=ALU.mult), [f"ktok{b}", SM + "2"], [f"kdec{b}"])
                    cur = 0
                    for kk in range(1, 7):
                        nx = 1 - cur
                        for hf in range(2):
                            def d1(e, hf=hf, cur=cur):
                                for h in range(hf * 4, hf * 4 + 4):
                                    r = e.matmul(ph(PA_, h), lhsT=Ptq[cur][:, h, :], rhs=Pq[cur][:, h, :], start=True, stop=True)
                                return r
                            P(d1, [f"Ptq{cur}", f"Pq{cur}"], [f"PA_{hf}"])
                        if kk < 6:
                            for hf in range(2):
                                def d2(e, hf=hf, cur=cur):
                                    for h in range(hf * 4, hf * 4 + 4):
                                        r = e.matmul(ph(PB_, h), lhsT=Pq[cur][:, h, :], rhs=Ptq[cur][:, h, :], start=True, stop=True)
                                    return r
                                P(d2, [f"Ptq{cur}", f"Pq{cur}"], [f"PB_{hf}"])
                        A(lambda e, nx=nx: e.copy(out=flat(Pq[nx]), in_=PA_[:]), [], pk("PA_") + [f"Pq{nx}"])
                        if kk < 6:
                            V(lambda e, nx=nx: e.tensor_copy(out=flat(Ptq[nx]), in_=PB_[:]), [], pk("PB_") + [f"Ptq{nx}"])
                        yield
                        for hf in range(2):
                            def d3(e, hf=hf, cur=cur, nx=nx):
                                for h in range(hf * 4, hf * 4 + 4):
                                    r = e.matmul(ph(PC_, h), lhsT=Pq[nx][:, h, :], rhs=Xq[cur][:, h, :], start=True, stop=True)
                                return r
                            P(d3, [f"Pq{nx}", xkey[cur]], [f"PC_{hf}"])
                        V(lambda e, cur=cur, nx=nx: e.tensor_tensor(out=flat(Xq[nx]), in0=PC_[:], in1=flat(Xq[cur]), op=ALU.add), [xkey[cur]], pk("PC_") + [xkey[nx]])
                        cur = nx
                        yield
                    assert cur == 0

                def apply(c):
                    b = c % 2
                    qTc, kTc, ktok, vtok, zc = ld[b]
                    sm = smq[b]
                    At = Atq[b]
                    kbg = kbgq[b]
                    vb = vbq[b]
                    kdec = kdecq[b]
                    X = Xq0[b]
                    Xk = f"Xq0{b}"
                    SM = f"sm{b}_"
                    sl = slice(c * 128, (c + 1) * 128)
                    for hf in range(2):
                        def w1(e, hf=hf):
                            for h in range(hf * 4, hf * 4 + 4):
                                r = e.matmul(ph(PD_, h), lhsT=kbg[:, h, :], rhs=X[:, h, :], start=True, stop=True)
                            return r
                        P(w1, [f"kbg{b}", Xk], [f"PD_{hf}"])
                    A(lambda e: e.mul(out=flat(nwT), in_=PD_[:], mul=-1.0), [], pk("PD_") + ["nwT"])
                    yield
                    for hf in range(2):
                        def w2(e, hf=hf):
                            for h in range(hf * 4, hf * 4 + 4):
                                e.matmul(ph(PD_, h), lhsT=X[:, h, :], rhs=vb[:, h, :], start=True, stop=False)
                                r = e.matmul(ph(PD_, h), lhsT=nwT[:, h, :], rhs=Sst[:, h, :], start=False, stop=True)
                            return r
                        P(w2, [f"vb{b}", Xk, "nwT", "Sst"], [f"PD_{hf}"])
                    V(lambda e: e.tensor_copy(out=flat(vn), in_=PD_[:]), [], pk("PD_") + ["vn"])
                    yield
                    for hf in range(2):
                        def o1(e, hf=hf):
                            for h in range(hf * 4, hf * 4 + 4):
                                r = e.matmul(ph(PD_, h), lhsT=qTc[:, h, :], rhs=Sst[:, h, :], start=True, stop=True)
                            return r
                        P(o1, [f"qTc{b}", "Sst"], [f"PD_{hf}"])
                    V(lambda e: e.tensor_tensor(out=osb[:], in0=ps3(PD_), in1=AP(sm, 48, 0, [[1, H8], [0, 128]]), op=ALU.mult), [SM + "0"], pk("PD_") + ["osb"])
                    yield
                    for hf in range(2):
                        def o2(e, hf=hf):
                            for h in range(hf * 4, hf * 4 + 4):
                                r = e.matmul(ph(PD_, h), lhsT=At[:, h, :], rhs=vn[:, h, :], start=True, stop=True)
                            return r
                        P(o2, [f"At{b}", "vn"], [f"PD_{hf}"])
                    V(lambda e: e.tensor_tensor(out=flat(osb), in0=PD_[:], in1=flat(osb), op=ALU.add), ["osb"], pk("PD_") + ["osb"])
                    yield
                    for hf in range(2):
                        def s1(e, hf=hf):
                            for h in range(hf * 4, hf * 4 + 4):
                                r = e.matmul(ph(PD_, h), lhsT=kdec[:, h, :], rhs=vn[:, h, :], start=True, stop=True)
                            return r
                        P(s1, [f"kdec{b}", "vn"], [f"PD_{hf}"])
                    V(lambda e: e.tensor_tensor(out=Sst[:], in0=Sst[:], in1=AP(sm, 48, 8, [[1, H8], [0, 128]]), op=ALU.mult), ["Sst", SM + "1"], ["Sst"])
                    V(lambda e: e.tensor_tensor(out=flat(Sst), in0=PD_[:], in1=flat(Sst), op=ALU.add), ["Sst"], pk("PD_") + ["Sst"])
                    yield
                    G(lambda e: e.tensor_tensor(out=sqo[:], in0=osb[:], in1=osb[:], op=ALU.mult), ["osb"], ["sqo"])
                    V(lambda e: e.reduce_sum(out=sm[:, 5, :], in_=sqo[:], axis=AX.X), ["sqo"], [SM + "5"])
                    A(lambda e: e.activation(out=sm[:, 5, :], in_=sm[:, 5, :], func=AF.Sqrt, bias=EPS, scale=1.0 / 128), [SM + "5"], [SM + "5"])
                    V(lambda e: e.reciprocal(out=sm[:, 5, :], in_=sm[:, 5, :]), [SM + "5"], [SM + "5"])
                    G(lambda e: e.tensor_tensor(out=sqo[:], in0=osb[:], in1=AP(sm, 48, 40, [[1, H8], [0, 128]]), op=ALU.mult), ["osb", SM + "5", "sqo"], ["sqo"])
                    G(lambda e: e.tensor_tensor(out=sqo[:], in0=sqo[:], in1=bc_mat(gdn), op=ALU.mult), ["sqo", "gdn"], ["sqo"])
                    G(lambda e: e.tensor_tensor(out=mixdn[b][:], in0=flat(sqo), in1=flat(zc), op=ALU.mult), ["sqo", f"zc{b}"], [f"mixdn{b}"])
                    DM("sp", lambda e: e.dma_start(out=mix_d.ap()[sl, 0:1024], in_=mixdn[b][:]), [f"mixdn{b}"], [f"mix_dn{c}"])
                    yield

                def interleave(gens):
                    gens = [g for g in gens if g is not None]
                    while gens:
                        for g in list(gens):
                            try:
                                next(g)
                            except StopIteration:
                                gens.remove(g)

                interleave([solve(0)])
                for c in range(NT):
                    interleave([solve(c + 1) if c + 1 < NT else None, apply(c)])
                if dbg and upto == 4:
                    mk.barrier()
                    dd = dbgout("mix", [S, D], BF16)
                    DM("sp", lambda e: e.dma_start(out=dd.ap(), in_=mix_d.ap()), [], ["dbgmix"])
                mk.flush()

        if upto >= 5:
            p57 = ExitStack()
            es.enter_context(p57)
            mk.pes = p57
            logits = mk.sb("logits", [128, NT, 64], F32)
            dest_i = mk.sb("dest_i", [128, NT, 8], I32)
            BEi4 = mk.sb("BEi4", [128, NBLK, 4], I32)
            BEi2 = mk.sb("BEi2", [128, NBLK, 2], I32)
            with ExitStack() as pes:
                mk.pes = pes
                wout = mk.sb("wout", [128, 16, D], BF16)
                wr = mk.sb("wr", [128, 16, 64], BF16)
                g1b = mk.sb("g1b", [128, D], F32)
                G2b = mk.sb("G2b", [128, D], F32)
                S2b = mk.sb("S2b", [128, D], F32)
                xt = [mk.sb(f"xt5{i}", [128, D], F32) for i in range(2)]
                mixt = [mk.sb(f"mixt{i}", [128, D], BF16) for i in range(2)]
                mixT = [mk.sb(f"mixT{i}", [128, 16, 128], BF16) for i in range(2)]
                h2T = mk.sb("h2T", [128, 16, 128], BF16)
                tmpf = mk.sb("tmpf5", [128, D], F32)
                tmp2 = mk.sb("tmp25", [128, D], F32)
                x1t = mk.sb("x1t", [128, D], F32)
                h2t = [mk.sb(f"h2t{i}", [128, D], BF16) for i in range(2)]
                junk = mk.sb("junk5", [128, D], BF16)
                st = mk.sb("st5", [128, NT, 2], F32)
                PT = [mk.ps(f"PT5{i}", [128, 1024], BF16) for i in range(2)]
                PO = mk.ps("PO5", [128, D], F32)
                PR = mk.ps("PR5", [128, 64], F32)
                PT2 = [mk.ps("PT52", [128, 1024], BF16), None]
                PT2[1] = PT2[0]
                for n in range(4):
                    DM("pool", lambda e, n=n: e.dma_start(out=wout[:, :, n * 512:(n + 1) * 512], in_=wout_d.ap().rearrange("(k p) n -> p k n", p=128)[:, :, n * 512:(n + 1) * 512]), w=[f"wout{n}"])
                DM("pool", lambda e: e.dma_start(out=wr[:], in_=wr_d.ap().rearrange("(k p) n -> p k n", p=128)), w=["wr"])
                DM("sp", lambda e: e.dma_start(out=g1b[:], in_=ada_d.ap()[2:3, :].partition_broadcast(128)), w=["g1b"])
                DM("sp", lambda e: e.dma_start(out=S2b[:], in_=ada_d.ap()[3:4, :].partition_broadcast(128)), w=["S2b"])
                DM("sp", lambda e: e.dma_start(out=G2b[:], in_=ada_d.ap()[4:5, :].partition_broadcast(128)), w=["G2b"])
                G(lambda e: e.memset(junk[:], 0.0), w=["junk5"])
                DM("sp", lambda e: e.dma_start(out=h2_d.ap()[S:S + 128, :], in_=junk[:]), ["junk5"], ["h2pad"])
                woutk = [f"wout{n}" for n in range(4)]

                def front(t):
                    b = t % 2
                    sl = slice(t * 128, (t + 1) * 128)
                    DM("sp", lambda e: e.dma_start(out=mixt[b][:], in_=mix_d.ap()[sl, :]), w=[f"mixt{b}"])
                    DM("sp", lambda e: e.dma_start(out=xt[b][:], in_=x_d.ap()[sl, :]), w=[f"xt5{b}"])
                    for hh in range(2):
                        def tr(e, hh=hh):
                            for k in range(8):
                                kk = hh * 8 + k
                                r = e.transpose(PT[hh][:, k * 128:(k + 1) * 128], mixt[b][:, kk * 128:(kk + 1) * 128], identb[:])
                            return r
                        P(tr, [f"mixt{b}", "identb"], [f"PT5{hh}"])
                        if hh == 0:
                            V(lambda e, hh=hh: e.tensor_copy(out=mixT[b][:, hh * 8:(hh + 1) * 8, :], in_=PT[hh][:].rearrange("p (k c) -> p k c", k=8)), [], [f"PT5{hh}", f"mixT{b}_{hh}"])
                        else:
                            A(lambda e, hh=hh: e.copy(out=mixT[b][:, hh * 8:(hh + 1) * 8, :], in_=PT[hh][:].rearrange("p (k c) -> p k c", k=8)), [], [f"PT5{hh}", f"mixT{b}_{hh}"])
                    yield
                    for n in range(4):
                        def mm(e, n=n):
                            for k in range(16):
                                r = e.matmul(PO[:, n * 512:(n + 1) * 512], lhsT=mixT[b][:, k, :], rhs=wout[:, k, n * 512:(n + 1) * 512], start=(k == 0), stop=(k == 15))
                            return r
                        P(mm, [f"mixT{b}_0", f"mixT{b}_1"] + woutk, [f"PO{n}"])
                        V(lambda e, n=n: e.tensor_tensor(out=tmpf[:, n * 512:(n + 1) * 512], in0=PO[:, n * 512:(n + 1) * 512], in1=g1b[:, n * 512:(n + 1) * 512], op=ALU.mult), ["g1b"], [f"PO{n}", f"tmpf5_{n}"])
                        yield

                def back(t):
                    b = t % 2
                    sl = slice(t * 128, (t + 1) * 128)
                    tk = [f"tmpf5_{n}" for n in range(4)]
                    V(lambda e: e.tensor_tensor(out=x1t[:], in0=tmpf[:], in1=xt[b][:], op=ALU.add), tk + [f"xt5{b}"], ["x1t"])
                    DM("sp", lambda e: e.dma_start(out=x1_d.ap()[sl, :], in_=x1t[:]), ["x1t"], [f"x1_d{t}"])
                    A(lambda e: e.activation(out=junk[:], in_=x1t[:], func=AF.Square, accum_out=st[:, t, 0:1]), ["x1t"], ["junk5", f"st5{t}"])
                    A(lambda e: e.activation(out=st[:, t, 1:2], in_=st[:, t, 0:1], func=AF.Sqrt, bias=EPS, scale=1.0 / D), [f"st5{t}"], [f"st5{t}"])
                    V(lambda e: e.reciprocal(out=st[:, t, 1:2], in_=st[:, t, 1:2]), [f"st5{t}"], [f"st5{t}"])
                    yield
                    V(lambda e: e.scalar_tensor_tensor(out=tmp2[:], in0=x1t[:], scalar=st[:, t, 1:2], in1=G2b[:], op0=ALU.mult, op1=ALU.mult), ["x1t", f"st5{t}", "G2b"], ["tmp2"])
                    V(lambda e: e.tensor_tensor(out=h2t[b][:], in0=tmp2[:], in1=S2b[:], op=ALU.add), ["tmp2", "S2b"], [f"h2t{b}"])
                    DM("sp", lambda e: e.dma_start(out=h2_d.ap()[sl, :], in_=h2t[b][:]), [f"h2t{b}"], [f"h2_d{t}"])
                    yield
                    for hh in range(2):
                        def tr2(e, hh=hh):
                            for k in range(8):
                                kk = hh * 8 + k
                                r = e.transpose(PT2[hh][:, k * 128:(k + 1) * 128], h2t[b][:, kk * 128:(kk + 1) * 128], identb[:])
                            return r
                        P(tr2, [f"h2t{b}", "identb"], ["PT52"])
                        if hh == 0:
                            V(lambda e, hh=hh: e.tensor_copy(out=h2T[:, hh * 8:(hh + 1) * 8, :], in_=PT2[hh][:].rearrange("p (k c) -> p k c", k=8)), [], ["PT52", f"h2T{hh}"])
                        else:
                            A(lambda e, hh=hh: e.copy(out=h2T[:, hh * 8:(hh + 1) * 8, :], in_=PT2[hh][:].rearrange("p (k c) -> p k c", k=8)), [], ["PT52", f"h2T{hh}"])
                    yield

                    def mmr(e):
                        for k in range(16):
                            r = e.matmul(PR[:], lhsT=h2T[:, k, :], rhs=wr[:, k, :], start=(k == 0), stop=(k == 15))
                        return r
                    P(mmr, ["h2T0", "h2T1", "wr"], ["PR5"])
                    V(lambda e: e.tensor_copy(out=logits[:, t, :], in_=PR[:]), [], ["PR5", f"logits{t}"])
                    yield

                def interleave5(gens):
                    gens = [g for g in gens if g is not None]
                    while gens:
                        for g in list(gens):
                            try:
                                next(g)
                            except StopIteration:
                                gens.remove(g)

                interleave5([front(0)])
                for t in range(NT):
                    interleave5([front(t + 1) if t + 1 < NT else None, back(t)])
                mk.flush()
            with ExitStack() as pes:
                mk.pes = pes
                NG = NT * 8
                scores = mk.sb("scores", [128, NT, 64], F32)
                biased = mk.sb("biased", [128, NT, 64], F32)
                tmpA = mk.sb("tmpA", [128, NT, 64], F32)
                mb = mk.sb("mb", [128, NT, 64], F32)
                sel = mk.sb("sel", [128, NT, 64], F32)
                wn = mk.sb("wn", [128, NT, 64], F32)
                pos = mk.sb("pos", [128, NT, 64], F32)
                rb = mk.sb("rb", [128, 64], F32)
                m1 = mk.sb("m1", [128, NG], F32)
                m2 = mk.sb("m2", [128, NG], F32)
                t8 = mk.sb("t8", [128, NT, 8], F32)
                v8 = mk.sb("v8", [128, NT, 8], F32)
                den = mk.sb("den", [128, NT], F32)
                selsum = mk.sb("selsum", [128, 64], F32)
                Ustr = mk.sb("Ustr", [128, 128], F32)
                cA = mk.sb("cA", [128, 64], F32)
                cB = mk.sb("cB", [128, 64], F32)
                nbf = mk.sb("nbf", [128, 64], F32)
                nbi = mk.sb("nbi", [128, 64], I32)
                rowbase = mk.sb("rowbase", [128, 64], F32)
                d6 = mk.sb("d6", [128, NT, 8], F32)
                meta = mk.sb("meta", [128, NT, 8, 2], F32)
                junk64 = mk.sb("junk64", [128, 64], F32)
                tok_i = mk.sb("tok_i", [128, NT], I32)
                tokf = mk.sb("tokf", [128, NT], F32)
                bidx_i = mk.sb("bidx_i", [128, NBLK], I32)
                bidxf = mk.sb("bidxf", [128, NBLK], F32)
                cmp = mk.sb("cmp", [128, NBLK, 64], F32)
                BEf = mk.sb("BEf", [128, NBLK], F32)
                BE4f = mk.sb("BE4f", [128, NBLK, 4], F32)
                rminit = mk.sb("rminit", [128, NROWS // 128, 2], F32)
                Ppos = mk.ps("Ppos", [128, 64], F32)
                DM("sp", lambda e: e.dma_start(out=rb[:], in_=rb_d.ap().partition_broadcast(128)), w=["rb"])
                G(lambda e: e.memset(rminit[:], 0.0), w=["rminit"])
                G(lambda e: e.memset(rminit[:, :, 0:1], float(S)), ["rminit"], ["rminit"])
                DM("sp", lambda e: e.dma_start(out=rm_d.ap().rearrange("(p a) c -> p a c", p=128), in_=rminit[:]), ["rminit"], ["rm_init"])
                G(lambda e: e.memset(d6[:], 0.0), w=["d6"])
                G(lambda e: e.memset(meta[:], 0.0), w=["meta"])
                G(lambda e: e.iota(tok_i[:], [[128, NT]], base=0, channel_multiplier=1), w=["tok_i"])
                G(lambda e: e.iota(bidx_i[:], [[1, NBLK]], base=0, channel_multiplier=0), w=["bidx_i"])
                V(lambda e: e.tensor_copy(out=tokf[:], in_=tok_i[:]), ["tok_i"], ["tokf"])
                V(lambda e: e.tensor_copy(out=bidxf[:], in_=bidx_i[:]), ["bidx_i"], ["bidxf"])
                V(lambda e: e.tensor_single_scalar(out=Ustr[:], in_=dif[:], scalar=0.0, op=ALU.is_gt), ["dif"], ["Ustr"])
                lk = [f"logits{t}" for t in range(NT)]
                fl = lambda t_: t_[:].rearrange("p t e -> p (t e)")
                g3 = lambda t_: t_[:].rearrange("p t (g e) -> p (t g) e", e=8)
                A(lambda e: e.activation(out=fl(scores), in_=fl(logits), func=AF.Sigmoid), lk, ["scores"])
                V(lambda e: e.tensor_tensor(out=biased[:], in0=scores[:], in1=AP(rb, 64, 0, [[0, NT], [1, 64]]), op=ALU.add), ["scores", "rb"], ["biased"])
                V(lambda e: e.reduce_max(out=m1[:], in_=g3(biased), axis=AX.X), ["biased"], ["m1"])
                V(lambda e: e.tensor_tensor(out=g3(tmpA), in0=g3(biased), in1=AP(m1, NG, 0, [[1, NG], [0, 8]]), op=ALU.is_equal), ["biased", "m1"], ["tmpA"])
                V(lambda e: e.scalar_tensor_tensor(out=fl(tmpA), in0=fl(tmpA), scalar=-1.0e9, in1=fl(biased), op0=ALU.mult, op1=ALU.add), ["tmpA", "biased"], ["tmpA"])
                V(lambda e: e.reduce_max(out=m2[:], in_=g3(tmpA), axis=AX.X), ["tmpA"], ["m2"])
                V(lambda e: e.tensor_tensor(out=m1[:], in0=m1[:], in1=m2[:], op=ALU.add), ["m1", "m2"], ["m1"])
                for t in range(NT):
                    V(lambda e, t=t: e.max(out=t8[:, t, :], in_=m1[:, t * 8:(t + 1) * 8]), ["m1"], [f"t8_{t}"])
                t8k = [f"t8_{t}" for t in range(NT)]
                V(lambda e: e.tensor_tensor(out=m2[:].rearrange("p (t g) -> p t g", g=8), in0=m1[:].rearrange("p (t g) -> p t g", g=8), in1=AP(t8, NT * 8, 3, [[8, NT], [0, 8]]), op=ALU.is_ge), ["m1", "m2"] + t8k, ["m2"])
                V(lambda e: e.tensor_scalar(out=m2[:], in0=m2[:], scalar1=1.0e9, scalar2=-1.0e9, op0=ALU.mult, op1=ALU.add), ["m2"], ["m2"])
                V(lambda e: e.tensor_tensor(out=g3(mb), in0=g3(biased), in1=AP(m2, NG, 0, [[1, NG], [0, 8]]), op=ALU.add), ["biased", "m2"], ["mb"])
                for t in range(NT):
                    V(lambda e, t=t: e.max(out=v8[:, t, :], in_=mb[:, t, :]), ["mb"], [f"v8_{t}"])
                v8k = [f"v8_{t}" for t in range(NT)]
                V(lambda e: e.tensor_tensor(out=sel[:], in0=mb[:], in1=AP(v8, NT * 8, 5, [[8, NT], [0, 64]]), op=ALU.is_ge), ["mb"] + v8k, ["sel"])
                V(lambda e: e.tensor_tensor(out=wn[:], in0=sel[:], in1=scores[:], op=ALU.mult), ["sel", "scores"], ["wn"])
                V(lambda e: e.reduce_sum(out=den[:], in_=wn[:], axis=AX.X), ["wn"], ["den"])
                V(lambda e: e.reciprocal(out=den[:], in_=den[:]), ["den"], ["den"])
                V(lambda e: e.tensor_single_scalar(out=den[:], in_=den[:], scalar=2.5, op=ALU.mult), ["den"], ["den"])
                V(lambda e: e.tensor_tensor(out=wn[:], in0=wn[:], in1=AP(den, NT, 0, [[1, NT], [0, 64]]), op=ALU.mult), ["wn", "den"], ["wn"])
                for t in range(NT):
                    def mp(e, t=t):
                        r = e.matmul(Ppos[:], lhsT=Ustr[:], rhs=sel[:, t, :], start=True, stop=(t == 0))
                        if t > 0:
                            r = e.matmul(Ppos[:], lhsT=onesf[:], rhs=selsum[:], start=False, stop=True)
                        return r
                    P(mp, ["Ustr", "onesf", "sel", "selsum"], ["Ppos"])
                    V(lambda e, t=t: e.tensor_copy(out=pos[:, t, :], in_=Ppos[:]), [], ["Ppos", f"pos{t}"])
                    if t == 0:
                        V(lambda e: e.tensor_copy(out=selsum[:], in_=sel[:, 0, :]), ["sel"], ["selsum"])
                    else:
                        V(lambda e, t=t: e.tensor_tensor(out=selsum[:], in0=selsum[:], in1=sel[:, t, :], op=ALU.add), ["sel", "selsum"], ["selsum"])
                P(lambda e: e.matmul(Ppos[:], lhsT=onesf[:], rhs=selsum[:], start=True, stop=True), ["onesf", "selsum"], ["Ppos"])
                V(lambda e: e.tensor_scalar(out=nbf[:], in0=Ppos[:], scalar1=float(BS - 1), scalar2=1.0 / BS, op0=ALU.add, op1=ALU.mult), [], ["Ppos", "nbf"])
                V(lambda e: e.tensor_single_scalar(out=nbf[:], in_=nbf[:], scalar=-0.5 + 1.0 / (2 * BS), op=ALU.add), ["nbf"], ["nbf"])
                V(lambda e: e.tensor_copy(out=nbi[:], in_=nbf[:]), ["nbf"], ["nbi"])
                V(lambda e: e.tensor_copy(out=nbf[:], in_=nbi[:]), ["nbi"], ["nbf"])
                V(lambda e: e.tensor_copy(out=cA[:], in_=nbf[:]), ["nbf"], ["cA"])
                ca, cb, can, cbn = cA, cB, "cA", "cB"
                for sft in (1, 2, 4, 8, 16, 32):
                    V(lambda e, ca=ca, cb=cb, sft=sft: e.tensor_copy(out=cb[:, 0:sft], in_=ca[:, 0:sft]), [can], [cbn])
                    V(lambda e, ca=ca, cb=cb, sft=sft: e.tensor_tensor(out=cb[:, sft:64], in0=ca[:, sft:64], in1=ca[:, 0:64 - sft], op=ALU.add), [can, cbn], [cbn])
                    ca, cb, can, cbn = cb, ca, cbn, can
                bsincl, bsk = ca, can
                V(lambda e: e.tensor_tensor(out=rowbase[:], in0=bsincl[:], in1=nbf[:], op=ALU.subtract), [bsk, "nbf"], ["rowbase"])
                V(lambda e: e.tensor_single_scalar(out=rowbase[:], in_=rowbase[:], scalar=float(BS), op=ALU.mult), ["rowbase"], ["rowbase"])
                posk = [f"pos{t}" for t in range(NT)]
                V(lambda e: e.tensor_tensor(out=pos[:], in0=pos[:], in1=AP(rowbase, 64, 0, [[0, NT], [1, 64]]), op=ALU.add), posk + ["rowbase"], ["posall"])
                for t in range(NT):
                    for k in range(6):
                        V(lambda e, t=t, k=k: e.scalar_tensor_tensor(out=junk64[:], in0=mb[:, t, :], scalar=v8[:, t, k:k + 1], in1=pos[:, t, :], op0=ALU.is_equal, op1=ALU.mult, accum_out=d6[:, t, k:k + 1]),
                          ["mb", "posall", "d6"] + v8k, ["junk64", f"d6_{t}_{k}"])
                        V(lambda e, t=t, k=k: e.scalar_tensor_tensor(out=junk64[:], in0=mb[:, t, :], scalar=v8[:, t, k:k + 1], in1=wn[:, t, :], op0=ALU.is_equal, op1=ALU.mult, accum_out=meta[:, t, k, 1:2]),
                          ["mb", "wn", "meta"] + v8k, ["junk64", f"meta_{t}_{k}"])
                d6k = [f"d6_{t}_{k}" for t in range(NT) for k in range(6)]
                mtk = [f"meta_{t}_{k}" for t in range(NT) for k in range(6)]
                V(lambda e: e.tensor_copy(out=dest_i[:], in_=d6[:]), d6k + ["d6"], ["dest_i"])
                V(lambda e: e.tensor_copy(out=meta[:, :, :, 0], in_=AP(tokf, NT, 0, [[1, NT], [0, 8]])), ["tokf", "meta"] + mtk, ["meta_tok"])
                for t in range(NT):
                    for k in range(6):
                        DM("pool", lambda e, t=t, k=k: e.indirect_dma_start(out=rm_d.ap(), out_offset=bass.IndirectOffsetOnAxis(ap=dest_i[:, t, k:k + 1], axis=0), in_=meta[:, t, k, :], in_offset=None),
                           ["dest_i", "meta_tok", "rm_init"] + mtk, [f"rm_{t}_{k}"])
                V(lambda e: e.tensor_tensor(out=cmp[:], in0=AP(bidxf, NBLK, 0, [[1, NBLK], [0, 64]]), in1=AP(bsincl, 64, 0, [[0, NBLK], [1, 64]]), op=ALU.is_ge), ["bidxf", bsk], ["cmp"])
                V(lambda e: e.reduce_sum(out=BEf[:], in_=cmp[:], axis=AX.X), ["cmp"], ["BEf"])
                V(lambda e: e.tensor_single_scalar(out=bidxf[:], in_=BEf[:], scalar=64.0, op=ALU.is_ge), ["BEf", "cmp"], ["bidxf"])
                V(lambda e: e.scalar_tensor_tensor(out=BEf[:], in0=bidxf[:], scalar=1000.0, in1=BEf[:], op0=ALU.mult, op1=ALU.add), ["bidxf", "BEf"], ["BEf"])
                V(lambda e: e.tensor_scalar(out=BEf[:], in0=BEf[:], scalar1=128.0, scalar2=tokf[:, 0:1], op0=ALU.mult, op1=ALU.add), ["BEf", "tokf"], ["BEf"])
                for j in range(4):
                    V(lambda e, j=j: e.tensor_scalar(out=BE4f[:, :, j], in0=BEf[:], scalar1=4.0, scalar2=float(j), op0=ALU.mult, op1=ALU.add), ["BEf"], [f"BE4f{j}"])
                V(lambda e: e.tensor_copy(out=BEi4[:], in_=BE4f[:]), [f"BE4f{j}" for j in range(4)], ["BEi4"])
                for j in range(2):
                    V(lambda e, j=j: e.tensor_scalar(out=BE4f[:, :, j], in0=BEf[:], scalar1=2.0, scalar2=float(j), op0=ALU.mult, op1=ALU.add), ["BEf", "BEi4"], [f"BE4f{j}"])
                V(lambda e: e.tensor_copy(out=BEi2[:], in_=BE4f[:, :, 0:2]), ["BE4f0", "BE4f1"], ["BEi2"])
                if dbg and upto == 5:
                    mk.barrier()
                    for nm, src, shp, dty in (("x1", x1_d, [S, D], F32), ("h2", h2_d, [S + 128, D], BF16), ("rm", rm_d, [NROWS, 2], F32)):
                        dd = dbgout(nm, shp, dty)
                        DM("sp", lambda e, dd=dd, src=src: e.dma_start(out=dd.ap(), in_=src.ap()), [], ["dbg_" + nm])
                    for nm, src, shp, dty in (("logits", logits, [128, NT, 64], F32), ("sel", sel, [128, NT, 64], F32), ("wn", wn, [128, NT, 64], F32), ("dest", dest_i, [128, NT, 8], I32), ("BEi4", BEi4, [128, NBLK, 4], I32)):
                        dd = dbgout(nm, shp, dty)
                        DM("sp", lambda e, dd=dd, src=src: e.dma_start(out=dd.ap(), in_=src[:]), [], ["dbg_" + nm])
                mk.flush()

        if upto >= 6:
            with ExitStack() as pes:
                mk.pes = pes
                NST = 4
                NTOT = NBLK + 8
                stg = [mk.sb(f"stg{i}", [128, 4096], F32) for i in range(NST)]
                wgu = [mk.sb(f"wgu{i}", [128, 16, 1024], BF16) for i in range(2)]
                wd = [mk.sb(f"wd{i}", [128, 4, 2048], BF16) for i in range(2)]
                rmt = [mk.sb(f"rmt{i}", [128, 2, 2], F32) for i in range(2)]
                toki = [mk.sb(f"toki{i}", [128, 2], I32) for i in range(2)]
                hg = [mk.sb(f"hg{i}", [128, D], BF16) for i in range(4)]
                hgT = mk.sb("hgT", [128, 16, BS], BF16)
                gsb = [mk.sb(f"gsb{i}", [128, BS], F32) for i in range(2)]
                actT = mk.sb("actT", [128, 4, BS], BF16)
                ysb = [mk.sb(f"ysb{i}", [128, 1024], BF16) for i in range(2)]
                PT = [mk.ps(f"PT6{i}", [128, 1024], BF16) for i in range(2)]
                PG = [mk.ps(f"PG6{i}", [128, 512], F32) for i in range(2)]
                PU = [mk.ps(f"PU6{i}", [128, 512], F32) for i in range(2)]
                PY = [mk.ps(f"PY6{i}", [128, 512], F32) for i in range(2)]
                state = {"sti": 0, "yi": 0, "ys": 0}

                order = []
                for i in range(NBLK // 2):
                    order += [i, NBLK - 1 - i]
                order += list(range(NBLK, NTOT))
                SHP = NBLK % 2

                def wb_of(pos):
                    return pos % 2 if pos < NBLK else SHP

                def rows(pos):
                    blk = order[pos]
                    mb_ = pos % 2
                    if blk < NBLK:
                        DM("sp", lambda e: e.dma_start(out=rmt[mb_][:], in_=rm_d.ap()[blk * BS:(blk + 1) * BS, :].rearrange("(s p) c -> p s c", p=128)), [], [f"rmt{mb_}"])
                        V(lambda e: e.tensor_copy(out=toki[mb_][:], in_=rmt[mb_][:, :, 0]), [f"rmt{mb_}"], [f"toki{mb_}"])
                    else:
                        G(lambda e: e.memset(rmt[mb_][:], 1.0), [], [f"rmt{mb_}"])
                    for sbi in range(2):
                        hi = mb_ * 2 + sbi
                        if blk < NBLK:
                            DM("pool", lambda e, sbi=sbi, hi=hi: e.indirect_dma_start(out=hg[hi][:], out_offset=None, in_=h2_d.ap(), in_offset=bass.IndirectOffsetOnAxis(ap=toki[mb_][:, sbi:sbi + 1], axis=0)),
                               [f"toki{mb_}"], [f"hg{hi}"])
                        else:
                            r0 = (blk - NBLK) * BS + sbi * 128
                            DM("sp", lambda e, hi=hi, r0=r0: e.dma_start(out=hg[hi][:], in_=h2_d.ap()[r0:r0 + 128, :]), [], [f"hg{hi}"])

                def wdma(pos, j):
                    blk = order[pos]
                    wbuf = wb_of(pos)
                    if blk < NBLK:
                        sb_ = state["sti"] % NST
                        state["sti"] += 1
                        if j < 4:
                            DM("pool", lambda e: e.indirect_dma_start(out=stg[sb_][:], out_offset=None, in_=wegu_d.ap(), in_offset=bass.IndirectOffsetOnAxis(ap=BEi4[:, blk, j:j + 1], axis=0), bounds_check=state["bc"], oob_is_err=False), ["BEi4"], [f"stg{sb_}"])
                        else:
                            DM("pool", lambda e: e.indirect_dma_start(out=stg[sb_][:], out_offset=None, in_=wed_d.ap(), in_offset=bass.IndirectOffsetOnAxis(ap=BEi2[:, blk, j - 4:j - 3], axis=0), bounds_check=state["bc"], oob_is_err=False), ["BEi2"], [f"stg{sb_}"])
                        return sb_
                    if pos == NBLK:
                        if j < 4:
                            DM("pool", lambda e: e.dma_start(out=wgu[wbuf][:, 4 * j:4 * j + 4, :].rearrange("p k n -> p (k n)"), in_=wsgu_d.ap()[:, j * 4096:(j + 1) * 4096]), [], [f"wgu{wbuf}_{j}"])
                        else:
                            DM("pool", lambda e: e.dma_start(out=wd[wbuf][:, 2 * (j - 4):2 * (j - 4) + 2, :].rearrange("p k n -> p (k n)"), in_=wsd_d.ap()[:, (j - 4) * 4096:(j - 3) * 4096]), [], [f"wd{wbuf}_{j - 4}"])
                    return None

                def wcast(pos, j, sb_):
                    if sb_ is None:
                        return
                    wbuf = wb_of(pos)
                    if j < 4:
                        dstw = wgu[wbuf][:, 4 * j:4 * j + 4, :].rearrange("p k n -> p (k n)")
                        wkey = f"wgu{wbuf}_{j}"
                    else:
                        dstw = wd[wbuf][:, 2 * (j - 4):2 * (j - 4) + 2, :].rearrange("p k n -> p (k n)")
                        wkey = f"wd{wbuf}_{j - 4}"
                    if j % 2 == 0:
                        A(lambda e: e.copy(out=dstw, in_=stg[sb_][:]), [f"stg{sb_}"], [wkey])
                    else:
                        V(lambda e: e.tensor_copy(out=dstw, in_=stg[sb_][:]), [f"stg{sb_}"], [wkey])

                def transposes(pos):
                    mb_ = pos % 2
                    for sbi in range(2):
                        hi = mb_ * 2 + sbi
                        for hh in range(2):
                            def tr(e, hi=hi, hh=hh):
                                for k in range(8):
                                    kk = hh * 8 + k
                                    r = e.transpose(PT[hh][:, k * 128:(k + 1) * 128], AP(hg[hi], D, kk, [[16, 128]]), identb[:])
                                return r
                            P(tr, [f"hg{hi}", "identb"], [f"PT6{hh}"])
                            if hh == 0:
                                V(lambda e, sbi=sbi, hh=hh: e.tensor_copy(out=hgT[:, hh * 8:(hh + 1) * 8, sbi * 128:(sbi + 1) * 128], in_=PT[hh][:].rearrange("p (k c) -> p k c", k=8)), [], [f"PT6{hh}", f"hgT{sbi}_{hh}"])
                            else:
                                A(lambda e, sbi=sbi, hh=hh: e.copy(out=hgT[:, hh * 8:(hh + 1) * 8, sbi * 128:(sbi + 1) * 128], in_=PT[hh][:].rearrange("p (k c) -> p k c", k=8)), [], [f"PT6{hh}", f"hgT{sbi}_{hh}"])

                hgTk = [f"hgT{a}_{b2}" for a in range(2) for b2 in range(2)]
                actk = [f"actT{kk}" for kk in range(4)]

                def gateup(pos, kk):
                    wbuf = wb_of(pos)
                    wguk = [f"wgu{wbuf}_{j}" for j in range(4)]
                    pb = kk % 2

                    def mg(e):
                        for k in range(16):
                            r = e.matmul(PG[pb][:, 0:BS], lhsT=AP(wgu[wbuf], 16384, k * 1024 + kk, [[4, 128]]), rhs=hgT[:, k, :], start=(k == 0), stop=(k == 15))
                        return r
                    P(mg, wguk + hgTk, [f"PG6{pb}"])

                    def mu(e):
                        for k in range(16):
                            r = e.matmul(PU[pb][:, 0:BS], lhsT=AP(wgu[wbuf], 16384, k * 1024 + 512 + kk, [[4, 128]]), rhs=hgT[:, k, :], start=(k == 0), stop=(k == 15))
                        return r
                    P(mu, wguk + hgTk, [f"PU6{pb}"])
                    A(lambda e: e.activation(out=gsb[pb][:], in_=PG[pb][:, 0:BS], func=AF.Silu), [], [f"PG6{pb}", f"gsb{pb}"])
                    V(lambda e: e.tensor_tensor(out=actT[:, kk, :], in0=PU[pb][:, 0:BS], in1=gsb[pb][:], op=ALU.mult), [f"gsb{pb}"], [f"PU6{pb}", f"actT{kk}"])

                def down(pos, sbi, half):
                    blk = order[pos]
                    wbuf = wb_of(pos)
                    mb_ = pos % 2
                    wdk = [f"wd{wbuf}_{j}" for j in range(2)]
                    ys = state["ys"] % 2
                    state["ys"] += 1
                    for n2 in range(2):
                        n = half * 2 + n2
                        yb = state["yi"] % 2
                        state["yi"] += 1

                        def md(e, n=n, yb=yb):
                            for kk in range(4):
                                r = e.matmul(PY[yb][:], lhsT=actT[:, kk, sbi * 128:(sbi + 1) * 128], rhs=wd[wbuf][:, kk, n * 512:(n + 1) * 512], start=(kk == 0), stop=(kk == 3))
                            return r
                        P(md, actk + wdk, [f"PY6{yb}"])
                        if n2 == 0:
                            A(lambda e, n2=n2, yb=yb: e.activation(out=ysb[ys][:, n2 * 512:(n2 + 1) * 512], in_=PY[yb][:], func=AF.Copy, scale=rmt[mb_][:, sbi, 1:2]), [f"rmt{mb_}"], [f"PY6{yb}", f"ysb{ys}_{n2}"])
                        else:
                            V(lambda e, n2=n2, yb=yb: e.tensor_scalar(out=ysb[ys][:, n2 * 512:(n2 + 1) * 512], in0=PY[yb][:], scalar1=rmt[mb_][:, sbi, 1:2], scalar2=None, op0=ALU.mult), [f"rmt{mb_}"], [f"PY6{yb}", f"ysb{ys}_{n2}"])
                    r0 = blk * BS + sbi * 128
                    DM("sp", lambda e: e.dma_start(out=Y_d.ap()[r0:r0 + 128, half * 1024:(half + 1) * 1024], in_=ysb[ys][:]), [f"ysb{ys}_0", f"ysb{ys}_1"], [f"Y_d{blk}_{sbi}_{half}"])

                def mkreg(e):
                    r = e.alloc_register("oobbound")
                    e.reg_mov(r, 64 * 128 * 4 - 1)
                    state["bc"] = e.snap(r)
                mk.q["pool"].append(mkreg)
                slot_of = {}

                def issue(b_, j):
                    if b_ < NTOT and b_ <= NBLK and (b_, j) not in slot_of:
                        slot_of[(b_, j)] = wdma(b_, j)

                def cast(b_, j):
                    if b_ < NTOT and b_ <= NBLK:
                        issue(b_, j)
                        wcast(b_, j, slot_of[(b_, j)])

                rows(0)
                for j in range(6):
                    cast(0, j)
                for j in range(4):
                    issue(1, j)
                for blk in range(NTOT):
                    nb = blk + 1
                    have_next = nb < NTOT
                    early = nb + 1 < NBLK
                    if have_next:
                        rows(nb)
                        for j in range(4):
                            issue(nb, j)
                    transposes(blk)
                    if have_next:
                        cast(nb, 0)
                        issue(nb, 4)
                        cast(nb, 1)
                        issue(nb, 5)
                    gateup(blk, 0)
                    if have_next:
                        cast(nb, 2)
                        if early:
                            issue(nb + 1, 0)
                    gateup(blk, 1)
                    if have_next:
                        cast(nb, 3)
                        if early:
                            issue(nb + 1, 1)
                    gateup(blk, 2)
                    if have_next:
                        cast(nb, 4)
                        if early:
                            issue(nb + 1, 2)
                    gateup(blk, 3)
                    if have_next:
                        cast(nb, 5)
                        if early:
                            issue(nb + 1, 3)
                    for sbi in range(2):
                        for half in range(2):
                            down(blk, sbi, half)
                mk.flush()
        if upto >= 7:
            with ExitStack() as pes:
                mk.pes = pes
                g2b = mk.sb("g2b", [128, D], F32)
                fgb = mk.sb("fgb", [128, D], F32)
                acc = [mk.sb(f"acc7{i}", [128, D], F32) for i in range(2)]
                gk = [mk.sb(f"gk{i}", [128, D], BF16) for i in range(8)]
                x1t = [mk.sb(f"x1t7{i}", [128, D], F32) for i in range(2)]
                junk = mk.sb("junk7", [128, D], BF16)
                tsum = mk.sb("tsum", [128, D], F32)
                st = mk.sb("st7", [128, NT, 2], F32)
                DM("sp", lambda e: e.dma_start(out=g2b[:], in_=ada_d.ap()[5:6, :].partition_broadcast(128)), w=["g2b"])
                DM("sp", lambda e: e.dma_start(out=fgb[:], in_=fng_d.ap().partition_broadcast(128)), w=["fgb"])
                gi = 0
                for t in range(NT):
                    b = t % 2
                    sl = slice(t * 128, (t + 1) * 128)
                    DM("sp", lambda e, b=b, sl=sl: e.dma_start(out=x1t[b][:], in_=x1_d.ap()[sl, :]), w=[f"x1t7{b}"])
                    gs = []
                    for k in range(7):
                        g_ = gi % 8
                        gi += 1
                        gs.append(g_)
                        if k == 6:
                            DM("sp", lambda e, g_=g_, t=t: e.dma_start(out=gk[g_][:], in_=Y_d.ap()[NROWS + t * 128:NROWS + (t + 1) * 128, :]), w=[f"gk{g_}"])
                        else:
                            DM("pool", lambda e, g_=g_, t=t, k=k: e.indirect_dma_start(out=gk[g_][:], out_offset=None, in_=Y_d.ap(), in_offset=bass.IndirectOffsetOnAxis(ap=dest_i[:, t, k:k + 1], axis=0)), ["dest_i"], [f"gk{g_}"])
                    G(lambda e, gs=gs: e.tensor_tensor(out=tsum[:], in0=gk[gs[0]][:], in1=gk[gs[1]][:], op=ALU.add), [f"gk{gs[0]}", f"gk{gs[1]}"], ["tsum"])
                    G(lambda e, gs=gs: e.tensor_tensor(out=tsum[:], in0=tsum[:], in1=gk[gs[2]][:], op=ALU.add), [f"gk{gs[2]}", "tsum"], ["tsum"])
                    V(lambda e, b=b, gs=gs: e.tensor_tensor(out=acc[b][:], in0=gk[gs[3]][:], in1=gk[gs[4]][:], op=ALU.add), [f"gk{gs[3]}", f"gk{gs[4]}"], [f"acc7{b}"])
                    for k in (5, 6):
                        V(lambda e, b=b, g_=gs[k]: e.tensor_tensor(out=acc[b][:], in0=acc[b][:], in1=gk[g_][:], op=ALU.add), [f"gk{gs[k]}", f"acc7{b}"], [f"acc7{b}"])
                    V(lambda e, b=b: e.tensor_tensor(out=acc[b][:], in0=acc[b][:], in1=tsum[:], op=ALU.add), ["tsum", f"acc7{b}"], [f"acc7{b}"])
                    V(lambda e, b=b: e.tensor_tensor(out=acc[b][:], in0=acc[b][:], in1=g2b[:], op=ALU.mult), ["g2b", f"acc7{b}"], [f"acc7{b}"])
                    V(lambda e, b=b: e.tensor_tensor(out=acc[b][:], in0=acc[b][:], in1=x1t[b][:], op=ALU.add), [f"x1t7{b}", f"acc7{b}"], [f"acc7{b}"])
                    A(lambda e, t=t, b=b: e.activation(out=junk[:], in_=acc[b][:], func=AF.Square, accum_out=st[:, t, 0:1]), [f"acc7{b}"], ["junk7", f"st7{t}"])
                    A(lambda e, t=t: e.activation(out=st[:, t, 1:2], in_=st[:, t, 0:1], func=AF.Sqrt, bias=EPS, scale=1.0 / D), [f"st7{t}"], [f"st7{t}"])
                    V(lambda e, t=t: e.reciprocal(out=st[:, t, 1:2], in_=st[:, t, 1:2]), [f"st7{t}"], [f"st7{t}"])
                    V(lambda e, t=t, b=b: e.scalar_tensor_tensor(out=acc[b][:], in0=acc[b][:], scalar=st[:, t, 1:2], in1=fgb[:], op0=ALU.mult, op1=ALU.mult), [f"acc7{b}", f"st7{t}", "fgb"], [f"acc7{b}"])
                    DM("sp", lambda e, b=b, sl=sl: e.dma_start(out=out_d.ap()[sl, :], in_=acc[b][:]), [f"acc7{b}"], [f"out{t}"])
                mk.flush()
        if upto >= 5:
            p57.close()

        if dbg and upto <= 2:
            pass
        mk.pes = pers
        mk.flush()
    return nc, dbg_d


def _host_inputs(b, inputs, with_experts=True):
    f = lambda a: np.ascontiguousarray(a, dtype=np.float32)
    half = 32
    inv = (10000.0 ** (-np.arange(half, dtype=np.float32) / half)).astype(np.float32)
    invf = np.zeros((64, 2), np.float32)
    invf[:, 0] = np.concatenate([inv, inv])
    invf[:32, 1] = -1.0
    invf[32:, 1] = 1.0
    m = {
        "x": f(inputs["x"][b]),
        "c": f(inputs["c"][b].reshape(16, 128).T),
        "positions": np.ascontiguousarray(inputs["positions"][b].reshape(1, S).astype(np.int32)),
        "w_ada": f(inputs["w_ada"][0]),
        "b_ada": f(inputs["b_ada"][0].reshape(1, -1)),
        "norm1_gain": f(inputs["norm1_gain"][0].reshape(1, -1)),
        "w_in": f(inputs["w_in"][0]),
        "dn_conv_w": f(inputs["dn_conv_w"][0].reshape(24, 128, 4).transpose(1, 0, 2)),
        "dn_a_log": f(inputs["dn_a_log"][0].reshape(1, 8)),
        "dn_dt_bias": f(inputs["dn_dt_bias"][0].reshape(1, 8)),
        "dn_norm_gain": f(inputs["dn_norm_gain"][0].reshape(1, 128)),
        "mla_q_norm_gain": f(inputs["mla_q_norm_gain"][0].reshape(4, 128).T),
        "w_q_up": f(inputs["w_q_up"][0]),
        "mla_kv_norm_gain": f(inputs["mla_kv_norm_gain"][0].reshape(4, 128).T),
        "w_kv_up": f(inputs["w_kv_up"][0]),
        "w_out": f(inputs["w_out"][0]),
        "norm2_gain": f(inputs["norm2_gain"][0].reshape(1, -1)),
        "w_router": f(inputs["w_router"][0]),
        "router_bias": f(inputs["router_bias"][0].reshape(1, 64)),
        "final_norm_gain": f(inputs["final_norm_gain"].reshape(1, -1)),
        "invf": invf,
    }
    if with_experts:
        m["w_exp_gate_up"] = f(inputs["w_exp_gate_up"][0]).reshape(64 * 128 * 4, 4096)
        m["w_exp_down"] = f(inputs["w_exp_down"][0]).reshape(64 * 128 * 2, 4096)
        m["w_sh_gate_up"] = f(inputs["w_sh_gate_up"][0]).reshape(128, 16 * 1024)
        m["w_sh_down"] = f(inputs["w_sh_down"][0]).reshape(128, 4 * 2048)
    return m


def kernel(**inputs):
    nc, _ = build()
    shared = None
    in_maps = []
    for b in range(8):
        m = _host_inputs(b, inputs)
        if shared is None:
            shared = m
        else:
            for k in m:
                if k not in ("x", "c", "positions"):
                    m[k] = shared[k]
        in_maps.append(m)
    res = run_bass_kernel_spmd(nc, in_maps, core_ids=list(range(8)))
    return np.stack([r["out"] for r in res.results], axis=0).astype(np.float32)
```

```python
import numpy as np
from contextlib import ExitStack
import concourse.bass as bass
import concourse.mybir as mybir
from concourse.bass_utils import run_bass_kernel_spmd

F32 = mybir.dt.float32
BF16 = mybir.dt.bfloat16
I32 = mybir.dt.int32
ALU = mybir.AluOpType
AF = mybir.ActivationFunctionType
AX = mybir.AxisListType

ENGS = ("pe", "act", "dve", "pool", "sp")
S = 2048
D = 2048
NT = 16
BS = 256
NBLK = 112
NROWS = NBLK * BS
EPS = 1e-6
TWO_PI = 6.283185307179586
PI = 3.141592653589793


class MK:
    NDMA = 20
    LIMIT = 30000

    def __init__(self, nc, es):
        self.nc = nc
        self.es = es
        self.q = {e: [] for e in ENGS}
        self.epoch = {e: 0 for e in ENGS}
        self.sem = {}
        self.cnt = {e: 0 for e in ENGS}
        for e in ENGS:
            self.sem[(e, 0)] = es.enter_context(nc.semaphore(f"s_{e}0"))
        self.dsem = {}
        self.dcnt = {}
        self.dnext = {}
        for e in ("sp", "pool", "act"):
            self.dsem[e] = [es.enter_context(nc.semaphore(f"d_{e}{i}")) for i in range(self.NDMA)]
            self.dcnt[e] = [0] * self.NDMA
            self.dnext[e] = 0
        self.seen = {e: {} for e in ENGS}
        self.last_w = {}
        self.readers = {}
        self.n_inst = 0
        self.pes = None
        self.uid = 0

    def sb(self, name, shape, dt):
        self.uid += 1
        return self.pes.enter_context(self.nc.sbuf_tensor(f"sb{self.uid}_{name}", list(shape), dt))

    def ps(self, name, shape, dt=F32):
        self.uid += 1
        return self.pes.enter_context(self.nc.psum_tensor(f"ps{self.uid}_{name}", list(shape), dt))

    def _semobj(self, key):
        if key[0] == "c":
            return self.sem[(key[1], key[2])]
        return self.dsem[key[1]][key[2]]

    def _deps(self, reads, writes):
        deps = set()
        for k in reads:
            t = self.last_w.get(k)
            if t is not None:
                deps.add(t)
        for k in writes:
            t = self.last_w.get(k)
            if t is not None:
                deps.add(t)
            for t in self.readers.get(k, ()):
                deps.add(t)
        return deps

    def _emit_waits(self, eng, deps):
        seen = self.seen[eng]
        best = {}
        for (key, val) in deps:
            if eng == "pe" and key[0] == "c" and key[1] == "pe":
                continue
            if seen.get(key, 0) >= val:
                continue
            if best.get(key, 0) < val:
                best[key] = val
        for key, val in best.items():
            seen[key] = val
            so = self._semobj(key)
            self.q[eng].append(lambda e, so=so, val=val: e.wait_ge(so, val))
            self.n_inst += 1

    def _commit(self, tok, reads, writes):
        for k in writes:
            self.last_w[k] = tok
            self.readers[k] = []
        for k in reads:
            self.readers.setdefault(k, []).append(tok)

    def op(self, eng, fn, reads=(), writes=()):
        deps = self._deps(reads, writes)
        self._emit_waits(eng, deps)
        if self.cnt[eng] >= self.LIMIT:
            self.epoch[eng] += 1
            self.cnt[eng] = 0
            self.sem[(eng, self.epoch[eng])] = self.es.enter_context(self.nc.semaphore(f"s_{eng}{self.epoch[eng]}"))
        self.cnt[eng] += 1
        so = self.sem[(eng, self.epoch[eng])]
        self.q[eng].append(lambda e, fn=fn, so=so: fn(e).then_inc(so, 1))
        self.n_inst += 1
        tok = (("c", eng, self.epoch[eng]), self.cnt[eng])
        self._commit(tok, reads, writes)
        return tok

    def dma(self, eng, fn, reads=(), writes=()):
        deps = self._deps(reads, writes)
        i = self.dnext[eng]
        self.dnext[eng] = (i + 1) % self.NDMA
        key = ("d", eng, i)
        if self.dcnt[eng][i] > 0:
            deps.add((key, self.dcnt[eng][i]))
        self._emit_waits(eng, deps)
        self.dcnt[eng][i] += 16
        so = self.dsem[eng][i]
        self.q[eng].append(lambda e, fn=fn, so=so: fn(e).then_inc(so, 16))
        self.n_inst += 1
        tok = (key, self.dcnt[eng][i])
        self._commit(tok, reads, writes)
        return tok

    def barrier(self):
        toks = set()
        for e in ENGS:
            if self.cnt[e] > 0:
                toks.add((("c", e, self.epoch[e]), self.cnt[e]))
        for e in self.dsem:
            for i in range(self.NDMA):
                if self.dcnt[e][i] > 0:
                    toks.add((("d", e, i), self.dcnt[e][i]))
        for e in ENGS:
            self._emit_waits(e, toks)

    def flush(self):
        self.barrier()
        nc = self.nc
        q = self.q
        with nc.Block() as block:
            @block.tensor
            def _(e):
                for f in q["pe"]:
                    f(e)

            @block.scalar
            def _(e):
                for f in q["act"]:
                    f(e)

            @block.vector
            def _(e):
                for f in q["dve"]:
                    f(e)

            @block.gpsimd
            def _(e):
                for f in q["pool"]:
                    f(e)

            @block.sync
            def _(e):
                for f in q["sp"]:
                    f(e)
        self.q = {e: [] for e in ENGS}


def AP(t, F, off, dims, npart=128):
    return bass.AP(t, off, [[F, npart]] + [list(d) for d in dims])


def build(upto=99, dbg=False, with_experts=True):
    nc = bass.Bass("TRN2", target_bir_lowering=False)
    dt = nc.dram_tensor
    x_d = dt("x", [S, D], F32, kind="ExternalInput")
    c_d = dt("c", [128, 16], F32, kind="ExternalInput")
    pos_d = dt("positions", [1, S], I32, kind="ExternalInput")
    w_ada_d = dt("w_ada", [D, 6 * D], F32, kind="ExternalInput")
    b_ada_d = dt("b_ada", [1, 6 * D], F32, kind="ExternalInput")
    n1g_d = dt("norm1_gain", [1, D], F32, kind="ExternalInput")
    w_in_d = dt("w_in", [D, 5200], F32, kind="ExternalInput")
    convw_d = dt("dn_conv_w", [128, 24, 4], F32, kind="ExternalInput")
    alog_d = dt("dn_a_log", [1, 8], F32, kind="ExternalInput")
    dtb_d = dt("dn_dt_bias", [1, 8], F32, kind="ExternalInput")
    dng_d = dt("dn_norm_gain", [1, 128], F32, kind="ExternalInput")
    qg_d = dt("mla_q_norm_gain", [128, 4], F32, kind="ExternalInput")
    wqu_d = dt("w_q_up", [512, 1536], F32, kind="ExternalInput")
    kvg_d = dt("mla_kv_norm_gain", [128, 4], F32, kind="ExternalInput")
    wkvu_d = dt("w_kv_up", [512, 2048], F32, kind="ExternalInput")
    wout_d = dt("w_out", [D, D], F32, kind="ExternalInput")
    n2g_d = dt("norm2_gain", [1, D], F32, kind="ExternalInput")
    wr_d = dt("w_router", [D, 64], F32, kind="ExternalInput")
    rb_d = dt("router_bias", [1, 64], F32, kind="ExternalInput")
    if with_experts:
        wegu_d = dt("w_exp_gate_up", [64 * 128 * 4, 4096], F32, kind="ExternalInput")
        wed_d = dt("w_exp_down", [64 * 128 * 2, 4096], F32, kind="ExternalInput")
        wsgu_d = dt("w_sh_gate_up", [128, 16 * 1024], F32, kind="ExternalInput")
        wsd_d = dt("w_sh_down", [128, 4 * 2048], F32, kind="ExternalInput")
    fng_d = dt("final_norm_gain", [1, D], F32, kind="ExternalInput")
    invf_d = dt("invf", [64, 2], F32, kind="ExternalInput")
    out_d = dt("out", [S, D], F32, kind="ExternalOutput")
    ada_d = dt("ada_s", [6, D], F32, kind="Internal")
    qT_d = dt("qT_s", [8, 128, S], F32, kind="Internal")
    kT_d = dt("kT_s", [8, 128, S], F32, kind="Internal")
    k_d = dt("k_s", [S, 8, 128], F32, kind="Internal")
    v_d = dt("v_s", [S, 8, 128], F32, kind="Internal")
    z_d = dt("z_s", [S, 1024], F32, kind="Internal")
    mix_d = dt("mix_s", [S, D], BF16, kind="Internal")
    x1_d = dt("x1_s", [S, D], F32, kind="Internal")
    h2_d = dt("h2_s", [S + 128, D], BF16, kind="Internal")
    rm_d = dt("rm_s", [NROWS, 2], F32, kind="Internal")
    Y_d = dt("Y_s", [NROWS + S, D], BF16, kind="Internal")
    dbg_d = {}

    def dbgout(name, shape, dtype=F32):
        dbg_d[name] = dt("dbg_" + name, list(shape), dtype, kind="ExternalOutput")
        return dbg_d[name]

    es = ExitStack()
    with es:
        mk = MK(nc, es)
        V = lambda fn, r=(), w=(): mk.op("dve", fn, r, w)
        A = lambda fn, r=(), w=(): mk.op("act", fn, r, w)
        P = lambda fn, r=(), w=(): mk.op("pe", fn, r, w)
        G = lambda fn, r=(), w=(): mk.op("pool", fn, r, w)
        DM = lambda eng, fn, r=(), w=(): mk.dma(eng, fn, r, w)

        pers = ExitStack()
        es.enter_context(pers)
        mk.pes = pers
        ident_i = mk.sb("ident_i", [128, 128], I32)
        identf = mk.sb("identf", [128, 128], F32)
        identb = mk.sb("identb", [128, 128], BF16)
        onesf = mk.sb("onesf", [128, 128], F32)
        dif = mk.sb("dif", [128, 128], F32)
        bg_sb = mk.sb("bg_sb", [128, NT, 16], F32)
        G(lambda e: e.iota(ident_i[:], [[1, 128]], base=0, channel_multiplier=-1), w=["ident_i"])
        V(lambda e: e.tensor_copy(out=dif[:], in_=ident_i[:]), ["ident_i"], ["dif"])
        V(lambda e: e.tensor_single_scalar(out=identf[:], in_=dif[:], scalar=0.0, op=ALU.is_equal), ["dif"], ["identf"])
        V(lambda e: e.tensor_copy(out=identb[:], in_=identf[:]), ["identf"], ["identb"])
        G(lambda e: e.memset(onesf[:], 1.0), w=["onesf"])

        if upto >= 0:
            with ExitStack() as pes:
                mk.pes = pes
                c_sb = mk.sb("c_sb", [128, 16], F32)
                sc = mk.sb("sc", [128, 16], F32)
                wa = [mk.sb(f"wa{i}", [128, 16, 512], F32) for i in range(2)]
                arow = mk.sb("arow", [1, 6 * D], F32)
                brow = mk.sb("brow", [1, 6 * D], F32)
                grow = mk.sb("grow", [1, 2 * D], F32)
                psa = [mk.ps(f"psa{i}", [128, 512], F32) for i in range(2)]
                DM("sp", lambda e: e.dma_start(out=c_sb[:], in_=c_d.ap()), w=["c_sb"])
                DM("sp", lambda e: e.dma_start(out=brow[:], in_=b_ada_d.ap()), w=["brow"])
                DM("sp", lambda e: e.dma_start(out=grow[:, 0:D], in_=n1g_d.ap()), w=["grow0"])
                DM("sp", lambda e: e.dma_start(out=grow[:, D:2 * D], in_=n2g_d.ap()), w=["grow1"])
                A(lambda e: e.activation(out=sc[:], in_=c_sb[:], func=AF.Silu), ["c_sb"], ["sc"])
                wsrc = w_ada_d.ap().rearrange("(k p) n -> p k n", p=128)
                for j in range(24):
                    b = j % 2
                    DM("sp", lambda e, j=j, b=b: e.dma_start(out=wa[b][:], in_=wsrc[:, :, j * 512:(j + 1) * 512]), w=[f"wa{b}"])

                    def mm(e, b=b):
                        for k in range(16):
                            r = e.matmul(psa[b][0:1, :], lhsT=sc[:, k:k + 1], rhs=wa[b][:, k, :], start=(k == 0), stop=(k == 15))
                        return r
                    P(mm, ["sc", f"wa{b}"], [f"psa{b}"])
                    V(lambda e, j=j, b=b: e.tensor_tensor(out=arow[:, j * 512:(j + 1) * 512], in0=psa[b][0:1, :], in1=brow[:, j * 512:(j + 1) * 512], op=ALU.add),
                      ["brow"], [f"psa{b}", f"arow{j}"])
                akeys = [f"arow{j}" for j in range(24)]
                V(lambda e: e.scalar_tensor_tensor(out=arow[:, D:2 * D], in0=arow[:, D:2 * D], scalar=1.0, in1=grow[:, 0:D], op0=ALU.add, op1=ALU.mult), akeys + ["grow0"], ["arowG1"])
                V(lambda e: e.scalar_tensor_tensor(out=arow[:, 4 * D:5 * D], in0=arow[:, 4 * D:5 * D], scalar=1.0, in1=grow[:, D:2 * D], op0=ALU.add, op1=ALU.mult), akeys + ["grow1"], ["arowG2"])
                DM("sp", lambda e: e.dma_start(out=ada_d.ap().rearrange("(o r) n -> o (r n)", o=1), in_=arow[:]), akeys + ["arowG1", "arowG2"], ["ada_d"])
                mk.flush()

        p13 = ExitStack()
        es.enter_context(p13)
        if upto >= 1:
            mk.pes = p13
            cqn = mk.sb("cqn", [128, 4, S], BF16)
            ckvn = mk.sb("ckvn", [128, 4, S], BF16)
            KTpe = mk.sb("KTpe", [64, S], BF16)
            CS = mk.sb("CS", [64, S], F32)
            SN = mk.sb("SN", [64, S], F32)
            p12 = ExitStack()
            p13.enter_context(p12)
            mk.pes = p12
            hT = mk.sb("hT", [128, 16, S], BF16)
            with ExitStack() as pes:
                mk.pes = pes
                G1b = mk.sb("G1b", [128, D], F32)
                S1b = mk.sb("S1b", [128, D], F32)
                xt = [mk.sb(f"xt{i}", [128, D], F32) for i in range(2)]
                junk = mk.sb("junk", [128, D], BF16)
                tmpf = mk.sb("tmpf", [128, D], F32)
                hbf = [mk.sb(f"hbf{i}", [128, D], BF16) for i in range(2)]
                st = mk.sb("st", [128, NT, 2], F32)
                pst = [mk.ps(f"pst{i}", [128, 1024], BF16) for i in range(2)]
                DM("sp", lambda e: e.dma_start(out=S1b[:], in_=ada_d.ap()[0:1, :].partition_broadcast(128)), ["ada_d"], ["S1b"])
                DM("sp", lambda e: e.dma_start(out=G1b[:], in_=ada_d.ap()[1:2, :].partition_broadcast(128)), ["ada_d"], ["G1b"])
                for t in range(NT):
                    b = t % 2
                    DM("sp", lambda e, t=t, b=b: e.dma_start(out=xt[b][:], in_=x_d.ap()[t * 128:(t + 1) * 128, :]), w=[f"xt{b}"])
                    A(lambda e, t=t, b=b: e.activation(out=junk[:], in_=xt[b][:], func=AF.Square, accum_out=st[:, t, 0:1]), [f"xt{b}"], ["junk", f"st{t}"])
                    A(lambda e, t=t: e.activation(out=st[:, t, 1:2], in_=st[:, t, 0:1], func=AF.Sqrt, bias=EPS, scale=1.0 / D), [f"st{t}"], [f"st{t}"])
                    V(lambda e, t=t: e.reciprocal(out=st[:, t, 1:2], in_=st[:, t, 1:2]), [f"st{t}"], [f"st{t}"])
                    V(lambda e, t=t, b=b: e.scalar_tensor_tensor(out=tmpf[:], in0=xt[b][:], scalar=st[:, t, 1:2], in1=G1b[:], op0=ALU.mult, op1=ALU.mult), [f"xt{b}", f"st{t}", "G1b"], ["tmpf"])
                    V(lambda e, b=b: e.tensor_tensor(out=hbf[b][:], in0=tmpf[:], in1=S1b[:], op=ALU.add), ["tmpf", "S1b"], [f"hbf{b}"])
                    for hh in range(2):
                        def tr(e, b=b, hh=hh):
                            for k in range(8):
                                kk = hh * 8 + k
                                r = e.transpose(pst[hh][:, k * 128:(k + 1) * 128], hbf[b][:, kk * 128:(kk + 1) * 128], identb[:])
                            return r
                        P(tr, [f"hbf{b}", "identb"], [f"pst{hh}"])
                        eng = V if hh == 0 else A
                        if hh == 0:
                            V(lambda e, t=t, hh=hh: e.tensor_copy(out=hT[:, hh * 8:(hh + 1) * 8, t * 128:(t + 1) * 128], in_=pst[hh][:].rearrange("p (k c) -> p k c", k=8)), [], [f"pst{hh}", f"hT{t}"])
                        else:
                            A(lambda e, t=t, hh=hh: e.copy(out=hT[:, hh * 8:(hh + 1) * 8, t * 128:(t + 1) * 128], in_=pst[hh][:].rearrange("p (k c) -> p k c", k=8)), [], [f"pst{hh}", f"hT{t}b"])
                if dbg and upto == 1:
                    dd = dbgout("hT", [128, 16, S], BF16)
                    DM("sp", lambda e: e.dma_start(out=dd.ap(), in_=hT[:]), [f"hT{t}" for t in range(NT)] + [f"hT{t}b" for t in range(NT)], ["dbg"])
                mk.flush()
        hTkeys = [f"hT{t}" for t in range(NT)] + [f"hT{t}b" for t in range(NT)]

        if upto >= 2:
            wsrc = w_in_d.ap().rearrange("(k p) n -> p k n", p=128)
            with ExitStack() as pes:
                mk.pes = pes
                wb = [mk.sb(f"wb{i}", [128, 16, 512], BF16) for i in range(2)]
                cw = mk.sb("cw", [128, 24, 4], F32)
                raws = [mk.sb(f"raw{i}", [128, 3 + S], F32) for i in range(2)]
                acc = mk.sb("acc", [128, S], F32)
                cs = mk.sb("cs", [128, S], F32)
                sq = mk.sb("sq", [128, S], F32)
                nrm = acc
                tok = mk.sb("tok", [128, NT, 128], F32)
                PQ = mk.ps("PQ", [128, S], F32)
                PN = mk.ps("PN", [128, 1024], F32)
                PT = mk.ps("PT", [128, 1024], F32)
                DM("sp", lambda e: e.dma_start(out=cw[:], in_=convw_d.ap()), w=["cw"])
                for i in range(2):
                    G(lambda e, i=i: e.memset(raws[i][:, 0:3], 0.0), w=[f"rawpad{i}"])

                def wload(blk):
                    b = blk % 2
                    DM("pool", lambda e: e.dma_start(out=wb[b][:], in_=wsrc[:, :, blk * 512:(blk + 1) * 512]), w=[f"wb{b}"])

                def stageA(cc):
                    blk, ci = cc // 4, cc % 4
                    b = blk % 2
                    rb = cc % 2
                    raw = raws[rb]
                    if ci == 0 and blk + 1 < 6:
                        wload(blk + 1)
                    for qd in range(4):
                        def mm(e, qd=qd):
                            for k in range(16):
                                r = e.matmul(PQ[:, qd * 512:(qd + 1) * 512], lhsT=wb[b][:, k, ci * 128:(ci + 1) * 128], rhs=hT[:, k, qd * 512:(qd + 1) * 512], start=(k == 0), stop=(k == 15))
                            return r
                        P(mm, [f"wb{b}"] + hTkeys, [f"PQ{qd}"])
                        A(lambda e, qd=qd: e.copy(out=raw[:, 3 + qd * 512:3 + (qd + 1) * 512], in_=PQ[:, qd * 512:(qd + 1) * 512]), [], [f"PQ{qd}", f"raw{rb}_{qd}"])
                        yield

                def stageB(cc):
                    rb = cc % 2
                    raw = raws[rb]
                    rk = [f"raw{rb}_{q}" for q in range(4)] + [f"rawpad{rb}"]
                    V(lambda e: e.tensor_scalar(out=acc[:], in0=raw[:, 3:3 + S], scalar1=cw[:, cc, 3:4], scalar2=None, op0=ALU.mult), rk + ["cw"], ["acc"])
                    for j in (2, 1, 0):
                        V(lambda e, j=j: e.scalar_tensor_tensor(out=acc[:], in0=raw[:, j:j + S], scalar=cw[:, cc, j:j + 1], in1=acc[:], op0=ALU.mult, op1=ALU.add), rk + ["cw", "acc"], ["acc"])
                    yield
                    A(lambda e: e.activation(out=cs[:], in_=acc[:], func=AF.Silu), ["acc"], ["cs"])
                    yield
                    if cc < 16:
                        head = cc % 8
                        isq = cc < 8
                        A(lambda e: e.activation(out=sq[:], in_=cs[:], func=AF.Square), ["cs"], ["sq"])
                        for hf in range(2):
                            def mm2(e, hf=hf):
                                for q2 in range(2):
                                    r = e.matmul(PN[:, q2 * 512:(q2 + 1) * 512], lhsT=onesf[:], rhs=sq[:, hf * 1024 + q2 * 512: hf * 1024 + (q2 + 1) * 512], start=True, stop=True)
                                return r
                            P(mm2, ["sq", "onesf"], ["PN"])
                            scl = 128.0 if isq else 1.0
                            A(lambda e, hf=hf, scl=scl: e.activation(out=nrm[:, hf * 1024:(hf + 1) * 1024], in_=PN[:], func=AF.Sqrt, bias=EPS * scl, scale=scl), [], ["PN", "acc"])
                            yield
                        V(lambda e: e.reciprocal(out=nrm[:], in_=nrm[:]), ["acc"], ["acc"])
                        V(lambda e: e.tensor_tensor(out=cs[:], in0=cs[:], in1=nrm[:], op=ALU.mult), ["cs", "acc"], ["cs"])
                        dst = qT_d if isq else kT_d
                        DM("sp", lambda e, dst=dst, head=head: e.dma_start(out=dst.ap()[head], in_=cs[:]), ["cs"], [f"qk_d{cc}"])
                        yield
                    if cc >= 8:
                        head = cc % 8
                        dst = k_d if cc < 16 else v_d
                        for g4 in range(4):
                            def tr(e, g4=g4):
                                for i in range(4):
                                    t = g4 * 4 + i
                                    r = e.transpose(PT[:, ((g4 % 2) * 4 + i) * 128:((g4 % 2) * 4 + i + 1) * 128], cs[:, t * 128:(t + 1) * 128], identf[:])
                                return r
                            P(tr, ["cs", "identf"], [f"PT{g4 % 2}"])
                            V(lambda e, g4=g4: e.tensor_copy(out=tok[:, g4 * 4:(g4 + 1) * 4, :], in_=PT[:, (g4 % 2) * 512:((g4 % 2) + 1) * 512].rearrange("p (t c) -> p t c", t=4)), [], [f"PT{g4 % 2}", f"tok{g4}"])
                            yield
                        DM("sp", lambda e, dst=dst, head=head: e.dma_start(out=dst.ap().rearrange("(t p) h c -> p t h c", p=128)[:, :, head, :], in_=tok[:]), [f"tok{g}" for g in range(4)], [f"kv_d{cc}"])
                    yield

                def interleave2(gens):
                    gens = [g for g in gens if g is not None]
                    while gens:
                        for g in list(gens):
                            try:
                                next(g)
                            except StopIteration:
                                gens.remove(g)

                wload(0)
                interleave2([stageA(0)])
                for cc in range(24):
                    interleave2([stageA(cc + 1) if cc + 1 < 24 else None, stageB(cc)])
                mk.flush()
            with ExitStack() as pes:
                mk.pes = pes
                wb = [mk.sb(f"wz{i}", [128, 16, 512], BF16) for i in range(2)]
                wsm = mk.sb("wsm", [128, 16, 16], BF16)
                zs = [mk.sb(f"zs{i}", [128, 512], F32) for i in range(2)]
                dtb = mk.sb("dtb", [128, 8], F32)
                nA = mk.sb("nA", [128, 8], F32)
                tmp8 = mk.sb("tmp8", [128, NT, 8], F32)
                PZ = [mk.ps(f"PZ{i}", [128, 512], F32) for i in range(2)]
                PB = mk.ps("PB", [128, NT * 16], F32)
                for blk in range(2):
                    DM("pool", lambda e, blk=blk: e.dma_start(out=wb[blk][:], in_=wsrc[:, :, 3072 + blk * 512:3072 + (blk + 1) * 512]), w=[f"wz{blk}"])
                DM("pool", lambda e: e.dma_start(out=wsm[:], in_=wsrc[:, :, 4096:4112]), w=["wsm"])
                DM("sp", lambda e: e.dma_start(out=dtb[:], in_=dtb_d.ap().partition_broadcast(128)), w=["dtb"])
                DM("sp", lambda e: e.dma_start(out=nA[:], in_=alog_d.ap().partition_broadcast(128)), w=["nA"])
                i = 0
                for blk in range(2):
                    for t in range(NT):
                        b = i % 2
                        i += 1

                        def mm(e, blk=blk, t=t, b=b):
                            for k in range(16):
                                r = e.matmul(PZ[b][:], lhsT=hT[:, k, t * 128:(t + 1) * 128], rhs=wb[blk][:, k, :], start=(k == 0), stop=(k == 15))
                            return r
                        P(mm, [f"wz{blk}"] + hTkeys, [f"PZ{b}"])
                        A(lambda e, b=b: e.activation(out=zs[b][:], in_=PZ[b][:], func=AF.Silu), [], [f"PZ{b}", f"zs{b}"])
                        DM("sp", lambda e, blk=blk, t=t, b=b: e.dma_start(out=z_d.ap()[t * 128:(t + 1) * 128, blk * 512:(blk + 1) * 512], in_=zs[b][:]), [f"zs{b}"], [f"z_d{blk}_{t}"])
                for t in range(NT):
                    def mm(e, t=t):
                        for k in range(16):
                            r = e.matmul(PB[:, t * 16:(t + 1) * 16], lhsT=hT[:, k, t * 128:(t + 1) * 128], rhs=wsm[:, k, :], start=(k == 0), stop=(k == 15))
                        return r
                    P(mm, ["wsm"] + hTkeys, ["PB"])
                PB3 = PB[:].rearrange("p (t c) -> p t c", t=NT)
                A(lambda e: e.activation(out=bg_sb[:, :, 0:8], in_=PB3[:, :, 0:8], func=AF.Sigmoid), [], ["PB", "bg_b"])
                V(lambda e: e.tensor_tensor(out=tmp8[:], in0=PB3[:, :, 8:16], in1=AP(dtb, 8, 0, [[0, NT], [1, 8]]), op=ALU.add), ["dtb"], ["PB", "tmp8"])
                A(lambda e: e.activation(out=tmp8[:], in_=tmp8[:], func=AF.Exp), ["tmp8"], ["tmp8"])
                A(lambda e: e.activation(out=tmp8[:], in_=tmp8[:], func=AF.Ln, bias=1.0, scale=1.0), ["tmp8"], ["tmp8"])
                A(lambda e: e.activation(out=nA[:], in_=nA[:], func=AF.Exp), ["nA"], ["nA"])
                V(lambda e: e.scalar_tensor_tensor(out=bg_sb[:, :, 8:16], in0=tmp8[:], scalar=-1.0, in1=AP(nA, 8, 0, [[0, NT], [1, 8]]), op0=ALU.mult, op1=ALU.mult), ["tmp8", "nA"], ["bg_g"])
                mk.flush()
            with ExitStack() as pes:
                mk.pes = pes
                wb = [mk.sb(f"wl{i}", [128, 16, 512], BF16) for i in range(2)]
                lat = mk.sb("lat", [128, 4, S], BF16)
                sq = mk.sb("sq2", [128, S], F32)
                rs = mk.sb("rs", [128, S], F32)
                gq = mk.sb("gq", [128, 8], F32)
                PQ = mk.ps("PQ2", [128, S], F32)
                PN = mk.ps("PN2", [128, S], F32)
                DM("sp", lambda e: e.dma_start(out=gq[:, 0:4], in_=qg_d.ap()), w=["gq0"])
                DM("sp", lambda e: e.dma_start(out=gq[:, 4:8], in_=kvg_d.ap()), w=["gq1"])
                for blk in range(2):
                    DM("pool", lambda e, blk=blk: e.dma_start(out=wb[blk][:], in_=wsrc[:, :, 4112 + blk * 512:4112 + (blk + 1) * 512]), w=[f"wl{blk}"])
                    for ci in range(4):
                        for qd in range(4):
                            def mm(e, blk=blk, ci=ci, qd=qd):
                                for k in range(16):
                                    r = e.matmul(PQ[:, qd * 512:(qd + 1) * 512], lhsT=wb[blk][:, k, ci * 128:(ci + 1) * 128], rhs=hT[:, k, qd * 512:(qd + 1) * 512], start=(k == 0), stop=(k == 15))
                                return r
                            P(mm, [f"wl{blk}"] + hTkeys, [f"PQ{qd}"])
                            V(lambda e, ci=ci, qd=qd: e.tensor_copy(out=lat[:, ci, qd * 512:(qd + 1) * 512], in_=PQ[:, qd * 512:(qd + 1) * 512]), [], [f"PQ{qd}", f"lat{ci}_{qd}"])
                            A(lambda e, qd=qd: e.activation(out=sq[:, qd * 512:(qd + 1) * 512], in_=PQ[:, qd * 512:(qd + 1) * 512], func=AF.Square), [], [f"PQ{qd}", f"sq{qd}"])
                            P(lambda e, ci=ci, qd=qd: e.matmul(PN[:, qd * 512:(qd + 1) * 512], lhsT=onesf[:], rhs=sq[:, qd * 512:(qd + 1) * 512], start=(ci == 0), stop=(ci == 3)), [f"sq{qd}", "onesf"], [f"PN{qd}"])
                    for qd in range(4):
                        A(lambda e, qd=qd: e.activation(out=rs[:, qd * 512:(qd + 1) * 512], in_=PN[:, qd * 512:(qd + 1) * 512], func=AF.Sqrt, bias=EPS, scale=1.0 / 512), [], [f"PN{qd}", f"rs{qd}"])
                        V(lambda e, qd=qd: e.reciprocal(out=rs[:, qd * 512:(qd + 1) * 512], in_=rs[:, qd * 512:(qd + 1) * 512]), [f"rs{qd}"], [f"rs{qd}"])
                    dstn = cqn if blk == 0 else ckvn
                    for ci in range(4):
                        V(lambda e, blk=blk, ci=ci, dstn=dstn: e.scalar_tensor_tensor(out=dstn[:, ci, :], in0=lat[:, ci, :], scalar=gq[:, blk * 4 + ci:blk * 4 + ci + 1], in1=rs[:], op0=ALU.mult, op1=ALU.mult),
                          [f"lat{ci}_{q}" for q in range(4)] + [f"rs{q}" for q in range(4)] + ["gq0", "gq1"], [f"latn{blk}_{ci}"])
                mk.flush()
            with ExitStack() as pes:
                mk.pes = pes
                wk = mk.sb("wk", [128, 16, 64], BF16)
                wks = mk.sb("wks", [128, 16, 64], BF16)
                posi = mk.sb("posi", [64, S], I32)
                ang = mk.sb("ang", [64, S], F32)
                kf = mk.sb("kf", [64, S], F32)
                ki = mk.sb("ki", [64, S], I32)
                invf = mk.sb("invf", [64, 2], F32)
                t1 = mk.sb("t1", [64, S], F32)
                PA = mk.ps("PA", [128, S], F32)
                PBm = mk.ps("PBm", [128, S], F32)
                DM("pool", lambda e: e.dma_start(out=wk[:], in_=wsrc[:, :, 5136:5200]), w=["wk"])
                DM("pool", lambda e: e.dma_start(out=wks[:, :, 0:32], in_=wsrc[:, :, 5168:5200]), w=["wks0"])
                DM("pool", lambda e: e.dma_start(out=wks[:, :, 32:64], in_=wsrc[:, :, 5136:5168]), w=["wks1"])
                DM("sp", lambda e: e.dma_start(out=invf[:], in_=invf_d.ap()), w=["invf"])
                DM("sp", lambda e: e.dma_start(out=posi[:], in_=pos_d.ap().partition_broadcast(64)), w=["posi"])
                V(lambda e: e.tensor_copy(out=ang[:], in_=posi[:]), ["posi"], ["ang"])
                V(lambda e: e.tensor_scalar(out=ang[:], in0=ang[:], scalar1=invf[:, 0:1], scalar2=None, op0=ALU.mult), ["ang", "invf"], ["ang"])
                for which, tab, shift in (("sin", SN, 0.0), ("cos", CS, PI / 2)):
                    V(lambda e, shift=shift: e.tensor_scalar(out=kf[:], in0=ang[:], scalar1=shift, scalar2=1.0 / TWO_PI, op0=ALU.add, op1=ALU.mult), ["ang"], ["kf"])
                    V(lambda e: e.tensor_copy(out=ki[:], in_=kf[:]), ["kf"], ["ki"])
                    V(lambda e: e.tensor_copy(out=kf[:], in_=ki[:]), ["ki"], ["kf"])
                    V(lambda e, shift=shift: e.scalar_tensor_tensor(out=kf[:], in0=kf[:], scalar=-TWO_PI, in1=ang[:], op0=ALU.mult, op1=ALU.add), ["kf", "ang"], ["kf"])
                    V(lambda e, shift=shift: e.tensor_scalar(out=kf[:], in0=kf[:], scalar1=shift, scalar2=PI, op0=ALU.add, op1=ALU.min), ["kf"], ["kf"])
                    V(lambda e: e.tensor_scalar(out=kf[:], in0=kf[:], scalar1=-PI, scalar2=None, op0=ALU.max), ["kf"], ["kf"])
                    A(lambda e, tab=tab: e.activation(out=tab[:], in_=kf[:], func=AF.Sin), ["kf"], [which])
                V(lambda e: e.tensor_scalar(out=SN[:], in0=SN[:], scalar1=invf[:, 1:2], scalar2=None, op0=ALU.mult), ["sin", "invf"], ["sin"])
                for qd in range(4):
                    def mm(e, qd=qd):
                        for k in range(16):
                            r = e.matmul(PA[0:64, qd * 512:(qd + 1) * 512], lhsT=wk[:, k, :], rhs=hT[:, k, qd * 512:(qd + 1) * 512], start=(k == 0), stop=(k == 15))
                        return r
                    P(mm, ["wk"] + hTkeys, [f"PA{qd}"])

                    def mm2(e, qd=qd):
                        for k in range(16):
                            r = e.matmul(PBm[0:64, qd * 512:(qd + 1) * 512], lhsT=wks[:, k, :], rhs=hT[:, k, qd * 512:(qd + 1) * 512], start=(k == 0), stop=(k == 15))
                        return r
                    P(mm2, ["wks0", "wks1"] + hTkeys, [f"PB{qd}"])
                    sl = slice(qd * 512, (qd + 1) * 512)
                    V(lambda e, sl=sl: e.tensor_tensor(out=t1[:, sl], in0=PA[0:64, sl], in1=CS[:, sl], op=ALU.mult), ["cos"], [f"PA{qd}", f"t1{qd}"])
                    V(lambda e, sl=sl: e.tensor_tensor(out=kf[:, sl], in0=PBm[0:64, sl], in1=SN[:, sl], op=ALU.mult), ["sin"], [f"PB{qd}", "kf"])
                    V(lambda e, sl=sl: e.tensor_tensor(out=KTpe[:, sl], in0=t1[:, sl], in1=kf[:, sl], op=ALU.add), [f"t1{qd}", "kf"], [f"KTpe{qd}"])
                if dbg and upto == 2:
                    d1 = dbgout("cqn", [128, 4, S], BF16)
                    d2 = dbgout("KTpe", [64, S], BF16)
                    d3 = dbgout("bg", [128, NT, 16], F32)
                    d4 = dbgout("ckvn", [128, 4, S], BF16)
                    mk.barrier()
                    DM("sp", lambda e: e.dma_start(out=d4.ap(), in_=ckvn[:]), [], ["dbg4"])
                    for nm, src, shp in (("qT", qT_d, [8, 128, S]), ("kT", kT_d, [8, 128, S]), ("k", k_d, [S, 8, 128]), ("v", v_d, [S, 8, 128]), ("z", z_d, [S, 1024]), ("ada", ada_d, [6, D])):
                        dd = dbgout(nm, shp, F32)
                        DM("sp", lambda e, dd=dd, src=src: e.dma_start(out=dd.ap(), in_=src.ap()), [], ["dbg_" + nm])
                    DM("sp", lambda e: e.dma_start(out=d1.ap(), in_=cqn[:]), [], ["dbg1"])
                    DM("sp", lambda e: e.dma_start(out=d2.ap(), in_=KTpe[:]), [], ["dbg2"])
                    DM("sp", lambda e: e.dma_start(out=d3.ap(), in_=bg_sb[:]), [], ["dbg3"])
                mk.flush()
            p12.close()

        if upto >= 3:
            with ExitStack() as pes:
                mk.pes = pes
                SC = 192.0 ** -0.5
                wq = mk.sb("wq", [128, 4, 1536], BF16)
                wqs = mk.sb("wqs", [128, 4, 8, 64], BF16)
                wkv = mk.sb("wkv", [128, 4, 2048], BF16)
                QTn = mk.sb("QTn", [128, S], BF16)
                QTp = mk.sb("QTp", [64, S], BF16)
                KTn = mk.sb("KTn", [128, S], BF16)
                Vaug = mk.sb("Vaug", [128, NT, 130], BF16)
                PTs = [mk.sb(f"PTs{i}", [128, 512], BF16) for i in range(4)]
                it0 = [0]
                mla_tok = mk.sb("mla_tok", [128, NT, 1024], BF16)
                t1 = mk.sb("t1m", [64, S], F32)
                t2 = mk.sb("t2m", [64, S], F32)
                triu = mk.sb("triu", [128, 128], BF16)
                rec = mk.sb("rec", [128, 4], F32)
                P0 = mk.ps("P0", [128, S], F32)
                P1 = mk.ps("P1", [128, S], F32)
                DM("pool", lambda e: e.dma_start(out=wq[:], in_=wqu_d.ap().rearrange("(k p) n -> p k n", p=128)), w=["wq"])
                DM("pool", lambda e: e.dma_start(out=wkv[:], in_=wkvu_d.ap().rearrange("(k p) n -> p k n", p=128)), w=["wkv"])
                src4 = wqu_d.ap().rearrange("(k p) (h c) -> p k h c", p=128, c=192)
                for k4 in range(4):
                    DM("pool", lambda e, k4=k4: e.dma_start(out=wqs[:, k4, :, 0:32], in_=src4[:, k4, :, 160:192]), w=[f"wqs0_{k4}"])
                    DM("pool", lambda e, k4=k4: e.dma_start(out=wqs[:, k4, :, 32:64], in_=src4[:, k4, :, 128:160]), w=[f"wqs1_{k4}"])
                V(lambda e: e.tensor_single_scalar(out=triu[:], in_=dif[:], scalar=0.0, op=ALU.is_ge), ["dif"], ["triu"])
                G(lambda e: e.memset(Vaug[:, :, 128:130], 1.0), w=["Vaug1"])
                latq = [f"latn0_{c}" for c in range(4)]
                latkv = [f"latn1_{c}" for c in range(4)]
                it = 0
                for h in range(8):
                    for qd in range(4):
                        sl = slice(qd * 512, (qd + 1) * 512)

                        def mm(e, h=h, sl=sl):
                            for k in range(4):
                                r = e.matmul(P0[:, sl], lhsT=wq[:, k, h * 192:h * 192 + 128], rhs=cqn[:, k, sl], start=(k == 0), stop=(k == 3))
                            return r
                        P(mm, ["wq"] + latq, [f"P0_{qd}"])
                        A(lambda e, sl=sl: e.mul(out=QTn[:, sl], in_=P0[:, sl], mul=SC), [], [f"P0_{qd}", f"QTn{qd}"])
                    for qd in range(4):
                        sl = slice(qd * 512, (qd + 1) * 512)

                        def mma(e, h=h, sl=sl):
                            for k in range(4):
                                r = e.matmul(P0[0:64, sl], lhsT=wq[:, k, h * 192 + 128:h * 192 + 192], rhs=cqn[:, k, sl], start=(k == 0), stop=(k == 3))
                            return r
                        P(mma, ["wq"] + latq, [f"P0_{qd}"])

                        def mmb(e, h=h, sl=sl):
                            for k in range(4):
                                r = e.matmul(P1[0:64, sl], lhsT=wqs[:, k, h, :], rhs=cqn[:, k, sl], start=(k == 0), stop=(k == 3))
                            return r
                        P(mmb, [f"wqs{a}_{b}" for a in range(2) for b in range(4)] + latq, [f"P1_{qd}"])
                        V(lambda e, sl=sl: e.scalar_tensor_tensor(out=t1[:, sl], in0=P0[0:64, sl], scalar=SC, in1=CS[:, sl], op0=ALU.mult, op1=ALU.mult), ["cos"], [f"P0_{qd}", f"t1m{qd}"])
                        V(lambda e, sl=sl: e.scalar_tensor_tensor(out=t2[:, sl], in0=P1[0:64, sl], scalar=SC, in1=SN[:, sl], op0=ALU.mult, op1=ALU.mult), ["sin"], [f"P1_{qd}", f"t2m{qd}"])
                        V(lambda e, sl=sl: e.tensor_tensor(out=QTp[:, sl], in0=t1[:, sl], in1=t2[:, sl], op=ALU.add), [f"t1m{qd}", f"t2m{qd}"], [f"QTp{qd}"])
                    for qd in range(4):
                        sl = slice(qd * 512, (qd + 1) * 512)

                        def mmk(e, h=h, sl=sl):
                            for k in range(4):
                                r = e.matmul(P0[:, sl], lhsT=wkv[:, k, h * 256:h * 256 + 128], rhs=ckvn[:, k, sl], start=(k == 0), stop=(k == 3))
                            return r
                        P(mmk, ["wkv"] + latkv, [f"P0_{qd}"])
                        V(lambda e, sl=sl: e.tensor_copy(out=KTn[:, sl], in_=P0[:, sl]), [], [f"P0_{qd}", f"KTn{qd}"])
                    for g4 in range(4):
                        def mmv(e, h=h, g4=g4):
                            for i in range(4):
                                t = g4 * 4 + i
                                for k in range(4):
                                    r = e.matmul(P1[:, t * 128:(t + 1) * 128], lhsT=ckvn[:, k, t * 128:(t + 1) * 128], rhs=wkv[:, k, h * 256 + 128:h * 256 + 256], start=(k == 0), stop=(k == 3))
                            return r
                        P(mmv, ["wkv"] + latkv, [f"P1_{g4}"])
                        A(lambda e, g4=g4: e.copy(out=Vaug[:, g4 * 4:(g4 + 1) * 4, 0:128], in_=P1[:, g4 * 512:(g4 + 1) * 512].rearrange("p (t c) -> p t c", t=4)), [], [f"P1_{g4}", f"Vaug_{g4}"])
                    qk = [f"QTn{q}" for q in range(4)] + [f"QTp{q}" for q in range(4)] + [f"KTn{q}" for q in range(4)] + [f"KTpe{q}" for q in range(4)]
                    vk = [f"Vaug_{g}" for g in range(4)] + ["Vaug1"]
                    for Gq in range(4):
                        its = []
                        for j in range(4 * Gq + 4):
                            qlo = max(Gq * 512, j * 128)
                            its.append((j, qlo, (Gq + 1) * 512 - qlo))
                        LA = 3

                        def emit_s(ii, its=its):
                            j, qlo, width = its[ii]
                            sbk = (ii + it0[0]) % 4

                            def mms(e):
                                e.matmul(P0[:, sbk * 512:sbk * 512 + width], lhsT=KTn[:, j * 128:(j + 1) * 128], rhs=QTn[:, qlo:qlo + width], start=True, stop=False)
                                return e.matmul(P0[:, sbk * 512:sbk * 512 + width], lhsT=KTpe[:, j * 128:(j + 1) * 128], rhs=QTp[:, qlo:qlo + width], start=False, stop=True)
                            P(mms, qk, [f"P0_{sbk}"])
                            A(lambda e: e.activation(out=PTs[sbk][:, 0:width], in_=P0[:, sbk * 512:sbk * 512 + width], func=AF.Exp), [], [f"P0_{sbk}", f"PTs{sbk}"])
                            if j >= 4 * Gq:
                                V(lambda e: e.tensor_tensor(out=PTs[sbk][:, 0:128], in0=PTs[sbk][:, 0:128], in1=triu[:], op=ALU.mult), ["triu", f"PTs{sbk}"], [f"PTs{sbk}"])

                        def emit_pv(ii, its=its, Gq=Gq):
                            j, qlo, width = its[ii]
                            sbk = (ii + it0[0]) % 4
                            qbs = list(range(qlo // 128, (Gq + 1) * 4))

                            def mmpv(e):
                                for qb in qbs:
                                    a = qb - 4 * Gq
                                    off = qb * 128 - qlo
                                    r = e.matmul(P1[:, a * 512:a * 512 + 129], lhsT=PTs[sbk][:, off:off + 128], rhs=Vaug[:, j, 0:129], start=(j == 0), stop=(j == qb))
                                return r
                            P(mmpv, [f"PTs{sbk}"] + vk, [f"P1_{qb - 4 * Gq}" for qb in qbs])

                        n_it = len(its)
                        for ii in range(min(LA, n_it)):
                            emit_s(ii)
                        for ii in range(n_it):
                            if ii + LA < n_it:
                                emit_s(ii + LA)
                            emit_pv(ii)
                        it0[0] += n_it
                        for a in range(4):
                            qb = 4 * Gq + a
                            V(lambda e, a=a: e.reciprocal(out=rec[:, a:a + 1], in_=P1[:, a * 512 + 128:a * 512 + 129]), [], [f"P1_{a}", f"rec{a}"])
                            V(lambda e, a=a, qb=qb, h=h: e.tensor_scalar(out=mla_tok[:, qb, h * 128:(h + 1) * 128], in0=P1[:, a * 512:a * 512 + 128], scalar1=rec[:, a:a + 1], scalar2=None, op0=ALU.mult), [f"rec{a}"], [f"P1_{a}", f"mla_tok{h}_{qb}"])
                mk.barrier()
                DM("sp", lambda e: e.dma_start(out=mix_d.ap().rearrange("(t p) c -> p t c", p=128)[:, :, 1024:2048], in_=mla_tok[:]), [], ["mix_mla"])
                if dbg and upto == 3:
                    dd = dbgout("mla", [128, NT, 1024], BF16)
                    DM("sp", lambda e: e.dma_start(out=dd.ap(), in_=mla_tok[:]), [], ["dbgm"])
                mk.flush()
        p13.close()

        if upto >= 4:
            with ExitStack() as pes:
                mk.pes = pes
                H8 = 8
                Uincl = mk.sb("Uincl", [128, 128], F32)
                offd = mk.sb("offd", [128, 128], F32)
                M2rep = mk.sb("M2rep", [128, 4, 128], F32)
                gdn = mk.sb("gdn", [128, 128], F32)
                Sst = mk.sb("Sst", [128, H8, 128], F32)
                ld = [[mk.sb(f"ld{n}{i}", [128, H8, 128], F32) for n in range(5)] for i in range(2)]
                gsmq = [mk.sb(f"gsm{i}", [128, 16], F32) for i in range(2)]
                smq = [mk.sb(f"sm{i}", [128, 6, 8], F32) for i in range(2)]
                rhsD = mk.sb("rhsD", [128, H8, 128], F32)
                E2 = mk.sb("E2", [128, H8, 128], F32)
                Amat = mk.sb("Amat", [128, H8, 128], F32)
                Atq = [mk.sb(f"At{i}", [128, H8, 128], F32) for i in range(2)]
                Pq = [mk.sb(f"Pq{i}", [128, H8, 128], F32) for i in range(2)]
                Ptq = [mk.sb(f"Ptq{i}", [128, H8, 128], F32) for i in range(2)]
                Xq0 = [mk.sb(f"Xq0{i}", [128, H8, 128], F32) for i in range(2)]
                Xq1 = mk.sb("Xq1", [128, H8, 128], F32)
                vbq = [mk.sb(f"vb{i}", [128, H8, 128], F32) for i in range(2)]
                kbgq = [mk.sb(f"kbg{i}", [128, H8, 128], F32) for i in range(2)]
                kdecq = [mk.sb(f"kdec{i}", [128, H8, 128], F32) for i in range(2)]
                nwT = mk.sb("nwT", [128, H8, 128], F32)
                vn = mk.sb("vn", [128, H8, 128], F32)
                osb = mk.sb("osb", [128, H8, 128], F32)
                sqo = mk.sb("sqo", [128, H8, 128], F32)
                mixdn = [mk.sb(f"mixdn{i}", [128, 1024], BF16) for i in range(2)]
                PA_ = mk.ps("PA_", [128, 1024], F32)
                PB_ = mk.ps("PB_", [128, 1024], F32)
                PC_ = mk.ps("PC_", [128, 1024], F32)
                PD_ = mk.ps("PD_", [128, 1024], F32)
                V(lambda e: e.tensor_single_scalar(out=Uincl[:], in_=dif[:], scalar=0.0, op=ALU.is_ge), ["dif"], ["Uincl"])
                V(lambda e: e.tensor_single_scalar(out=offd[:], in_=dif[:], scalar=0.0, op=ALU.not_equal), ["dif"], ["offd"])
                for a in range(4):
                    V(lambda e, a=a: e.tensor_single_scalar(out=M2rep[:, a, :], in_=dif[:], scalar=0.0, op=ALU.is_gt), ["dif"], [f"M2rep{a}"])
                    V(lambda e, a=a: e.tensor_single_scalar(out=M2rep[:, a, :], in_=M2rep[:, a, :], scalar=1.0e4, op=ALU.mult), [f"M2rep{a}"], [f"M2rep{a}"])
                M2k = [f"M2rep{a}" for a in range(4)]
                DM("sp", lambda e: e.dma_start(out=gdn[:], in_=dng_d.ap().partition_broadcast(128)), w=["gdn"])
                G(lambda e: e.memset(Sst[:], 0.0), w=["Sst"])
                F = H8 * 128

                def bc_col(t, Ft, col0):
                    return AP(t, Ft, col0, [[1, H8], [0, 128]])

                def bc_mat(t):
                    return AP(t, 128, 0, [[0, H8], [1, 128]])

                def flat(t):
                    return t[:].rearrange("p h c -> p (h c)")

                def ph(ps, h):
                    return ps[:, h * 128:(h + 1) * 128]

                def ps3(ps):
                    return ps[:].rearrange("p (h c) -> p h c", h=H8)

                def pk(name):
                    return [name + "0", name + "1"]

                def solve(c):
                    b = c % 2
                    qTc, kTc, ktok, vtok, zc = ld[b]
                    sm = smq[b]
                    gsm = gsmq[b]
                    At = Atq[b]
                    kbg = kbgq[b]
                    vb = vbq[b]
                    kdec = kdecq[b]
                    Xq = [Xq0[b], Xq1]
                    SM = f"sm{b}_"
                    sl = slice(c * 128, (c + 1) * 128)
                    DM("sp", lambda e: e.dma_start(out=qTc[:], in_=qT_d.ap().rearrange("h d s -> d h s")[:, :, sl]), [], [f"qTc{b}"])
                    DM("sp", lambda e: e.dma_start(out=kTc[:], in_=kT_d.ap().rearrange("h d s -> d h s")[:, :, sl]), [], [f"kTc{b}"])
                    DM("sp", lambda e: e.dma_start(out=ktok[:], in_=k_d.ap()[sl]), [], [f"ktok{b}"])
                    DM("sp", lambda e: e.dma_start(out=vtok[:], in_=v_d.ap()[sl]), [], [f"vtok{b}"])
                    DM("sp", lambda e: e.dma_start(out=flat(zc), in_=z_d.ap()[sl, :]), [], [f"zc{b}"])

                    def mm1(e):
                        e.matmul(PC_[:, 0:8], lhsT=Uincl[:], rhs=bg_sb[:, c, 8:16], start=True, stop=True)
                        return e.matmul(PC_[:, 8:16], lhsT=onesf[:], rhs=bg_sb[:, c, 8:16], start=True, stop=True)
                    P(mm1, ["Uincl", "onesf", "bg_g"], ["PC_0"])
                    V(lambda e: e.tensor_copy(out=gsm[:], in_=PC_[:, 0:16]), [], ["PC_0", f"gsm{b}"])
                    A(lambda e: e.activation(out=sm[:, 0, :], in_=gsm[:, 0:8], func=AF.Exp), [f"gsm{b}"], [SM + "0"])
                    A(lambda e: e.activation(out=sm[:, 1, :], in_=gsm[:, 8:16], func=AF.Exp), [f"gsm{b}"], [SM + "1"])
                    V(lambda e: e.tensor_tensor(out=sm[:, 2, :], in0=gsm[:, 8:16], in1=gsm[:, 0:8], op=ALU.subtract), [f"gsm{b}"], [SM + "2"])
                    A(lambda e: e.activation(out=sm[:, 2, :], in_=sm[:, 2, :], func=AF.Exp), [SM + "2"], [SM + "2"])
                    V(lambda e: e.tensor_tensor(out=sm[:, 3, :], in0=bg_sb[:, c, 0:8], in1=sm[:, 0, :], op=ALU.mult), ["bg_b", SM + "0"], [SM + "3"])
                    V(lambda e: e.tensor_single_scalar(out=sm[:, 4, :], in_=bg_sb[:, c, 0:8], scalar=-1.0, op=ALU.mult), ["bg_b"], [SM + "4"])
                    V(lambda e: e.tensor_tensor(out=rhsD[:], in0=bc_mat(identf), in1=bc_col(gsm, 16, 0), op=ALU.mult), ["identf", f"gsm{b}"], ["rhsD"])
                    yield
                    for hf in range(2):
                        def mm3(e, hf=hf):
                            e.matmul(PA_[:, hf * 512:(hf + 1) * 512], lhsT=onesf[:], rhs=flat(rhsD)[:, hf * 512:(hf + 1) * 512], start=True, stop=False)
                            return e.matmul(PA_[:, hf * 512:(hf + 1) * 512], lhsT=identf[:], rhs=M2rep[:].rearrange("p a c -> p (a c)"), start=False, stop=True)
                        P(mm3, ["onesf", "identf", "rhsD"] + M2k, [f"PA_{hf}"])
                    for h in range(H8):
                        A(lambda e, h=h: e.activation(out=E2[:, h, :], in_=ph(PA_, h), func=AF.Exp, bias=gsm[:, h:h + 1], scale=-1.0), [f"gsm{b}"], [f"PA_{h // 4}", f"E2_{h}"])
                    E2k = [f"E2_{h}" for h in range(H8)]
                    for hf in range(2):
                        def mm4(e, hf=hf):
                            for h in range(hf * 4, hf * 4 + 4):
                                r = e.matmul(ph(PB_, h), lhsT=qTc[:, h, :], rhs=kTc[:, h, :], start=True, stop=True)
                            return r
                        P(mm4, [f"qTc{b}", f"kTc{b}"], [f"PB_{hf}"])

                        def mm5(e, hf=hf):
                            for h in range(hf * 4, hf * 4 + 4):
                                r = e.matmul(ph(PC_, h), lhsT=kTc[:, h, :], rhs=kTc[:, h, :], start=True, stop=True)
                            return r
                        P(mm5, [f"kTc{b}"], [f"PC_{hf}"])
                    yield
                    V(lambda e: e.tensor_tensor(out=Amat[:], in0=ps3(PB_), in1=E2[:], op=ALU.mult), E2k, pk("PB_") + ["Amat"])
                    V(lambda e: e.tensor_tensor(out=Pq[0][:], in0=ps3(PC_), in1=bc_mat(offd), op=ALU.mult), ["offd"], pk("PC_") + ["Pq0"])
                    V(lambda e: e.tensor_tensor(out=Pq[0][:], in0=Pq[0][:], in1=E2[:], op=ALU.mult), E2k + ["Pq0"], ["Pq0"])
                    V(lambda e: e.tensor_tensor(out=Pq[0][:], in0=Pq[0][:], in1=AP(sm, 48, 32, [[1, H8], [0, 128]]), op=ALU.mult), [SM + "4", "Pq0"], ["Pq0"])
                    for hf in range(2):
                        def tr1(e, hf=hf):
                            for h in range(hf * 4, hf * 4 + 4):
                                r = e.transpose(ph(PA_, h), Amat[:, h, :], identf[:])
                            return r
                        P(tr1, ["Amat", "identf"], [f"PA_{hf}"])

                        def tr2(e, hf=hf):
                            for h in range(hf * 4, hf * 4 + 4):
                                r = e.transpose(ph(PB_, h), Pq[0][:, h, :], identf[:])
                            return r
                        P(tr2, ["Pq0", "identf"], [f"PB_{hf}"])
                    yield
                    A(lambda e: e.copy(out=flat(At), in_=PA_[:]), [], pk("PA_") + [f"At{b}"])
                    V(lambda e: e.tensor_copy(out=flat(Ptq[0]), in_=PB_[:]), [], pk("PB_") + ["Ptq0"])
                    V(lambda e: e.tensor_tensor(out=Xq[0][:], in0=Ptq[0][:], in1=bc_mat(identf), op=ALU.add), ["Ptq0", "identf"], [f"Xq0{b}"])
                    xkey = [f"Xq0{b}", "Xq1"]
                    G(lambda e: e.tensor_tensor(out=vb[:], in0=vtok[:], in1=AP(bg_sb, NT * 16, c * 16, [[1, H8], [0, 128]]), op=ALU.mult), [f"vtok{b}", "bg_b"], [f"vb{b}"])
                    G(lambda e: e.tensor_tensor(out=kbg[:], in0=ktok[:], in1=AP(sm, 48, 24, [[1, H8], [0, 128]]), op=ALU.mult), [f"ktok{b}", SM + "3"], [f"kbg{b}"])
                    G(lambda e: e.tensor_tensor(out=kdec[:], in0=ktok[:], in1=AP(sm, 48, 16, [[1, H8], [0, 128]]), op=ALU.mult), [f"ktok{b}", SM + "2"], [f"kdec{b}"])
                    cur = 0
                    for kk in range(1, 7):
                        nx = 1 - cur
                        for hf in range(2):
                            def d1(e, hf=hf, cur=cur):
                                for h in range(hf * 4, hf * 4 + 4):
                                    r = e.matmul(ph(PA_, h), lhsT=Ptq[cur][:, h, :], rhs=Pq[cur][:, h, :], start=True, stop=True)
                                return r
                            P(d1, [f"Ptq{cur}", f"Pq{cur}"], [f"PA_{hf}"])
                        if kk < 6:
                            for hf in range(2):
                                def d2(e, hf=hf, cur=cur):
                                    for h in range(hf * 4, hf * 4 + 4):
                                        r = e.matmul(ph(PB_, h), lhsT=Pq[cur][:, h, :], rhs=Ptq[cur][:, h, :], start=True, stop=True)
                                    return r
                                P(d2, [f"Ptq{cur}", f"Pq{cur}"], [f"PB_{hf}"])
                        A(lambda e, nx=nx: e.copy(out=flat(Pq[nx]), in_=PA_[:]), [], pk("PA_") + [f"Pq{nx}"])
                        if kk < 6:
                            V(lambda e, nx=nx: e.tensor_copy(out=flat(Ptq[nx]), in_=PB_[:]), [], pk("PB_") + [f"Ptq{nx}"])
                        yield
                        for hf in range(2):
                            def d3(e, hf=hf, cur=cur, nx=nx):
                                for h in range(hf * 4, hf * 4 + 4):
                                    r = e.matmul(ph(PC_, h), lhsT=Pq[nx][:, h, :], rhs=Xq[cur][:, h, :], start=True, stop=True)
                                return r
                            P(d3, [f"Pq{nx}", xkey[cur]], [f"PC_{hf}"])
                        V(lambda e, cur=cur, nx=nx: e.tensor_tensor(out=flat(Xq[nx]), in0=PC_[:], in1=flat(Xq[cur]), op=ALU.add), [xkey[cur]], pk("PC_") + [xkey[nx]])
                        cur = nx
                        yield
                    assert cur == 0

                def apply(c):
                    b = c % 2
                    qTc, kTc, ktok, vtok, zc = ld[b]
                    sm = smq[b]
                    At = Atq[b]
                    kbg = kbgq[b]
                    vb = vbq[b]
                    kdec = kdecq[b]
                    X = Xq0[b]
                    Xk = f"Xq0{b}"
                    SM = f"sm{b}_"
                    sl = slice(c * 128, (c + 1) * 128)
                    for hf in range(2):
                        def w1(e, hf=hf):
                            for h in range(hf * 4, hf * 4 + 4):
                                r = e.matmul(ph(PD_, h), lhsT=kbg[:, h, :], rhs=X[:, h, :], start=True, stop=True)
                            return r
                        P(w1, [f"kbg{b}", Xk], [f"PD_{hf}"])
                    A(lambda e: e.mul(out=flat(nwT), in_=PD_[:], mul=-1.0), [], pk("PD_") + ["nwT"])
                    yield
                    for hf in range(2):
                        def w2(e, hf=hf):
                            for h in range(hf * 4, hf * 4 + 4):
                                e.matmul(ph(PD_, h), lhsT=X[:, h, :], rhs=vb[:, h, :], start=True, stop=False)
                                r = e.matmul(ph(PD_, h), lhsT=nwT[:, h, :], rhs=Sst[:, h, :], start=False, stop=True)
                            return r
                        P(w2, [f"vb{b}", Xk, "nwT", "Sst"], [f"PD_{hf}"])
                    V(lambda e: e.tensor_copy(out=flat(vn), in_=PD_[:]), [], pk("PD_") + ["vn"])
                    yield
                    for hf in range(2):
                        def o1(e, hf=hf):
                            for h in range(hf * 4, hf * 4 + 4):
                                r = e.matmul(ph(PD_, h), lhsT=qTc[:, h, :], rhs=Sst[:, h, :], start=True, stop=True)
                            return r
                        P(o1, [f"qTc{b}", "Sst"], [f"PD_{hf}"])
                    V(lambda e: e.tensor_tensor(out=osb[:], in0=ps3(PD_), in1=AP(sm, 48, 0, [[1, H8], [0, 128]]), op=ALU.mult), [SM + "0"], pk("PD_") + ["osb"])
                    yield
                    for hf in range(2):
                        def o2(e, hf=hf):
                            for h in range(hf * 4, hf * 4 + 4):
                                r = e.matmul(ph(PD_, h), lhsT=At[:, h, :], rhs=vn[:, h, :], start=True, stop=True)
                            return r
                        P(o2, [f"At{b}", "vn"], [f"PD_{hf}"])
                    V(lambda e: e.tensor_tensor(out=flat(osb), in0=PD_[:], in1=flat(osb), op=ALU.add), ["osb"], pk("PD_") + ["osb"])
                    yield
                    for hf in range(2):
                        def s1(e, hf=hf):
                            for h in range(hf * 4, hf * 4 + 4):
                                r = e.matmul(ph(PD_, h), lhsT=kdec[:, h, :], rhs=vn[:, h, :], start=True, stop=True)
                            return r
                        P(s1, [f"kdec{b}", "vn"], [f"PD_{hf}"])
                    V(lambda e: e.tensor_tensor(out=Sst[:], in0=Sst[:], in1=AP(sm, 48, 8, [[1, H8], [0, 128]]), op=ALU.mult), ["Sst", SM + "1"], ["Sst"])
                    V(lambda e: e.tensor_tensor(out=flat(Sst), in0=PD_[:], in1=flat(Sst), op=ALU.add), ["Sst"], pk("PD_") + ["Sst"])
                    yield
                    G(lambda e: e.tensor_tensor(out=sqo[:], in0=osb[:], in1=osb[:], op=ALU.mult), ["osb"], ["sqo"])
                    V(lambda e: e.reduce_sum(out=sm[:, 5, :], in_=sqo[:], axis=AX.X), ["sqo"], [SM + "5"])
                    A(lambda e: e.activation(out=sm[:, 5, :], in_=sm[:, 5, :], func=AF.Sqrt, bias=EPS, scale=1.0 / 128), [SM + "5"], [SM + "5"])
                    V(lambda e: e.reciprocal(out=sm[:, 5, :], in_=sm[:, 5, :]), [SM + "5"], [SM + "5"])
                    G(lambda e: e.tensor_tensor(out=sqo[:], in0=osb[:], in1=AP(sm, 48, 40, [[1, H8], [0, 128]]), op=ALU.mult), ["osb", SM + "5", "sqo"], ["sqo"])
                    G(lambda e: e.tensor_tensor(out=sqo[:], in0=sqo[:], in1=bc_mat(gdn), op=ALU.mult), ["sqo", "gdn"], ["sqo"])
                    G(lambda e: e.tensor_tensor(out=mixdn[b][:], in0=flat(sqo), in1=flat(zc), op=ALU.mult), ["sqo", f"zc{b}"], [f"mixdn{b}"])
                    DM("sp", lambda e: e.dma_start(out=mix_d.ap()[sl, 0:1024], in_=mixdn[b][:]), [f"mixdn{b}"], [f"mix_dn{c}"])
                    yield

                def interleave(gens):
                    gens = [g for g in gens if g is not None]
                    while gens:
                        for g in list(gens):
                            try:
                                next(g)
                            except StopIteration:
                                gens.remove(g)

                interleave([solve(0)])
                for c in range(NT):
                    interleave([solve(c + 1) if c + 1 < NT else None, apply(c)])
                if dbg and upto == 4:
                    mk.barrier()
                    dd = dbgout("mix", [S, D], BF16)
                    DM("sp", lambda e: e.dma_start(out=dd.ap(), in_=mix_d.ap()), [], ["dbgmix"])
                mk.flush()

        if upto >= 5:
            p57 = ExitStack()
            es.enter_context(p57)
            mk.pes = p57
            logits = mk.sb("logits", [128, NT, 64], F32)
            dest_i = mk.sb("dest_i", [128, NT, 8], I32)
            BEi4 = mk.sb("BEi4", [128, NBLK, 4], I32)
            BEi2 = mk.sb("BEi2", [128, NBLK, 2], I32)
            with ExitStack() as pes:
                mk.pes = pes
                wout = mk.sb("wout", [128, 16, D], BF16)
                wr = mk.sb("wr", [128, 16, 64], BF16)
                g1b = mk.sb("g1b", [128, D], F32)
                G2b = mk.sb("G2b", [128, D], F32)
                S2b = mk.sb("S2b", [128, D], F32)
                xt = [mk.sb(f"xt5{i}", [128, D], F32) for i in range(2)]
                mixt = [mk.sb(f"mixt{i}", [128, D], BF16) for i in range(2)]
                mixT = [mk.sb(f"mixT{i}", [128, 16, 128], BF16) for i in range(2)]
                h2T = mk.sb("h2T", [128, 16, 128], BF16)
                tmpf = mk.sb("tmpf5", [128, D], F32)
                tmp2 = mk.sb("tmp25", [128, D], F32)
                x1t = mk.sb("x1t", [128, D], F32)
                h2t = [mk.sb(f"h2t{i}", [128, D], BF16) for i in range(2)]
                junk = mk.sb("junk5", [128, D], BF16)
                st = mk.sb("st5", [128, NT, 2], F32)
                PT = [mk.ps(f"PT5{i}", [128, 1024], BF16) for i in range(2)]
                PO = mk.ps("PO5", [128, D], F32)
                PR = mk.ps("PR5", [128, 64], F32)
                PT2 = [mk.ps("PT52", [128, 1024], BF16), None]
                PT2[1] = PT2[0]
                for n in range(4):
                    DM("pool", lambda e, n=n: e.dma_start(out=wout[:, :, n * 512:(n + 1) * 512], in_=wout_d.ap().rearrange("(k p) n -> p k n", p=128)[:, :, n * 512:(n + 1) * 512]), w=[f"wout{n}"])
                DM("pool", lambda e: e.dma_start(out=wr[:], in_=wr_d.ap().rearrange("(k p) n -> p k n", p=128)), w=["wr"])
                DM("sp", lambda e: e.dma_start(out=g1b[:], in_=ada_d.ap()[2:3, :].partition_broadcast(128)), w=["g1b"])
                DM("sp", lambda e: e.dma_start(out=S2b[:], in_=ada_d.ap()[3:4, :].partition_broadcast(128)), w=["S2b"])
                DM("sp", lambda e: e.dma_start(out=G2b[:], in_=ada_d.ap()[4:5, :].partition_broadcast(128)), w=["G2b"])
                G(lambda e: e.memset(junk[:], 0.0), w=["junk5"])
                DM("sp", lambda e: e.dma_start(out=h2_d.ap()[S:S + 128, :], in_=junk[:]), ["junk5"], ["h2pad"])
                woutk = [f"wout{n}" for n in range(4)]

                def front(t):
                    b = t % 2
                    sl = slice(t * 128, (t + 1) * 128)
                    DM("sp", lambda e: e.dma_start(out=mixt[b][:], in_=mix_d.ap()[sl, :]), w=[f"mixt{b}"])
                    DM("sp", lambda e: e.dma_start(out=xt[b][:], in_=x_d.ap()[sl, :]), w=[f"xt5{b}"])
                    for hh in range(2):
                        def tr(e, hh=hh):
                            for k in range(8):
                                kk = hh * 8 + k
                                r = e.transpose(PT[hh][:, k * 128:(k + 1) * 128], mixt[b][:, kk * 128:(kk + 1) * 128], identb[:])
                            return r
                        P(tr, [f"mixt{b}", "identb"], [f"PT5{hh}"])
                        if hh == 0:
                            V(lambda e, hh=hh: e.tensor_copy(out=mixT[b][:, hh * 8:(hh + 1) * 8, :], in_=PT[hh][:].rearrange("p (k c) -> p k c", k=8)), [], [f"PT5{hh}", f"mixT{b}_{hh}"])
                        else:
                            A(lambda e, hh=hh: e.copy(out=mixT[b][:, hh * 8:(hh + 1) * 8, :], in_=PT[hh][:].rearrange("p (k c) -> p k c", k=8)), [], [f"PT5{hh}", f"mixT{b}_{hh}"])
                    yield
                    for n in range(4):
                        def mm(e, n=n):
                            for k in range(16):
                                r = e.matmul(PO[:, n * 512:(n + 1) * 512], lhsT=mixT[b][:, k, :], rhs=wout[:, k, n * 512:(n + 1) * 512], start=(k == 0), stop=(k == 15))
                            return r
                        P(mm, [f"mixT{b}_0", f"mixT{b}_1"] + woutk, [f"PO{n}"])
                        V(lambda e, n=n: e.tensor_tensor(out=tmpf[:, n * 512:(n + 1) * 512], in0=PO[:, n * 512:(n + 1) * 512], in1=g1b[:, n * 512:(n + 1) * 512], op=ALU.mult), ["g1b"], [f"PO{n}", f"tmpf5_{n}"])
                        yield

                def back(t):
                    b = t % 2
                    sl = slice(t * 128, (t + 1) * 128)
                    tk = [f"tmpf5_{n}" for n in range(4)]
                    V(lambda e: e.tensor_tensor(out=x1t[:], in0=tmpf[:], in1=xt[b][:], op=ALU.add), tk + [f"xt5{b}"], ["x1t"])
                    DM("sp", lambda e: e.dma_start(out=x1_d.ap()[sl, :], in_=x1t[:]), ["x1t"], [f"x1_d{t}"])
                    A(lambda e: e.activation(out=junk[:], in_=x1t[:], func=AF.Square, accum_out=st[:, t, 0:1]), ["x1t"], ["junk5", f"st5{t}"])
                    A(lambda e: e.activation(out=st[:, t, 1:2], in_=st[:, t, 0:1], func=AF.Sqrt, bias=EPS, scale=1.0 / D), [f"st5{t}"], [f"st5{t}"])
                    V(lambda e: e.reciprocal(out=st[:, t, 1:2], in_=st[:, t, 1:2]), [f"st5{t}"], [f"st5{t}"])
                    yield
                    V(lambda e: e.scalar_tensor_tensor(out=tmp2[:], in0=x1t[:], scalar=st[:, t, 1:2], in1=G2b[:], op0=ALU.mult, op1=ALU.mult), ["x1t", f"st5{t}", "G2b"], ["tmp2"])
                    V(lambda e: e.tensor_tensor(out=h2t[b][:], in0=tmp2[:], in1=S2b[:], op=ALU.add), ["tmp2", "S2b"], [f"h2t{b}"])
                    DM("sp", lambda e: e.dma_start(out=h2_d.ap()[sl, :], in_=h2t[b][:]), [f"h2t{b}"], [f"h2_d{t}"])
                    yield
                    for hh in range(2):
                        def tr2(e, hh=hh):
                            for k in range(8):
                                kk = hh * 8 + k
                                r = e.transpose(PT2[hh][:, k * 128:(k + 1) * 128], h2t[b][:, kk * 128:(kk + 1) * 128], identb[:])
                            return r
                        P(tr2, [f"h2t{b}", "identb"], ["PT52"])
                        if hh == 0:
                            V(lambda e, hh=hh: e.tensor_copy(out=h2T[:, hh * 8:(hh + 1) * 8, :], in_=PT2[hh][:].rearrange("p (k c) -> p k c", k=8)), [], ["PT52", f"h2T{hh}"])
                        else:
                            A(lambda e, hh=hh: e.copy(out=h2T[:, hh * 8:(hh + 1) * 8, :], in_=PT2[hh][:].rearrange("p (k c) -> p k c", k=8)), [], ["PT52", f"h2T{hh}"])
                    yield

                    def mmr(e):
                        for k in range(16):
                            r = e.matmul(PR[:], lhsT=h2T[:, k, :], rhs=wr[:, k, :], start=(k == 0), stop=(k == 15))
                        return r
                    P(mmr, ["h2T0", "h2T1", "wr"], ["PR5"])
                    V(lambda e: e.tensor_copy(out=logits[:, t, :], in_=PR[:]), [], ["PR5", f"logits{t}"])
                    yield

                def interleave5(gens):
                    gens = [g for g in gens if g is not None]
                    while gens:
                        for g in list(gens):
                            try:
                                next(g)
                            except StopIteration:
                                gens.remove(g)

                interleave5([front(0)])
                for t in range(NT):
                    interleave5([front(t + 1) if t + 1 < NT else None, back(t)])
                mk.flush()
            with ExitStack() as pes:
                mk.pes = pes
                NG = NT * 8
                scores = mk.sb("scores", [128, NT, 64], F32)
                biased = mk.sb("biased", [128, NT, 64], F32)
                tmpA = mk.sb("tmpA", [128, NT, 64], F32)
                mb = mk.sb("mb", [128, NT, 64], F32)
                sel = mk.sb("sel", [128, NT, 64], F32)
                wn = mk.sb("wn", [128, NT, 64], F32)
                pos = mk.sb("pos", [128, NT, 64], F32)
                rb = mk.sb("rb", [128, 64], F32)
                m1 = mk.sb("m1", [128, NG], F32)
                m2 = mk.sb("m2", [128, NG], F32)
                t8 = mk.sb("t8", [128, NT, 8], F32)
                v8 = mk.sb("v8", [128, NT, 8], F32)
                den = mk.sb("den", [128, NT], F32)
                selsum = mk.sb("selsum", [128, 64], F32)
                Ustr = mk.sb("Ustr", [128, 128], F32)
                cA = mk.sb("cA", [128, 64], F32)
                cB = mk.sb("cB", [128, 64], F32)
                nbf = mk.sb("nbf", [128, 64], F32)
                nbi = mk.sb("nbi", [128, 64], I32)
                rowbase = mk.sb("rowbase", [128, 64], F32)
                d6 = mk.sb("d6", [128, NT, 8], F32)
                meta = mk.sb("meta", [128, NT, 8, 2], F32)
                junk64 = mk.sb("junk64", [128, 64], F32)
                tok_i = mk.sb("tok_i", [128, NT], I32)
                tokf = mk.sb("tokf", [128, NT], F32)
                bidx_i = mk.sb("bidx_i", [128, NBLK], I32)
                bidxf = mk.sb("bidxf", [128, NBLK], F32)
                cmp = mk.sb("cmp", [128, NBLK, 64], F32)
                BEf = mk.sb("BEf", [128, NBLK], F32)
                BE4f = mk.sb("BE4f", [128, NBLK, 4], F32)
                rminit = mk.sb("rminit", [128, NROWS // 128, 2], F32)
                Ppos = mk.ps("Ppos", [128, 64], F32)
                DM("sp", lambda e: e.dma_start(out=rb[:], in_=rb_d.ap().partition_broadcast(128)), w=["rb"])
                G(lambda e: e.memset(rminit[:], 0.0), w=["rminit"])
                G(lambda e: e.memset(rminit[:, :, 0:1], float(S)), ["rminit"], ["rminit"])
                DM("sp", lambda e: e.dma_start(out=rm_d.ap().rearrange("(p a) c -> p a c", p=128), in_=rminit[:]), ["rminit"], ["rm_init"])
                G(lambda e: e.memset(d6[:], 0.0), w=["d6"])
                G(lambda e: e.memset(meta[:], 0.0), w=["meta"])
                G(lambda e: e.iota(tok_i[:], [[128, NT]], base=0, channel_multiplier=1), w=["tok_i"])
                G(lambda e: e.iota(bidx_i[:], [[1, NBLK]], base=0, channel_multiplier=0), w=["bidx_i"])
                V(lambda e: e.tensor_copy(out=tokf[:], in_=tok_i[:]), ["tok_i"], ["tokf"])
                V(lambda e: e.tensor_copy(out=bidxf[:], in_=bidx_i[:]), ["bidx_i"], ["bidxf"])
                V(lambda e: e.tensor_single_scalar(out=Ustr[:], in_=dif[:], scalar=0.0, op=ALU.is_gt), ["dif"], ["Ustr"])
                lk = [f"logits{t}" for t in range(NT)]
                fl = lambda t_: t_[:].rearrange("p t e -> p (t e)")
                g3 = lambda t_: t_[:].rearrange("p t (g e) -> p (t g) e", e=8)
                A(lambda e: e.activation(out=fl(scores), in_=fl(logits), func=AF.Sigmoid), lk, ["scores"])
                V(lambda e: e.tensor_tensor(out=biased[:], in0=scores[:], in1=AP(rb, 64, 0, [[0, NT], [1, 64]]), op=ALU.add), ["scores", "rb"], ["biased"])
                V(lambda e: e.reduce_max(out=m1[:], in_=g3(biased), axis=AX.X), ["biased"], ["m1"])
                V(lambda e: e.tensor_tensor(out=g3(tmpA), in0=g3(biased), in1=AP(m1, NG, 0, [[1, NG], [0, 8]]), op=ALU.is_equal), ["biased", "m1"], ["tmpA"])
                V(lambda e: e.scalar_tensor_tensor(out=fl(tmpA), in0=fl(tmpA), scalar=-1.0e9, in1=fl(biased), op0=ALU.mult, op1=ALU.add), ["tmpA", "biased"], ["tmpA"])
                V(lambda e: e.reduce_max(out=m2[:], in_=g3(tmpA), axis=AX.X), ["tmpA"], ["m2"])
                V(lambda e: e.tensor_tensor(out=m1[:], in0=m1[:], in1=m2[:], op=ALU.add), ["m1", "m2"], ["m1"])
                for t in range(NT):
                    V(lambda e, t=t: e.max(out=t8[:, t, :], in_=m1[:, t * 8:(t + 1) * 8]), ["m1"], [f"t8_{t}"])
                t8k = [f"t8_{t}" for t in range(NT)]
                V(lambda e: e.tensor_tensor(out=m2[:].rearrange("p (t g) -> p t g", g=8), in0=m1[:].rearrange("p (t g) -> p t g", g=8), in1=AP(t8, NT * 8, 3, [[8, NT], [0, 8]]), op=ALU.is_ge), ["m1", "m2"] + t8k, ["m2"])
                V(lambda e: e.tensor_scalar(out=m2[:], in0=m2[:], scalar1=1.0e9, scalar2=-1.0e9, op0=ALU.mult, op1=ALU.add), ["m2"], ["m2"])
                V(lambda e: e.tensor_tensor(out=g3(mb), in0=g3(biased), in1=AP(m2, NG, 0, [[1, NG], [0, 8]]), op=ALU.add), ["biased", "m2"], ["mb"])
                for t in range(NT):
                    V(lambda e, t=t: e.max(out=v8[:, t, :], in_=mb[:, t, :]), ["mb"], [f"v8_{t}"])
                v8k = [f"v8_{t}" for t in range(NT)]
                V(lambda e: e.tensor_tensor(out=sel[:], in0=mb[:], in1=AP(v8, NT * 8, 5, [[8, NT], [0, 64]]), op=ALU.is_ge), ["mb"] + v8k, ["sel"])
                V(lambda e: e.tensor_tensor(out=wn[:], in0=sel[:], in1=scores[:], op=ALU.mult), ["sel", "scores"], ["wn"])
                V(lambda e: e.reduce_sum(out=den[:], in_=wn[:], axis=AX.X), ["wn"], ["den"])
                V(lambda e: e.reciprocal(out=den[:], in_=den[:]), ["den"], ["den"])
                V(lambda e: e.tensor_single_scalar(out=den[:], in_=den[:], scalar=2.5, op=ALU.mult), ["den"], ["den"])
                V(lambda e: e.tensor_tensor(out=wn[:], in0=wn[:], in1=AP(den, NT, 0, [[1, NT], [0, 64]]), op=ALU.mult), ["wn", "den"], ["wn"])
                for t in range(NT):
                    def mp(e, t=t):
                        r = e.matmul(Ppos[:], lhsT=Ustr[:], rhs=sel[:, t, :], start=True, stop=(t == 0))
                        if t > 0:
                            r = e.matmul(Ppos[:], lhsT=onesf[:], rhs=selsum[:], start=False, stop=True)
                        return r
                    P(mp, ["Ustr", "onesf", "sel", "selsum"], ["Ppos"])
                    V(lambda e, t=t: e.tensor_copy(out=pos[:, t, :], in_=Ppos[:]), [], ["Ppos", f"pos{t}"])
                    if t == 0:
                        V(lambda e: e.tensor_copy(out=selsum[:], in_=sel[:, 0, :]), ["sel"], ["selsum"])
                    else:
                        V(lambda e, t=t: e.tensor_tensor(out=selsum[:], in0=selsum[:], in1=sel[:, t, :], op=ALU.add), ["sel", "selsum"], ["selsum"])
                P(lambda e: e.matmul(Ppos[:], lhsT=onesf[:], rhs=selsum[:], start=True, stop=True), ["onesf", "selsum"], ["Ppos"])
                V(lambda e: e.tensor_scalar(out=nbf[:], in0=Ppos[:], scalar1=float(BS - 1), scalar2=1.0 / BS, op0=ALU.add, op1=ALU.mult), [], ["Ppos", "nbf"])
                V(lambda e: e.tensor_single_scalar(out=nbf[:], in_=nbf[:], scalar=-0.5 + 1.0 / (2 * BS), op=ALU.add), ["nbf"], ["nbf"])
                V(lambda e: e.tensor_copy(out=nbi[:], in_=nbf[:]), ["nbf"], ["nbi"])
                V(lambda e: e.tensor_copy(out=nbf[:], in_=nbi[:]), ["nbi"], ["nbf"])
                V(lambda e: e.tensor_copy(out=cA[:], in_=nbf[:]), ["nbf"], ["cA"])
                ca, cb, can, cbn = cA, cB, "cA", "cB"
                for sft in (1, 2, 4, 8, 16, 32):
                    V(lambda e, ca=ca, cb=cb, sft=sft: e.tensor_copy(out=cb[:, 0:sft], in_=ca[:, 0:sft]), [can], [cbn])
                    V(lambda e, ca=ca, cb=cb, sft=sft: e.tensor_tensor(out=cb[:, sft:64], in0=ca[:, sft:64], in1=ca[:, 0:64 - sft], op=ALU.add), [can, cbn], [cbn])
                    ca, cb, can, cbn = cb, ca, cbn, can
                bsincl, bsk = ca, can
                V(lambda e: e.tensor_tensor(out=rowbase[:], in0=bsincl[:], in1=nbf[:], op=ALU.subtract), [bsk, "nbf"], ["rowbase"])
                V(lambda e: e.tensor_single_scalar(out=rowbase[:], in_=rowbase[:], scalar=float(BS), op=ALU.mult), ["rowbase"], ["rowbase"])
                posk = [f"pos{t}" for t in range(NT)]
                V(lambda e: e.tensor_tensor(out=pos[:], in0=pos[:], in1=AP(rowbase, 64, 0, [[0, NT], [1, 64]]), op=ALU.add), posk + ["rowbase"], ["posall"])
                for t in range(NT):
                    for k in range(6):
                        V(lambda e, t=t, k=k: e.scalar_tensor_tensor(out=junk64[:], in0=mb[:, t, :], scalar=v8[:, t, k:k + 1], in1=pos[:, t, :], op0=ALU.is_equal, op1=ALU.mult, accum_out=d6[:, t, k:k + 1]),
                          ["mb", "posall", "d6"] + v8k, ["junk64", f"d6_{t}_{k}"])
                        V(lambda e, t=t, k=k: e.scalar_tensor_tensor(out=junk64[:], in0=mb[:, t, :], scalar=v8[:, t, k:k + 1], in1=wn[:, t, :], op0=ALU.is_equal, op1=ALU.mult, accum_out=meta[:, t, k, 1:2]),
                          ["mb", "wn", "meta"] + v8k, ["junk64", f"meta_{t}_{k}"])
                d6k = [f"d6_{t}_{k}" for t in range(NT) for k in range(6)]
                mtk = [f"meta_{t}_{k}" for t in range(NT) for k in range(6)]
                V(lambda e: e.tensor_copy(out=dest_i[:], in_=d6[:]), d6k + ["d6"], ["dest_i"])
                V(lambda e: e.tensor_copy(out=meta[:, :, :, 0], in_=AP(tokf, NT, 0, [[1, NT], [0, 8]])), ["tokf", "meta"] + mtk, ["meta_tok"])
                for t in range(NT):
                    for k in range(6):
                        DM("pool", lambda e, t=t, k=k: e.indirect_dma_start(out=rm_d.ap(), out_offset=bass.IndirectOffsetOnAxis(ap=dest_i[:, t, k:k + 1], axis=0), in_=meta[:, t, k, :], in_offset=None),
                           ["dest_i", "meta_tok", "rm_init"] + mtk, [f"rm_{t}_{k}"])
                V(lambda e: e.tensor_tensor(out=cmp[:], in0=AP(bidxf, NBLK, 0, [[1, NBLK], [0, 64]]), in1=AP(bsincl, 64, 0, [[0, NBLK], [1, 64]]), op=ALU.is_ge), ["bidxf", bsk], ["cmp"])
                V(lambda e: e.reduce_sum(out=BEf[:], in_=cmp[:], axis=AX.X), ["cmp"], ["BEf"])
                V(lambda e: e.tensor_single_scalar(out=bidxf[:], in_=BEf[:], scalar=64.0, op=ALU.is_ge), ["BEf", "cmp"], ["bidxf"])
                V(lambda e: e.scalar_tensor_tensor(out=BEf[:], in0=bidxf[:], scalar=1000.0, in1=BEf[:], op0=ALU.mult, op1=ALU.add), ["bidxf", "BEf"], ["BEf"])
                V(lambda e: e.tensor_scalar(out=BEf[:], in0=BEf[:], scalar1=128.0, scalar2=tokf[:, 0:1], op0=ALU.mult, op1=ALU.add), ["BEf", "tokf"], ["BEf"])
                for j in range(4):
                    V(lambda e, j=j: e.tensor_scalar(out=BE4f[:, :, j], in0=BEf[:], scalar1=4.0, scalar2=float(j), op0=ALU.mult, op1=ALU.add), ["BEf"], [f"BE4f{j}"])
                V(lambda e: e.tensor_copy(out=BEi4[:], in_=BE4f[:]), [f"BE4f{j}" for j in range(4)], ["BEi4"])
                for j in range(2):
                    V(lambda e, j=j: e.tensor_scalar(out=BE4f[:, :, j], in0=BEf[:], scalar1=2.0, scalar2=float(j), op0=ALU.mult, op1=ALU.add), ["BEf", "BEi4"], [f"BE4f{j}"])
                V(lambda e: e.tensor_copy(out=BEi2[:], in_=BE4f[:, :, 0:2]), ["BE4f0", "BE4f1"], ["BEi2"])
                if dbg and upto == 5:
                    mk.barrier()
                    for nm, src, shp, dty in (("x1", x1_d, [S, D], F32), ("h2", h2_d, [S + 128, D], BF16), ("rm", rm_d, [NROWS, 2], F32)):
                        dd = dbgout(nm, shp, dty)
                        DM("sp", lambda e, dd=dd, src=src: e.dma_start(out=dd.ap(), in_=src.ap()), [], ["dbg_" + nm])
                    for nm, src, shp, dty in (("logits", logits, [128, NT, 64], F32), ("sel", sel, [128, NT, 64], F32), ("wn", wn, [128, NT, 64], F32), ("dest", dest_i, [128, NT, 8], I32), ("BEi4", BEi4, [128, NBLK, 4], I32)):
                        dd = dbgout(nm, shp, dty)
                        DM("sp", lambda e, dd=dd, src=src: e.dma_start(out=dd.ap(), in_=src[:]), [], ["dbg_" + nm])
                mk.flush()

        if upto >= 6:
            with ExitStack() as pes:
                mk.pes = pes
                NST = 4
                NTOT = NBLK + 8
                stg = [mk.sb(f"stg{i}", [128, 4096], F32) for i in range(NST)]
                wgu = [mk.sb(f"wgu{i}", [128, 16, 1024], BF16) for i in range(2)]
                wd = [mk.sb(f"wd{i}", [128, 4, 2048], BF16) for i in range(2)]
                rmt = [mk.sb(f"rmt{i}", [128, 2, 2], F32) for i in range(2)]
                toki = [mk.sb(f"toki{i}", [128, 2], I32) for i in range(2)]
                hg = [mk.sb(f"hg{i}", [128, D], BF16) for i in range(4)]
                hgT = mk.sb("hgT", [128, 16, BS], BF16)
                gsb = [mk.sb(f"gsb{i}", [128, BS], F32) for i in range(2)]
                actT = mk.sb("actT", [128, 4, BS], BF16)
                ysb = [mk.sb(f"ysb{i}", [128, 1024], BF16) for i in range(2)]
                PT = [mk.ps(f"PT6{i}", [128, 1024], BF16) for i in range(2)]
                PG = [mk.ps(f"PG6{i}", [128, 512], F32) for i in range(2)]
                PU = [mk.ps(f"PU6{i}", [128, 512], F32) for i in range(2)]
                PY = [mk.ps(f"PY6{i}", [128, 512], F32) for i in range(2)]
                state = {"sti": 0, "yi": 0, "ys": 0}

                order = []
                for i in range(NBLK // 2):
                    order += [i, NBLK - 1 - i]
                order += list(range(NBLK, NTOT))
                SHP = NBLK % 2

                def wb_of(pos):
                    return pos % 2 if pos < NBLK else SHP

                def rows(pos):
                    blk = order[pos]
                    mb_ = pos % 2
                    if blk < NBLK:
                        DM("sp", lambda e: e.dma_start(out=rmt[mb_][:], in_=rm_d.ap()[blk * BS:(blk + 1) * BS, :].rearrange("(s p) c -> p s c", p=128)), [], [f"rmt{mb_}"])
                        V(lambda e: e.tensor_copy(out=toki[mb_][:], in_=rmt[mb_][:, :, 0]), [f"rmt{mb_}"], [f"toki{mb_}"])
                    else:
                        G(lambda e: e.memset(rmt[mb_][:], 1.0), [], [f"rmt{mb_}"])
                    for sbi in range(2):
                        hi = mb_ * 2 + sbi
                        if blk < NBLK:
                            DM("pool", lambda e, sbi=sbi, hi=hi: e.indirect_dma_start(out=hg[hi][:], out_offset=None, in_=h2_d.ap(), in_offset=bass.IndirectOffsetOnAxis(ap=toki[mb_][:, sbi:sbi + 1], axis=0)),
                               [f"toki{mb_}"], [f"hg{hi}"])
                        else:
                            r0 = (blk - NBLK) * BS + sbi * 128
                            DM("sp", lambda e, hi=hi, r0=r0: e.dma_start(out=hg[hi][:], in_=h2_d.ap()[r0:r0 + 128, :]), [], [f"hg{hi}"])

                def wdma(pos, j):
                    blk = order[pos]
                    wbuf = wb_of(pos)
                    if blk < NBLK:
                        sb_ = state["sti"] % NST
                        state["sti"] += 1
                        if j < 4:
                            DM("pool", lambda e: e.indirect_dma_start(out=stg[sb_][:], out_offset=None, in_=wegu_d.ap(), in_offset=bass.IndirectOffsetOnAxis(ap=BEi4[:, blk, j:j + 1], axis=0), bounds_check=state["bc"], oob_is_err=False), ["BEi4"], [f"stg{sb_}"])
                        else:
                            DM("pool", lambda e: e.indirect_dma_start(out=stg[sb_][:], out_offset=None, in_=wed_d.ap(), in_offset=bass.IndirectOffsetOnAxis(ap=BEi2[:, blk, j - 4:j - 3], axis=0), bounds_check=state["bc"], oob_is_err=False), ["BEi2"], [f"stg{sb_}"])
                        return sb_
                    if pos == NBLK:
                        if j < 4:
                            DM("pool", lambda e: e.dma_start(out=wgu[wbuf][:, 4 * j:4 * j + 4, :].rearrange("p k n -> p (k n)"), in_=wsgu_d.ap()[:, j * 4096:(j + 1) * 4096]), [], [f"wgu{wbuf}_{j}"])
                        else:
                            DM("pool", lambda e: e.dma_start(out=wd[wbuf][:, 2 * (j - 4):2 * (j - 4) + 2, :].rearrange("p k n -> p (k n)"), in_=wsd_d.ap()[:, (j - 4) * 4096:(j - 3) * 4096]), [], [f"wd{wbuf}_{j - 4}"])
                    return None

                def wcast(pos, j, sb_):
                    if sb_ is None:
                        return
                    wbuf = wb_of(pos)
                    if j < 4:
                        dstw = wgu[wbuf][:, 4 * j:4 * j + 4, :].rearrange("p k n -> p (k n)")
                        wkey = f"wgu{wbuf}_{j}"
                    else:
                        dstw = wd[wbuf][:, 2 * (j - 4):2 * (j - 4) + 2, :].rearrange("p k n -> p (k n)")
                        wkey = f"wd{wbuf}_{j - 4}"
                    if j % 2 == 0:
                        A(lambda e: e.copy(out=dstw, in_=stg[sb_][:]), [f"stg{sb_}"], [wkey])
                    else:
                        V(lambda e: e.tensor_copy(out=dstw, in_=stg[sb_][:]), [f"stg{sb_}"], [wkey])

                def transposes(pos):
                    mb_ = pos % 2
                    for sbi in range(2):
                        hi = mb_ * 2 + sbi
                        for hh in range(2):
                            def tr(e, hi=hi, hh=hh):
                                for k in range(8):
                                    kk = hh * 8 + k
                                    r = e.transpose(PT[hh][:, k * 128:(k + 1) * 128], AP(hg[hi], D, kk, [[16, 128]]), identb[:])
                                return r
                            P(tr, [f"hg{hi}", "identb"], [f"PT6{hh}"])
                            if hh == 0:
                                V(lambda e, sbi=sbi, hh=hh: e.tensor_copy(out=hgT[:, hh * 8:(hh + 1) * 8, sbi * 128:(sbi + 1) * 128], in_=PT[hh][:].rearrange("p (k c) -> p k c", k=8)), [], [f"PT6{hh}", f"hgT{sbi}_{hh}"])
                            else:
                                A(lambda e, sbi=sbi, hh=hh: e.copy(out=hgT[:, hh * 8:(hh + 1) * 8, sbi * 128:(sbi + 1) * 128], in_=PT[hh][:].rearrange("p (k c) -> p k c", k=8)), [], [f"PT6{hh}", f"hgT{sbi}_{hh}"])

                hgTk = [f"hgT{a}_{b2}" for a in range(2) for b2 in range(2)]
                actk = [f"actT{kk}" for kk in range(4)]

                def gateup(pos, kk):
                    wbuf = wb_of(pos)
                    wguk = [f"wgu{wbuf}_{j}" for j in range(4)]
                    pb = kk % 2

                    def mg(e):
                        for k in range(16):
                            r = e.matmul(PG[pb][:, 0:BS], lhsT=AP(wgu[wbuf], 16384, k * 1024 + kk, [[4, 128]]), rhs=hgT[:, k, :], start=(k == 0), stop=(k == 15))
                        return r
                    P(mg, wguk + hgTk, [f"PG6{pb}"])

                    def mu(e):
                        for k in range(16):
                            r = e.matmul(PU[pb][:, 0:BS], lhsT=AP(wgu[wbuf], 16384, k * 1024 + 512 + kk, [[4, 128]]), rhs=hgT[:, k, :], start=(k == 0), stop=(k == 15))
                        return r
                    P(mu, wguk + hgTk, [f"PU6{pb}"])
                    A(lambda e: e.activation(out=gsb[pb][:], in_=PG[pb][:, 0:BS], func=AF.Silu), [], [f"PG6{pb}", f"gsb{pb}"])
                    V(lambda e: e.tensor_tensor(out=actT[:, kk, :], in0=PU[pb][:, 0:BS], in1=gsb[pb][:], op=ALU.mult), [f"gsb{pb}"], [f"PU6{pb}", f"actT{kk}"])

                def down(pos, sbi, half):
                    blk = order[pos]
                    wbuf = wb_of(pos)
                    mb_ = pos % 2
                    wdk = [f"wd{wbuf}_{j}" for j in range(2)]
                    ys = state["ys"] % 2
                    state["ys"] += 1
                    for n2 in range(2):
                        n = half * 2 + n2
                        yb = state["yi"] % 2
                        state["yi"] += 1

                        def md(e, n=n, yb=yb):
                            for kk in range(4):
                                r = e.matmul(PY[yb][:], lhsT=actT[:, kk, sbi * 128:(sbi + 1) * 128], rhs=wd[wbuf][:, kk, n * 512:(n + 1) * 512], start=(kk == 0), stop=(kk == 3))
                            return r
                        P(md, actk + wdk, [f"PY6{yb}"])
                        if n2 == 0:
                            A(lambda e, n2=n2, yb=yb: e.activation(out=ysb[ys][:, n2 * 512:(n2 + 1) * 512], in_=PY[yb][:], func=AF.Copy, scale=rmt[mb_][:, sbi, 1:2]), [f"rmt{mb_}"], [f"PY6{yb}", f"ysb{ys}_{n2}"])
                        else:
                            V(lambda e, n2=n2, yb=yb: e.tensor_scalar(out=ysb[ys][:, n2 * 512:(n2 + 1) * 512], in0=PY[yb][:], scalar1=rmt[mb_][:, sbi, 1:2], scalar2=None, op0=ALU.mult), [f"rmt{mb_}"], [f"PY6{yb}", f"ysb{ys}_{n2}"])
                    r0 = blk * BS + sbi * 128
                    DM("sp", lambda e: e.dma_start(out=Y_d.ap()[r0:r0 + 128, half * 1024:(half + 1) * 1024], in_=ysb[ys][:]), [f"ysb{ys}_0", f"ysb{ys}_1"], [f"Y_d{blk}_{sbi}_{half}"])

                def mkreg(e):
                    r = e.alloc_register("oobbound")
                    e.reg_mov(r, 64 * 128 * 4 - 1)
                    state["bc"] = e.snap(r)
                mk.q["pool"].append(mkreg)
                slot_of = {}

                def issue(b_, j):
                    if b_ < NTOT and b_ <= NBLK and (b_, j) not in slot_of:
                        slot_of[(b_, j)] = wdma(b_, j)

                def cast(b_, j):
                    if b_ < NTOT and b_ <= NBLK:
                        issue(b_, j)
                        wcast(b_, j, slot_of[(b_, j)])

                def issue_dyn(p_, j):
                    if p_ < NBLK:
                        issue(p_, j)

                rows(0)
                for j in range(4):
                    issue(0, j)
                for j in range(4):
                    cast(0, j)
                issue(0, 4)
                issue(0, 5)
                issue_dyn(1, 0)
                issue_dyn(1, 1)
                for blk in range(NTOT):
                    nb = blk + 1
                    have_next = nb < NTOT
                    if have_next:
                        rows(nb)
                        if nb == NBLK:
                            for j in range(6):
                                issue(nb, j)
                    transposes(blk)
                    cast(blk, 4)
                    if have_next:
                        issue_dyn(nb, 2)
                    cast(blk, 5)
                    if have_next:
                        issue_dyn(nb, 3)
                    gateup(blk, 0)
                    if have_next:
                        cast(nb, 0)
                        issue_dyn(nb, 4)
                    gateup(blk, 1)
                    if have_next:
                        cast(nb, 1)
                        issue_dyn(nb, 5)
                    gateup(blk, 2)
                    if have_next:
                        cast(nb, 2)
                        issue_dyn(nb + 1, 0)
                    gateup(blk, 3)
                    if have_next:
                        cast(nb, 3)
                        issue_dyn(nb + 1, 1)
                    for sbi in range(2):
                        for half in range(2):
                            down(blk, sbi, half)
                mk.flush()
        if upto >= 7:
            with ExitStack() as pes:
                mk.pes = pes
                g2b = mk.sb("g2b", [128, D], F32)
                fgb = mk.sb("fgb", [128, D], F32)
                acc = [mk.sb(f"acc7{i}", [128, D], F32) for i in range(2)]
                gk = [mk.sb(f"gk{i}", [128, D], BF16) for i in range(8)]
                x1t = [mk.sb(f"x1t7{i}", [128, D], F32) for i in range(2)]
                junk = mk.sb("junk7", [128, D], BF16)
                tsum = mk.sb("tsum", [128, D], F32)
                st = mk.sb("st7", [128, NT, 2], F32)
                DM("sp", lambda e: e.dma_start(out=g2b[:], in_=ada_d.ap()[5:6, :].partition_broadcast(128)), w=["g2b"])
                DM("sp", lambda e: e.dma_start(out=fgb[:], in_=fng_d.ap().partition_broadcast(128)), w=["fgb"])
                gi = 0
                for t in range(NT):
                    b = t % 2
                    sl = slice(t * 128, (t + 1) * 128)
                    DM("sp", lambda e, b=b, sl=sl: e.dma_start(out=x1t[b][:], in_=x1_d.ap()[sl, :]), w=[f"x1t7{b}"])
                    gs = []
                    for k in range(7):
                        g_ = gi % 8
                        gi += 1
                        gs.append(g_)
                        if k == 6:
                            DM("sp", lambda e, g_=g_, t=t: e.dma_start(out=gk[g_][:], in_=Y_d.ap()[NROWS + t * 128:NROWS + (t + 1) * 128, :]), w=[f"gk{g_}"])
                        else:
                            DM("pool", lambda e, g_=g_, t=t, k=k: e.indirect_dma_start(out=gk[g_][:], out_offset=None, in_=Y_d.ap(), in_offset=bass.IndirectOffsetOnAxis(ap=dest_i[:, t, k:k + 1], axis=0)), ["dest_i"], [f"gk{g_}"])
                    G(lambda e, gs=gs: e.tensor_tensor(out=tsum[:], in0=gk[gs[0]][:], in1=gk[gs[1]][:], op=ALU.add), [f"gk{gs[0]}", f"gk{gs[1]}"], ["tsum"])
                    G(lambda e, gs=gs: e.tensor_tensor(out=tsum[:], in0=tsum[:], in1=gk[gs[2]][:], op=ALU.add), [f"gk{gs[2]}", "tsum"], ["tsum"])
                    V(lambda e, b=b, gs=gs: e.tensor_tensor(out=acc[b][:], in0=gk[gs[3]][:], in1=gk[gs[4]][:], op=ALU.add), [f"gk{gs[3]}", f"gk{gs[4]}"], [f"acc7{b}"])
                    for k in (5, 6):
                        V(lambda e, b=b, g_=gs[k]: e.tensor_tensor(out=acc[b][:], in0=acc[b][:], in1=gk[g_][:], op=ALU.add), [f"gk{gs[k]}", f"acc7{b}"], [f"acc7{b}"])
                    V(lambda e, b=b: e.tensor_tensor(out=acc[b][:], in0=acc[b][:], in1=tsum[:], op=ALU.add), ["tsum", f"acc7{b}"], [f"acc7{b}"])
                    V(lambda e, b=b: e.tensor_tensor(out=acc[b][:], in0=acc[b][:], in1=g2b[:], op=ALU.mult), ["g2b", f"acc7{b}"], [f"acc7{b}"])
                    V(lambda e, b=b: e.tensor_tensor(out=acc[b][:], in0=acc[b][:], in1=x1t[b][:], op=ALU.add), [f"x1t7{b}", f"acc7{b}"], [f"acc7{b}"])
                    A(lambda e, t=t, b=b: e.activation(out=junk[:], in_=acc[b][:], func=AF.Square, accum_out=st[:, t, 0:1]), [f"acc7{b}"], ["junk7", f"st7{t}"])
                    A(lambda e, t=t: e.activation(out=st[:, t, 1:2], in_=st[:, t, 0:1], func=AF.Sqrt, bias=EPS, scale=1.0 / D), [f"st7{t}"], [f"st7{t}"])
                    V(lambda e, t=t: e.reciprocal(out=st[:, t, 1:2], in_=st[:, t, 1:2]), [f"st7{t}"], [f"st7{t}"])
                    V(lambda e, t=t, b=b: e.scalar_tensor_tensor(out=acc[b][:], in0=acc[b][:], scalar=st[:, t, 1:2], in1=fgb[:], op0=ALU.mult, op1=ALU.mult), [f"acc7{b}", f"st7{t}", "fgb"], [f"acc7{b}"])
                    DM("sp", lambda e, b=b, sl=sl: e.dma_start(out=out_d.ap()[sl, :], in_=acc[b][:]), [f"acc7{b}"], [f"out{t}"])
                mk.flush()
        if upto >= 5:
            p57.close()

        if dbg and upto <= 2:
            pass
        mk.pes = pers
        mk.flush()
    return nc, dbg_d


def _host_inputs(b, inputs, with_experts=True):
    f = lambda a: np.ascontiguousarray(a, dtype=np.float32)
    half = 32
    inv = (10000.0 ** (-np.arange(half, dtype=np.float32) / half)).astype(np.float32)
    invf = np.zeros((64, 2), np.float32)
    invf[:, 0] = np.concatenate([inv, inv])
    invf[:32, 1] = -1.0
    invf[32:, 1] = 1.0
    m = {
        "x": f(inputs["x"][b]),
        "c": f(inputs["c"][b].reshape(16, 128).T),
        "positions": np.ascontiguousarray(inputs["positions"][b].reshape(1, S).astype(np.int32)),
        "w_ada": f(inputs["w_ada"][0]),
        "b_ada": f(inputs["b_ada"][0].reshape(1, -1)),
        "norm1_gain": f(inputs["norm1_gain"][0].reshape(1, -1)),
        "w_in": f(inputs["w_in"][0]),
        "dn_conv_w": f(inputs["dn_conv_w"][0].reshape(24, 128, 4).transpose(1, 0, 2)),
        "dn_a_log": f(inputs["dn_a_log"][0].reshape(1, 8)),
        "dn_dt_bias": f(inputs["dn_dt_bias"][0].reshape(1, 8)),
        "dn_norm_gain": f(inputs["dn_norm_gain"][0].reshape(1, 128)),
        "mla_q_norm_gain": f(inputs["mla_q_norm_gain"][0].reshape(4, 128).T),
        "w_q_up": f(inputs["w_q_up"][0]),
        "mla_kv_norm_gain": f(inputs["mla_kv_norm_gain"][0].reshape(4, 128).T),
        "w_kv_up": f(inputs["w_kv_up"][0]),
        "w_out": f(inputs["w_out"][0]),
        "norm2_gain": f(inputs["norm2_gain"][0].reshape(1, -1)),
        "w_router": f(inputs["w_router"][0]),
        "router_bias": f(inputs["router_bias"][0].reshape(1, 64)),
        "final_norm_gain": f(inputs["final_norm_gain"].reshape(1, -1)),
        "invf": invf,
    }
    if with_experts:
        m["w_exp_gate_up"] = f(inputs["w_exp_gate_up"][0]).reshape(64 * 128 * 4, 4096)
        m["w_exp_down"] = f(inputs["w_exp_down"][0]).reshape(64 * 128 * 2, 4096)
        m["w_sh_gate_up"] = f(inputs["w_sh_gate_up"][0]).reshape(128, 16 * 1024)
        m["w_sh_down"] = f(inputs["w_sh_down"][0]).reshape(128, 4 * 2048)
    return m


def kernel(**inputs):
    nc, _ = build()
    shared = None
    in_maps = []
    for b in range(8):
        m = _host_inputs(b, inputs)
        if shared is None:
            shared = m
        else:
            for k in m:
                if k not in ("x", "c", "positions"):
                    m[k] = shared[k]
        in_maps.append(m)
    res = run_bass_kernel_spmd(nc, in_maps, core_ids=list(range(8)))
    return np.stack([r["out"] for r in res.results], axis=0).astype(np.float32)
```

```python
import numpy as np
from contextlib import ExitStack
import concourse.bass as bass
import concourse.mybir as mybir
from concourse.bass_utils import run_bass_kernel_spmd

F32 = mybir.dt.float32
BF16 = mybir.dt.bfloat16
I32 = mybir.dt.int32
ALU = mybir.AluOpType
AF = mybir.ActivationFunctionType
AX = mybir.AxisListType

ENGS = ("pe", "act", "dve", "pool", "sp")
S = 2048
D = 2048
NT = 16
BS = 256
NBLK = 112
NROWS = NBLK * BS
EPS = 1e-6
TWO_PI = 6.283185307179586
PI = 3.141592653589793


class MK:
    NDMA = 20
    LIMIT = 30000

    def __init__(self, nc, es):
        self.nc = nc
        self.es = es
        self.q = {e: [] for e in ENGS}
        self.epoch = {e: 0 for e in ENGS}
        self.sem = {}
        self.cnt = {e: 0 for e in ENGS}
        for e in ENGS:
            self.sem[(e, 0)] = es.enter_context(nc.semaphore(f"s_{e}0"))
        self.dsem = {}
        self.dcnt = {}
        self.dnext = {}
        for e in ("sp", "pool", "act"):
            self.dsem[e] = [es.enter_context(nc.semaphore(f"d_{e}{i}")) for i in range(self.NDMA)]
            self.dcnt[e] = [0] * self.NDMA
            self.dnext[e] = 0
        self.seen = {e: {} for e in ENGS}
        self.last_w = {}
        self.readers = {}
        self.n_inst = 0
        self.pes = None
        self.uid = 0

    def sb(self, name, shape, dt):
        self.uid += 1
        return self.pes.enter_context(self.nc.sbuf_tensor(f"sb{self.uid}_{name}", list(shape), dt))

    def ps(self, name, shape, dt=F32):
        self.uid += 1
        return self.pes.enter_context(self.nc.psum_tensor(f"ps{self.uid}_{name}", list(shape), dt))

    def _semobj(self, key):
        if key[0] == "c":
            return self.sem[(key[1], key[2])]
        return self.dsem[key[1]][key[2]]

    def _deps(self, reads, writes):
        deps = set()
        for k in reads:
            t = self.last_w.get(k)
            if t is not None:
                deps.add(t)
        for k in writes:
            t = self.last_w.get(k)
            if t is not None:
                deps.add(t)
            for t in self.readers.get(k, ()):
                deps.add(t)
        return deps

    def _emit_waits(self, eng, deps):
        seen = self.seen[eng]
        best = {}
        for (key, val) in deps:
            if eng == "pe" and key[0] == "c" and key[1] == "pe":
                continue
            if seen.get(key, 0) >= val:
                continue
            if best.get(key, 0) < val:
                best[key] = val
        for key, val in best.items():
            seen[key] = val
            so = self._semobj(key)
            self.q[eng].append(lambda e, so=so, val=val: e.wait_ge(so, val))
            self.n_inst += 1

    def _commit(self, tok, reads, writes):
        for k in writes:
            self.last_w[k] = tok
            self.readers[k] = []
        for k in reads:
            self.readers.setdefault(k, []).append(tok)

    def op(self, eng, fn, reads=(), writes=()):
        deps = self._deps(reads, writes)
        self._emit_waits(eng, deps)
        if self.cnt[eng] >= self.LIMIT:
            self.epoch[eng] += 1
            self.cnt[eng] = 0
            self.sem[(eng, self.epoch[eng])] = self.es.enter_context(self.nc.semaphore(f"s_{eng}{self.epoch[eng]}"))
        self.cnt[eng] += 1
        so = self.sem[(eng, self.epoch[eng])]
        self.q[eng].append(lambda e, fn=fn, so=so: fn(e).then_inc(so, 1))
        self.n_inst += 1
        tok = (("c", eng, self.epoch[eng]), self.cnt[eng])
        self._commit(tok, reads, writes)
        return tok

    def dma(self, eng, fn, reads=(), writes=()):
        deps = self._deps(reads, writes)
        i = self.dnext[eng]
        self.dnext[eng] = (i + 1) % self.NDMA
        key = ("d", eng, i)
        if self.dcnt[eng][i] > 0:
            deps.add((key, self.dcnt[eng][i]))
        self._emit_waits(eng, deps)
        self.dcnt[eng][i] += 16
        so = self.dsem[eng][i]
        self.q[eng].append(lambda e, fn=fn, so=so: fn(e).then_inc(so, 16))
        self.n_inst += 1
        tok = (key, self.dcnt[eng][i])
        self._commit(tok, reads, writes)
        return tok

    def barrier(self):
        toks = set()
        for e in ENGS:
            if self.cnt[e] > 0:
                toks.add((("c", e, self.epoch[e]), self.cnt[e]))
        for e in self.dsem:
            for i in range(self.NDMA):
                if self.dcnt[e][i] > 0:
                    toks.add((("d", e, i), self.dcnt[e][i]))
        for e in ENGS:
            self._emit_waits(e, toks)

    def flush(self):
        self.barrier()
        nc = self.nc
        q = self.q
        with nc.Block() as block:
            @block.tensor
            def _(e):
                for f in q["pe"]:
                    f(e)

            @block.scalar
            def _(e):
                for f in q["act"]:
                    f(e)

            @block.vector
            def _(e):
                for f in q["dve"]:
                    f(e)

            @block.gpsimd
            def _(e):
                for f in q["pool"]:
                    f(e)

            @block.sync
            def _(e):
                for f in q["sp"]:
                    f(e)
        self.q = {e: [] for e in ENGS}


def AP(t, F, off, dims, npart=128):
    return bass.AP(t, off, [[F, npart]] + [list(d) for d in dims])


def build(upto=99, dbg=False, with_experts=True):
    nc = bass.Bass("TRN2", target_bir_lowering=False)
    dt = nc.dram_tensor
    x_d = dt("x", [S, D], F32, kind="ExternalInput")
    c_d = dt("c", [128, 16], F32, kind="ExternalInput")
    pos_d = dt("positions", [1, S], I32, kind="ExternalInput")
    w_ada_d = dt("w_ada", [D, 6 * D], F32, kind="ExternalInput")
    b_ada_d = dt("b_ada", [1, 6 * D], F32, kind="ExternalInput")
    n1g_d = dt("norm1_gain", [1, D], F32, kind="ExternalInput")
    w_in_d = dt("w_in", [D, 5200], F32, kind="ExternalInput")
    convw_d = dt("dn_conv_w", [128, 24, 4], F32, kind="ExternalInput")
    alog_d = dt("dn_a_log", [1, 8], F32, kind="ExternalInput")
    dtb_d = dt("dn_dt_bias", [1, 8], F32, kind="ExternalInput")
    dng_d = dt("dn_norm_gain", [1, 128], F32, kind="ExternalInput")
    qg_d = dt("mla_q_norm_gain", [128, 4], F32, kind="ExternalInput")
    wqu_d = dt("w_q_up", [512, 1536], F32, kind="ExternalInput")
    kvg_d = dt("mla_kv_norm_gain", [128, 4], F32, kind="ExternalInput")
    wkvu_d = dt("w_kv_up", [512, 2048], F32, kind="ExternalInput")
    wout_d = dt("w_out", [D, D], F32, kind="ExternalInput")
    n2g_d = dt("norm2_gain", [1, D], F32, kind="ExternalInput")
    wr_d = dt("w_router", [D, 64], F32, kind="ExternalInput")
    rb_d = dt("router_bias", [1, 64], F32, kind="ExternalInput")
    if with_experts:
        wegu_d = dt("w_exp_gate_up", [64 * 128 * 4, 4096], F32, kind="ExternalInput")
        wed_d = dt("w_exp_down", [64 * 128 * 2, 4096], F32, kind="ExternalInput")
        wsgu_d = dt("w_sh_gate_up", [128, 16 * 1024], F32, kind="ExternalInput")
        wsd_d = dt("w_sh_down", [128, 4 * 2048], F32, kind="ExternalInput")
    fng_d = dt("final_norm_gain", [1, D], F32, kind="ExternalInput")
    invf_d = dt("invf", [64, 2], F32, kind="ExternalInput")
    out_d = dt("out", [S, D], F32, kind="ExternalOutput")
    ada_d = dt("ada_s", [6, D], F32, kind="Internal")
    qT_d = dt("qT_s", [8, 128, S], F32, kind="Internal")
    kT_d = dt("kT_s", [8, 128, S], F32, kind="Internal")
    k_d = dt("k_s", [S, 8, 128], F32, kind="Internal")
    v_d = dt("v_s", [S, 8, 128], F32, kind="Internal")
    z_d = dt("z_s", [S, 1024], F32, kind="Internal")
    mix_d = dt("mix_s", [S, D], BF16, kind="Internal")
    x1_d = dt("x1_s", [S, D], F32, kind="Internal")
    h2_d = dt("h2_s", [S + 128, D], BF16, kind="Internal")
    rm_d = dt("rm_s", [NROWS, 2], F32, kind="Internal")
    Y_d = dt("Y_s", [NROWS + S, D], BF16, kind="Internal")
    dbg_d = {}

    def dbgout(name, shape, dtype=F32):
        dbg_d[name] = dt("dbg_" + name, list(shape), dtype, kind="ExternalOutput")
        return dbg_d[name]

    es = ExitStack()
    with es:
        mk = MK(nc, es)
        V = lambda fn, r=(), w=(): mk.op("dve", fn, r, w)
        A = lambda fn, r=(), w=(): mk.op("act", fn, r, w)
        P = lambda fn, r=(), w=(): mk.op("pe", fn, r, w)
        G = lambda fn, r=(), w=(): mk.op("pool", fn, r, w)
        DM = lambda eng, fn, r=(), w=(): mk.dma(eng, fn, r, w)

        pers = ExitStack()
        es.enter_context(pers)
        mk.pes = pers
        ident_i = mk.sb("ident_i", [128, 128], I32)
        identf = mk.sb("identf", [128, 128], F32)
        identb = mk.sb("identb", [128, 128], BF16)
        onesf = mk.sb("onesf", [128, 128], F32)
        dif = mk.sb("dif", [128, 128], F32)
        bg_sb = mk.sb("bg_sb", [128, NT, 16], F32)
        G(lambda e: e.iota(ident_i[:], [[1, 128]], base=0, channel_multiplier=-1), w=["ident_i"])
        V(lambda e: e.tensor_copy(out=dif[:], in_=ident_i[:]), ["ident_i"], ["dif"])
        V(lambda e: e.tensor_single_scalar(out=identf[:], in_=dif[:], scalar=0.0, op=ALU.is_equal), ["dif"], ["identf"])
        V(lambda e: e.tensor_copy(out=identb[:], in_=identf[:]), ["identf"], ["identb"])
        G(lambda e: e.memset(onesf[:], 1.0), w=["onesf"])

        if upto >= 0:
            with ExitStack() as pes:
                mk.pes = pes
                c_sb = mk.sb("c_sb", [128, 16], F32)
                sc = mk.sb("sc", [128, 16], F32)
                wa = [mk.sb(f"wa{i}", [128, 16, 512], F32) for i in range(2)]
                arow = mk.sb("arow", [1, 6 * D], F32)
                brow = mk.sb("brow", [1, 6 * D], F32)
                grow = mk.sb("grow", [1, 2 * D], F32)
                psa = [mk.ps(f"psa{i}", [128, 512], F32) for i in range(2)]
                DM("sp", lambda e: e.dma_start(out=c_sb[:], in_=c_d.ap()), w=["c_sb"])
                DM("sp", lambda e: e.dma_start(out=brow[:], in_=b_ada_d.ap()), w=["brow"])
                DM("sp", lambda e: e.dma_start(out=grow[:, 0:D], in_=n1g_d.ap()), w=["grow0"])
                DM("sp", lambda e: e.dma_start(out=grow[:, D:2 * D], in_=n2g_d.ap()), w=["grow1"])
                A(lambda e: e.activation(out=sc[:], in_=c_sb[:], func=AF.Silu), ["c_sb"], ["sc"])
                wsrc = w_ada_d.ap().rearrange("(k p) n -> p k n", p=128)
                for j in range(24):
                    b = j % 2
                    DM("sp", lambda e, j=j, b=b: e.dma_start(out=wa[b][:], in_=wsrc[:, :, j * 512:(j + 1) * 512]), w=[f"wa{b}"])

                    def mm(e, b=b):
                        for k in range(16):
                            r = e.matmul(psa[b][0:1, :], lhsT=sc[:, k:k + 1], rhs=wa[b][:, k, :], start=(k == 0), stop=(k == 15))
                        return r
                    P(mm, ["sc", f"wa{b}"], [f"psa{b}"])
                    V(lambda e, j=j, b=b: e.tensor_tensor(out=arow[:, j * 512:(j + 1) * 512], in0=psa[b][0:1, :], in1=brow[:, j * 512:(j + 1) * 512], op=ALU.add),
                      ["brow"], [f"psa{b}", f"arow{j}"])
                akeys = [f"arow{j}" for j in range(24)]
                V(lambda e: e.scalar_tensor_tensor(out=arow[:, D:2 * D], in0=arow[:, D:2 * D], scalar=1.0, in1=grow[:, 0:D], op0=ALU.add, op1=ALU.mult), akeys + ["grow0"], ["arowG1"])
                V(lambda e: e.scalar_tensor_tensor(out=arow[:, 4 * D:5 * D], in0=arow[:, 4 * D:5 * D], scalar=1.0, in1=grow[:, D:2 * D], op0=ALU.add, op1=ALU.mult), akeys + ["grow1"], ["arowG2"])
                DM("sp", lambda e: e.dma_start(out=ada_d.ap().rearrange("(o r) n -> o (r n)", o=1), in_=arow[:]), akeys + ["arowG1", "arowG2"], ["ada_d"])
                mk.flush()

        p13 = ExitStack()
        es.enter_context(p13)
        if upto >= 1:
            mk.pes = p13
            cqn = mk.sb("cqn", [128, 4, S], BF16)
            ckvn = mk.sb("ckvn", [128, 4, S], BF16)
            KTpe = mk.sb("KTpe", [64, S], BF16)
            CS = mk.sb("CS", [64, S], F32)
            SN = mk.sb("SN", [64, S], F32)
            p12 = ExitStack()
            p13.enter_context(p12)
            mk.pes = p12
            hT = mk.sb("hT", [128, 16, S], BF16)
            with ExitStack() as pes:
                mk.pes = pes
                G1b = mk.sb("G1b", [128, D], F32)
                S1b = mk.sb("S1b", [128, D], F32)
                xt = [mk.sb(f"xt{i}", [128, D], F32) for i in range(2)]
                junk = mk.sb("junk", [128, D], BF16)
                tmpf = mk.sb("tmpf", [128, D], F32)
                hbf = [mk.sb(f"hbf{i}", [128, D], BF16) for i in range(2)]
                st = mk.sb("st", [128, NT, 2], F32)
                pst = [mk.ps(f"pst{i}", [128, 1024], BF16) for i in range(2)]
                DM("sp", lambda e: e.dma_start(out=S1b[:], in_=ada_d.ap()[0:1, :].partition_broadcast(128)), ["ada_d"], ["S1b"])
                DM("sp", lambda e: e.dma_start(out=G1b[:], in_=ada_d.ap()[1:2, :].partition_broadcast(128)), ["ada_d"], ["G1b"])
                for t in range(NT):
                    b = t % 2
                    DM("sp", lambda e, t=t, b=b: e.dma_start(out=xt[b][:], in_=x_d.ap()[t * 128:(t + 1) * 128, :]), w=[f"xt{b}"])
                    A(lambda e, t=t, b=b: e.activation(out=junk[:], in_=xt[b][:], func=AF.Square, accum_out=st[:, t, 0:1]), [f"xt{b}"], ["junk", f"st{t}"])
                    A(lambda e, t=t: e.activation(out=st[:, t, 1:2], in_=st[:, t, 0:1], func=AF.Sqrt, bias=EPS, scale=1.0 / D), [f"st{t}"], [f"st{t}"])
                    V(lambda e, t=t: e.reciprocal(out=st[:, t, 1:2], in_=st[:, t, 1:2]), [f"st{t}"], [f"st{t}"])
                    V(lambda e, t=t, b=b: e.scalar_tensor_tensor(out=tmpf[:], in0=xt[b][:], scalar=st[:, t, 1:2], in1=G1b[:], op0=ALU.mult, op1=ALU.mult), [f"xt{b}", f"st{t}", "G1b"], ["tmpf"])
                    V(lambda e, b=b: e.tensor_tensor(out=hbf[b][:], in0=tmpf[:], in1=S1b[:], op=ALU.add), ["tmpf", "S1b"], [f"hbf{b}"])
                    for hh in range(2):
                        def tr(e, b=b, hh=hh):
                            for k in range(8):
                                kk = hh * 8 + k
                                r = e.transpose(pst[hh][:, k * 128:(k + 1) * 128], hbf[b][:, kk * 128:(kk + 1) * 128], identb[:])
                            return r
                        P(tr, [f"hbf{b}", "identb"], [f"pst{hh}"])
                        eng = V if hh == 0 else A
                        if hh == 0:
                            V(lambda e, t=t, hh=hh: e.tensor_copy(out=hT[:, hh * 8:(hh + 1) * 8, t * 128:(t + 1) * 128], in_=pst[hh][:].rearrange("p (k c) -> p k c", k=8)), [], [f"pst{hh}", f"hT{t}"])
                        else:
                            A(lambda e, t=t, hh=hh: e.copy(out=hT[:, hh * 8:(hh + 1) * 8, t * 128:(t + 1) * 128], in_=pst[hh][:].rearrange("p (k c) -> p k c", k=8)), [], [f"pst{hh}", f"hT{t}b"])
                if dbg and upto == 1:
                    dd = dbgout("hT", [128, 16, S], BF16)
                    DM("sp", lambda e: e.dma_start(out=dd.ap(), in_=hT[:]), [f"hT{t}" for t in range(NT)] + [f"hT{t}b" for t in range(NT)], ["dbg"])
                mk.flush()
        hTkeys = [f"hT{t}" for t in range(NT)] + [f"hT{t}b" for t in range(NT)]

        if upto >= 2:
            wsrc = w_in_d.ap().rearrange("(k p) n -> p k n", p=128)
            with ExitStack() as pes:
                mk.pes = pes
                wb = [mk.sb(f"wb{i}", [128, 16, 512], BF16) for i in range(2)]
                cw = mk.sb("cw", [128, 24, 4], F32)
                raws = [mk.sb(f"raw{i}", [128, 3 + S], F32) for i in range(2)]
                acc = mk.sb("acc", [128, S], F32)
                cs = mk.sb("cs", [128, S], F32)
                sq = mk.sb("sq", [128, S], F32)
                nrm = acc
                tok = mk.sb("tok", [128, NT, 128], F32)
                PQ = mk.ps("PQ", [128, S], F32)
                PN = mk.ps("PN", [128, 1024], F32)
                PT = mk.ps("PT", [128, 1024], F32)
                DM("sp", lambda e: e.dma_start(out=cw[:], in_=convw_d.ap()), w=["cw"])
                for i in range(2):
                    G(lambda e, i=i: e.memset(raws[i][:, 0:3], 0.0), w=[f"rawpad{i}"])

                def wload(blk):
                    b = blk % 2
                    DM("pool", lambda e: e.dma_start(out=wb[b][:], in_=wsrc[:, :, blk * 512:(blk + 1) * 512]), w=[f"wb{b}"])

                def stageA(cc):
                    blk, ci = cc // 4, cc % 4
                    b = blk % 2
                    rb = cc % 2
                    raw = raws[rb]
                    if ci == 0 and blk + 1 < 6:
                        wload(blk + 1)
                    for qd in range(4):
                        def mm(e, qd=qd):
                            for k in range(16):
                                r = e.matmul(PQ[:, qd * 512:(qd + 1) * 512], lhsT=wb[b][:, k, ci * 128:(ci + 1) * 128], rhs=hT[:, k, qd * 512:(qd + 1) * 512], start=(k == 0), stop=(k == 15))
                            return r
                        P(mm, [f"wb{b}"] + hTkeys, [f"PQ{qd}"])
                        A(lambda e, qd=qd: e.copy(out=raw[:, 3 + qd * 512:3 + (qd + 1) * 512], in_=PQ[:, qd * 512:(qd + 1) * 512]), [], [f"PQ{qd}", f"raw{rb}_{qd}"])
                        yield

                def stageB(cc):
                    rb = cc % 2
                    raw = raws[rb]
                    rk = [f"raw{rb}_{q}" for q in range(4)] + [f"rawpad{rb}"]
                    V(lambda e: e.tensor_scalar(out=acc[:], in0=raw[:, 3:3 + S], scalar1=cw[:, cc, 3:4], scalar2=None, op0=ALU.mult), rk + ["cw"], ["acc"])
                    for j in (2, 1, 0):
                        V(lambda e, j=j: e.scalar_tensor_tensor(out=acc[:], in0=raw[:, j:j + S], scalar=cw[:, cc, j:j + 1], in1=acc[:], op0=ALU.mult, op1=ALU.add), rk + ["cw", "acc"], ["acc"])
                    yield
                    A(lambda e: e.activation(out=cs[:], in_=acc[:], func=AF.Silu), ["acc"], ["cs"])
                    yield
                    if cc < 16:
                        head = cc % 8
                        isq = cc < 8
                        A(lambda e: e.activation(out=sq[:], in_=cs[:], func=AF.Square), ["cs"], ["sq"])
                        for hf in range(2):
                            def mm2(e, hf=hf):
                                for q2 in range(2):
                                    r = e.matmul(PN[:, q2 * 512:(q2 + 1) * 512], lhsT=onesf[:], rhs=sq[:, hf * 1024 + q2 * 512: hf * 1024 + (q2 + 1) * 512], start=True, stop=True)
                                return r
                            P(mm2, ["sq", "onesf"], ["PN"])
                            scl = 128.0 if isq else 1.0
                            A(lambda e, hf=hf, scl=scl: e.activation(out=nrm[:, hf * 1024:(hf + 1) * 1024], in_=PN[:], func=AF.Sqrt, bias=EPS * scl, scale=scl), [], ["PN", "acc"])
                            yield
                        V(lambda e: e.reciprocal(out=nrm[:], in_=nrm[:]), ["acc"], ["acc"])
                        V(lambda e: e.tensor_tensor(out=cs[:], in0=cs[:], in1=nrm[:], op=ALU.mult), ["cs", "acc"], ["cs"])
                        dst = qT_d if isq else kT_d
                        DM("sp", lambda e, dst=dst, head=head: e.dma_start(out=dst.ap()[head], in_=cs[:]), ["cs"], [f"qk_d{cc}"])
                        yield
                    if cc >= 8:
                        head = cc % 8
                        dst = k_d if cc < 16 else v_d
                        for g4 in range(4):
                            def tr(e, g4=g4):
                                for i in range(4):
                                    t = g4 * 4 + i
                                    r = e.transpose(PT[:, ((g4 % 2) * 4 + i) * 128:((g4 % 2) * 4 + i + 1) * 128], cs[:, t * 128:(t + 1) * 128], identf[:])
                                return r
                            P(tr, ["cs", "identf"], [f"PT{g4 % 2}"])
                            V(lambda e, g4=g4: e.tensor_copy(out=tok[:, g4 * 4:(g4 + 1) * 4, :], in_=PT[:, (g4 % 2) * 512:((g4 % 2) + 1) * 512].rearrange("p (t c) -> p t c", t=4)), [], [f"PT{g4 % 2}", f"tok{g4}"])
                            yield
                        DM("sp", lambda e, dst=dst, head=head: e.dma_start(out=dst.ap().rearrange("(t p) h c -> p t h c", p=128)[:, :, head, :], in_=tok[:]), [f"tok{g}" for g in range(4)], [f"kv_d{cc}"])
                    yield

                def interleave2(gens):
                    gens = [g for g in gens if g is not None]
                    while gens:
                        for g in list(gens):
                            try:
                                next(g)
                            except StopIteration:
                                gens.remove(g)

                wload(0)
                interleave2([stageA(0)])
                for cc in range(24):
                    interleave2([stageA(cc + 1) if cc + 1 < 24 else None, stageB(cc)])
                mk.flush()
            with ExitStack() as pes:
                mk.pes = pes
                wb = [mk.sb(f"wz{i}", [128, 16, 512], BF16) for i in range(2)]
                wsm = mk.sb("wsm", [128, 16, 16], BF16)
                zs = [mk.sb(f"zs{i}", [128, 512], F32) for i in range(2)]
                dtb = mk.sb("dtb", [128, 8], F32)
                nA = mk.sb("nA", [128, 8], F32)
                tmp8 = mk.sb("tmp8", [128, NT, 8], F32)
                PZ = [mk.ps(f"PZ{i}", [128, 512], F32) for i in range(2)]
                PB = mk.ps("PB", [128, NT * 16], F32)
                for blk in range(2):
                    DM("pool", lambda e, blk=blk: e.dma_start(out=wb[blk][:], in_=wsrc[:, :, 3072 + blk * 512:3072 + (blk + 1) * 512]), w=[f"wz{blk}"])
                DM("pool", lambda e: e.dma_start(out=wsm[:], in_=wsrc[:, :, 4096:4112]), w=["wsm"])
                DM("sp", lambda e: e.dma_start(out=dtb[:], in_=dtb_d.ap().partition_broadcast(128)), w=["dtb"])
                DM("sp", lambda e: e.dma_start(out=nA[:], in_=alog_d.ap().partition_broadcast(128)), w=["nA"])
                i = 0
                for blk in range(2):
                    for t in range(NT):
                        b = i % 2
                        i += 1

                        def mm(e, blk=blk, t=t, b=b):
                            for k in range(16):
                                r = e.matmul(PZ[b][:], lhsT=hT[:, k, t * 128:(t + 1) * 128], rhs=wb[blk][:, k, :], start=(k == 0), stop=(k == 15))
                            return r
                        P(mm, [f"wz{blk}"] + hTkeys, [f"PZ{b}"])
                        A(lambda e, b=b: e.activation(out=zs[b][:], in_=PZ[b][:], func=AF.Silu), [], [f"PZ{b}", f"zs{b}"])
                        DM("sp", lambda e, blk=blk, t=t, b=b: e.dma_start(out=z_d.ap()[t * 128:(t + 1) * 128, blk * 512:(blk + 1) * 512], in_=zs[b][:]), [f"zs{b}"], [f"z_d{blk}_{t}"])
                for t in range(NT):
                    def mm(e, t=t):
                        for k in range(16):
                            r = e.matmul(PB[:, t * 16:(t + 1) * 16], lhsT=hT[:, k, t * 128:(t + 1) * 128], rhs=wsm[:, k, :], start=(k == 0), stop=(k == 15))
                        return r
                    P(mm, ["wsm"] + hTkeys, ["PB"])
                PB3 = PB[:].rearrange("p (t c) -> p t c", t=NT)
                A(lambda e: e.activation(out=bg_sb[:, :, 0:8], in_=PB3[:, :, 0:8], func=AF.Sigmoid), [], ["PB", "bg_b"])
                V(lambda e: e.tensor_tensor(out=tmp8[:], in0=PB3[:, :, 8:16], in1=AP(dtb, 8, 0, [[0, NT], [1, 8]]), op=ALU.add), ["dtb"], ["PB", "tmp8"])
                A(lambda e: e.activation(out=tmp8[:], in_=tmp8[:], func=AF.Exp), ["tmp8"], ["tmp8"])
                A(lambda e: e.activation(out=tmp8[:], in_=tmp8[:], func=AF.Ln, bias=1.0, scale=1.0), ["tmp8"], ["tmp8"])
                A(lambda e: e.activation(out=nA[:], in_=nA[:], func=AF.Exp), ["nA"], ["nA"])
                V(lambda e: e.scalar_tensor_tensor(out=bg_sb[:, :, 8:16], in0=tmp8[:], scalar=-1.0, in1=AP(nA, 8, 0, [[0, NT], [1, 8]]), op0=ALU.mult, op1=ALU.mult), ["tmp8", "nA"], ["bg_g"])
                mk.flush()
            with ExitStack() as pes:
                mk.pes = pes
                wb = [mk.sb(f"wl{i}", [128, 16, 512], BF16) for i in range(2)]
                lat = mk.sb("lat", [128, 4, S], BF16)
                sq = mk.sb("sq2", [128, S], F32)
                rs = mk.sb("rs", [128, S], F32)
                gq = mk.sb("gq", [128, 8], F32)
                PQ = mk.ps("PQ2", [128, S], F32)
                PN = mk.ps("PN2", [128, S], F32)
                DM("sp", lambda e: e.dma_start(out=gq[:, 0:4], in_=qg_d.ap()), w=["gq0"])
                DM("sp", lambda e: e.dma_start(out=gq[:, 4:8], in_=kvg_d.ap()), w=["gq1"])
                for blk in range(2):
                    DM("pool", lambda e, blk=blk: e.dma_start(out=wb[blk][:], in_=wsrc[:, :, 4112 + blk * 512:4112 + (blk + 1) * 512]), w=[f"wl{blk}"])
                    for ci in range(4):
                        for qd in range(4):
                            def mm(e, blk=blk, ci=ci, qd=qd):
                                for k in range(16):
                                    r = e.matmul(PQ[:, qd * 512:(qd + 1) * 512], lhsT=wb[blk][:, k, ci * 128:(ci + 1) * 128], rhs=hT[:, k, qd * 512:(qd + 1) * 512], start=(k == 0), stop=(k == 15))
                                return r
                            P(mm, [f"wl{blk}"] + hTkeys, [f"PQ{qd}"])
                            V(lambda e, ci=ci, qd=qd: e.tensor_copy(out=lat[:, ci, qd * 512:(qd + 1) * 512], in_=PQ[:, qd * 512:(qd + 1) * 512]), [], [f"PQ{qd}", f"lat{ci}_{qd}"])
                            A(lambda e, qd=qd: e.activation(out=sq[:, qd * 512:(qd + 1) * 512], in_=PQ[:, qd * 512:(qd + 1) * 512], func=AF.Square), [], [f"PQ{qd}", f"sq{qd}"])
                            P(lambda e, ci=ci, qd=qd: e.matmul(PN[:, qd * 512:(qd + 1) * 512], lhsT=onesf[:], rhs=sq[:, qd * 512:(qd + 1) * 512], start=(ci == 0), stop=(ci == 3)), [f"sq{qd}", "onesf"], [f"PN{qd}"])
                    for qd in range(4):
                        A(lambda e, qd=qd: e.activation(out=rs[:, qd * 512:(qd + 1) * 512], in_=PN[:, qd * 512:(qd + 1) * 512], func=AF.Sqrt, bias=EPS, scale=1.0 / 512), [], [f"PN{qd}", f"rs{qd}"])
                        V(lambda e, qd=qd: e.reciprocal(out=rs[:, qd * 512:(qd + 1) * 512], in_=rs[:, qd * 512:(qd + 1) * 512]), [f"rs{qd}"], [f"rs{qd}"])
                    dstn = cqn if blk == 0 else ckvn
                    for ci in range(4):
                        V(lambda e, blk=blk, ci=ci, dstn=dstn: e.scalar_tensor_tensor(out=dstn[:, ci, :], in0=lat[:, ci, :], scalar=gq[:, blk * 4 + ci:blk * 4 + ci + 1], in1=rs[:], op0=ALU.mult, op1=ALU.mult),
                          [f"lat{ci}_{q}" for q in range(4)] + [f"rs{q}" for q in range(4)] + ["gq0", "gq1"], [f"latn{blk}_{ci}"])
                mk.flush()
            with ExitStack() as pes:
                mk.pes = pes
                wk = mk.sb("wk", [128, 16, 64], BF16)
                wks = mk.sb("wks", [128, 16, 64], BF16)
                posi = mk.sb("posi", [64, S], I32)
                ang = mk.sb("ang", [64, S], F32)
                kf = mk.sb("kf", [64, S], F32)
                ki = mk.sb("ki", [64, S], I32)
                invf = mk.sb("invf", [64, 2], F32)
                t1 = mk.sb("t1", [64, S], F32)
                PA = mk.ps("PA", [128, S], F32)
                PBm = mk.ps("PBm", [128, S], F32)
                DM("pool", lambda e: e.dma_start(out=wk[:], in_=wsrc[:, :, 5136:5200]), w=["wk"])
                DM("pool", lambda e: e.dma_start(out=wks[:, :, 0:32], in_=wsrc[:, :, 5168:5200]), w=["wks0"])
                DM("pool", lambda e: e.dma_start(out=wks[:, :, 32:64], in_=wsrc[:, :, 5136:5168]), w=["wks1"])
                DM("sp", lambda e: e.dma_start(out=invf[:], in_=invf_d.ap()), w=["invf"])
                DM("sp", lambda e: e.dma_start(out=posi[:], in_=pos_d.ap().partition_broadcast(64)), w=["posi"])
                V(lambda e: e.tensor_copy(out=ang[:], in_=posi[:]), ["posi"], ["ang"])
                V(lambda e: e.tensor_scalar(out=ang[:], in0=ang[:], scalar1=invf[:, 0:1], scalar2=None, op0=ALU.mult), ["ang", "invf"], ["ang"])
                for which, tab, shift in (("sin", SN, 0.0), ("cos", CS, PI / 2)):
                    V(lambda e, shift=shift: e.tensor_scalar(out=kf[:], in0=ang[:], scalar1=shift, scalar2=1.0 / TWO_PI, op0=ALU.add, op1=ALU.mult), ["ang"], ["kf"])
                    V(lambda e: e.tensor_copy(out=ki[:], in_=kf[:]), ["kf"], ["ki"])
                    V(lambda e: e.tensor_copy(out=kf[:], in_=ki[:]), ["ki"], ["kf"])
                    V(lambda e, shift=shift: e.scalar_tensor_tensor(out=kf[:], in0=kf[:], scalar=-TWO_PI, in1=ang[:], op0=ALU.mult, op1=ALU.add), ["kf", "ang"], ["kf"])
                    V(lambda e, shift=shift: e.tensor_scalar(out=kf[:], in0=kf[:], scalar1=shift, scalar2=PI, op0=ALU.add, op1=ALU.min), ["kf"], ["kf"])
                    V(lambda e: e.tensor_scalar(out=kf[:], in0=kf[:], scalar1=-PI, scalar2=None, op0=ALU.max), ["kf"], ["kf"])
                    A(lambda e, tab=tab: e.activation(out=tab[:], in_=kf[:], func=AF.Sin), ["kf"], [which])
                V(lambda e: e.tensor_scalar(out=SN[:], in0=SN[:], scalar1=invf[:, 1:2], scalar2=None, op0=ALU.mult), ["sin", "invf"], ["sin"])
                for qd in range(4):
                    def mm(e, qd=qd):
                        for k in range(16):
                            r = e.matmul(PA[0:64, qd * 512:(qd + 1) * 512], lhsT=wk[:, k, :], rhs=hT[:, k, qd * 512:(qd + 1) * 512], start=(k == 0), stop=(k == 15))
                        return r
                    P(mm, ["wk"] + hTkeys, [f"PA{qd}"])

                    def mm2(e, qd=qd):
                        for k in range(16):
                            r = e.matmul(PBm[0:64, qd * 512:(qd + 1) * 512], lhsT=wks[:, k, :], rhs=hT[:, k, qd * 512:(qd + 1) * 512], start=(k == 0), stop=(k == 15))
                        return r
                    P(mm2, ["wks0", "wks1"] + hTkeys, [f"PB{qd}"])
                    sl = slice(qd * 512, (qd + 1) * 512)
                    V(lambda e, sl=sl: e.tensor_tensor(out=t1[:, sl], in0=PA[0:64, sl], in1=CS[:, sl], op=ALU.mult), ["cos"], [f"PA{qd}", f"t1{qd}"])
                    V(lambda e, sl=sl: e.tensor_tensor(out=kf[:, sl], in0=PBm[0:64, sl], in1=SN[:, sl], op=ALU.mult), ["sin"], [f"PB{qd}", "kf"])
                    V(lambda e, sl=sl: e.tensor_tensor(out=KTpe[:, sl], in0=t1[:, sl], in1=kf[:, sl], op=ALU.add), [f"t1{qd}", "kf"], [f"KTpe{qd}"])
                if dbg and upto == 2:
                    d1 = dbgout("cqn", [128, 4, S], BF16)
                    d2 = dbgout("KTpe", [64, S], BF16)
                    d3 = dbgout("bg", [128, NT, 16], F32)
                    d4 = dbgout("ckvn", [128, 4, S], BF16)
                    mk.barrier()
                    DM("sp", lambda e: e.dma_start(out=d4.ap(), in_=ckvn[:]), [], ["dbg4"])
                    for nm, src, shp in (("qT", qT_d, [8, 128, S]), ("kT", kT_d, [8, 128, S]), ("k", k_d, [S, 8, 128]), ("v", v_d, [S, 8, 128]), ("z", z_d, [S, 1024]), ("ada", ada_d, [6, D])):
                        dd = dbgout(nm, shp, F32)
                        DM("sp", lambda e, dd=dd, src=src: e.dma_start(out=dd.ap(), in_=src.ap()), [], ["dbg_" + nm])
                    DM("sp", lambda e: e.dma_start(out=d1.ap(), in_=cqn[:]), [], ["dbg1"])
                    DM("sp", lambda e: e.dma_start(out=d2.ap(), in_=KTpe[:]), [], ["dbg2"])
                    DM("sp", lambda e: e.dma_start(out=d3.ap(), in_=bg_sb[:]), [], ["dbg3"])
                mk.flush()
            p12.close()

        if upto >= 3:
            with ExitStack() as pes:
                mk.pes = pes
                SC = 192.0 ** -0.5
                wq = mk.sb("wq", [128, 4, 1536], BF16)
                wqs = mk.sb("wqs", [128, 4, 8, 64], BF16)
                wkv = mk.sb("wkv", [128, 4, 2048], BF16)
                QTn = mk.sb("QTn", [128, S], BF16)
                QTp = mk.sb("QTp", [64, S], BF16)
                KTn = mk.sb("KTn", [128, S], BF16)
                Vaug = mk.sb("Vaug", [128, NT, 130], BF16)
                PTs = [mk.sb(f"PTs{i}", [128, 512], BF16) for i in range(4)]
                it0 = [0]
                mla_tok = mk.sb("mla_tok", [128, NT, 1024], BF16)
                t1 = mk.sb("t1m", [64, S], F32)
                t2 = mk.sb("t2m", [64, S], F32)
                triu = mk.sb("triu", [128, 128], BF16)
                rec = mk.sb("rec", [128, 4], F32)
                P0 = mk.ps("P0", [128, S], F32)
                P1 = mk.ps("P1", [128, S], F32)
                DM("pool", lambda e: e.dma_start(out=wq[:], in_=wqu_d.ap().rearrange("(k p) n -> p k n", p=128)), w=["wq"])
                DM("pool", lambda e: e.dma_start(out=wkv[:], in_=wkvu_d.ap().rearrange("(k p) n -> p k n", p=128)), w=["wkv"])
                src4 = wqu_d.ap().rearrange("(k p) (h c) -> p k h c", p=128, c=192)
                for k4 in range(4):
                    DM("pool", lambda e, k4=k4: e.dma_start(out=wqs[:, k4, :, 0:32], in_=src4[:, k4, :, 160:192]), w=[f"wqs0_{k4}"])
                    DM("pool", lambda e, k4=k4: e.dma_start(out=wqs[:, k4, :, 32:64], in_=src4[:, k4, :, 128:160]), w=[f"wqs1_{k4}"])
                V(lambda e: e.tensor_single_scalar(out=triu[:], in_=dif[:], scalar=0.0, op=ALU.is_ge), ["dif"], ["triu"])
                G(lambda e: e.memset(Vaug[:, :, 128:130], 1.0), w=["Vaug1"])
                latq = [f"latn0_{c}" for c in range(4)]
                latkv = [f"latn1_{c}" for c in range(4)]
                it = 0
                for h in range(8):
                    for qd in range(4):
                        sl = slice(qd * 512, (qd + 1) * 512)

                        def mm(e, h=h, sl=sl):
                            for k in range(4):
                                r = e.matmul(P0[:, sl], lhsT=wq[:, k, h * 192:h * 192 + 128], rhs=cqn[:, k, sl], start=(k == 0), stop=(k == 3))
                            return r
                        P(mm, ["wq"] + latq, [f"P0_{qd}"])
                        A(lambda e, sl=sl: e.mul(out=QTn[:, sl], in_=P0[:, sl], mul=SC), [], [f"P0_{qd}", f"QTn{qd}"])
                    for qd in range(4):
                        sl = slice(qd * 512, (qd + 1) * 512)

                        def mma(e, h=h, sl=sl):
                            for k in range(4):
                                r = e.matmul(P0[0:64, sl], lhsT=wq[:, k, h * 192 + 128:h * 192 + 192], rhs=cqn[:, k, sl], start=(k == 0), stop=(k == 3))
                            return r
                        P(mma, ["wq"] + latq, [f"P0_{qd}"])

                        def mmb(e, h=h, sl=sl):
                            for k in range(4):
                                r = e.matmul(P1[0:64, sl], lhsT=wqs[:, k, h, :], rhs=cqn[:, k, sl], start=(k == 0), stop=(k == 3))
                            return r
                        P(mmb, [f"wqs{a}_{b}" for a in range(2) for b in range(4)] + latq, [f"P1_{qd}"])
                        V(lambda e, sl=sl: e.scalar_tensor_tensor(out=t1[:, sl], in0=P0[0:64, sl], scalar=SC, in1=CS[:, sl], op0=ALU.mult, op1=ALU.mult), ["cos"], [f"P0_{qd}", f"t1m{qd}"])
                        V(lambda e, sl=sl: e.scalar_tensor_tensor(out=t2[:, sl], in0=P1[0:64, sl], scalar=SC, in1=SN[:, sl], op0=ALU.mult, op1=ALU.mult), ["sin"], [f"P1_{qd}", f"t2m{qd}"])
                        V(lambda e, sl=sl: e.tensor_tensor(out=QTp[:, sl], in0=t1[:, sl], in1=t2[:, sl], op=ALU.add), [f"t1m{qd}", f"t2m{qd}"], [f"QTp{qd}"])
                    for qd in range(4):
                        sl = slice(qd * 512, (qd + 1) * 512)

                        def mmk(e, h=h, sl=sl):
                            for k in range(4):
                                r = e.matmul(P0[:, sl], lhsT=wkv[:, k, h * 256:h * 256 + 128], rhs=ckvn[:, k, sl], start=(k == 0), stop=(k == 3))
                            return r
                        P(mmk, ["wkv"] + latkv, [f"P0_{qd}"])
                        V(lambda e, sl=sl: e.tensor_copy(out=KTn[:, sl], in_=P0[:, sl]), [], [f"P0_{qd}", f"KTn{qd}"])
                    for g4 in range(4):
                        def mmv(e, h=h, g4=g4):
                            for i in range(4):
                                t = g4 * 4 + i
                                for k in range(4):
                                    r = e.matmul(P1[:, t * 128:(t + 1) * 128], lhsT=ckvn[:, k, t * 128:(t + 1) * 128], rhs=wkv[:, k, h * 256 + 128:h * 256 + 256], start=(k == 0), stop=(k == 3))
                            return r
                        P(mmv, ["wkv"] + latkv, [f"P1_{g4}"])
                        A(lambda e, g4=g4: e.copy(out=Vaug[:, g4 * 4:(g4 + 1) * 4, 0:128], in_=P1[:, g4 * 512:(g4 + 1) * 512].rearrange("p (t c) -> p t c", t=4)), [], [f"P1_{g4}", f"Vaug_{g4}"])
                    qk = [f"QTn{q}" for q in range(4)] + [f"QTp{q}" for q in range(4)] + [f"KTn{q}" for q in range(4)] + [f"KTpe{q}" for q in range(4)]
                    vk = [f"Vaug_{g}" for g in range(4)] + ["Vaug1"]
                    for Gq in range(4):
                        its = []
                        for j in range(4 * Gq + 4):
                            qlo = max(Gq * 512, j * 128)
                            its.append((j, qlo, (Gq + 1) * 512 - qlo))
                        LA = 3

                        def emit_s(ii, its=its):
                            j, qlo, width = its[ii]
                            sbk = (ii + it0[0]) % 4

                            def mms(e):
                                e.matmul(P0[:, sbk * 512:sbk * 512 + width], lhsT=KTn[:, j * 128:(j + 1) * 128], rhs=QTn[:, qlo:qlo + width], start=True, stop=False)
                                return e.matmul(P0[:, sbk * 512:sbk * 512 + width], lhsT=KTpe[:, j * 128:(j + 1) * 128], rhs=QTp[:, qlo:qlo + width], start=False, stop=True)
                            P(mms, qk, [f"P0_{sbk}"])
                            A(lambda e: e.activation(out=PTs[sbk][:, 0:width], in_=P0[:, sbk * 512:sbk * 512 + width], func=AF.Exp), [], [f"P0_{sbk}", f"PTs{sbk}"])
                            if j >= 4 * Gq:
                                V(lambda e: e.tensor_tensor(out=PTs[sbk][:, 0:128], in0=PTs[sbk][:, 0:128], in1=triu[:], op=ALU.mult), ["triu", f"PTs{sbk}"], [f"PTs{sbk}"])

                        def emit_pv(ii, its=its, Gq=Gq):
                            j, qlo, width = its[ii]
                            sbk = (ii + it0[0]) % 4
                            qbs = list(range(qlo // 128, (Gq + 1) * 4))

                            def mmpv(e):
                                for qb in qbs:
                                    a = qb - 4 * Gq
                                    off = qb * 128 - qlo
                                    r = e.matmul(P1[:, a * 512:a * 512 + 129], lhsT=PTs[sbk][:, off:off + 128], rhs=Vaug[:, j, 0:129], start=(j == 0), stop=(j == qb))
                                return r
                            P(mmpv, [f"PTs{sbk}"] + vk, [f"P1_{qb - 4 * Gq}" for qb in qbs])

                        n_it = len(its)
                        for ii in range(min(LA, n_it)):
                            emit_s(ii)
                        for ii in range(n_it):
                            if ii + LA < n_it:
                                emit_s(ii + LA)
                            emit_pv(ii)
                        it0[0] += n_it
                        for a in range(4):
                            qb = 4 * Gq + a
                            V(lambda e, a=a: e.reciprocal(out=rec[:, a:a + 1], in_=P1[:, a * 512 + 128:a * 512 + 129]), [], [f"P1_{a}", f"rec{a}"])
                            V(lambda e, a=a, qb=qb, h=h: e.tensor_scalar(out=mla_tok[:, qb, h * 128:(h + 1) * 128], in0=P1[:, a * 512:a * 512 + 128], scalar1=rec[:, a:a + 1], scalar2=None, op0=ALU.mult), [f"rec{a}"], [f"P1_{a}", f"mla_tok{h}_{qb}"])
                mk.barrier()
                DM("sp", lambda e: e.dma_start(out=mix_d.ap().rearrange("(t p) c -> p t c", p=128)[:, :, 1024:2048], in_=mla_tok[:]), [], ["mix_mla"])
                if dbg and upto == 3:
                    dd = dbgout("mla", [128, NT, 1024], BF16)
                    DM("sp", lambda e: e.dma_start(out=dd.ap(), in_=mla_tok[:]), [], ["dbgm"])
                mk.flush()
        p13.close()

        if upto >= 4:
            with ExitStack() as pes:
                mk.pes = pes
                H8 = 8
                Uincl = mk.sb("Uincl", [128, 128], F32)
                offd = mk.sb("offd", [128, 128], F32)
                M2rep = mk.sb("M2rep", [128, 4, 128], F32)
                gdn = mk.sb("gdn", [128, 128], F32)
                Sst = mk.sb("Sst", [128, H8, 128], F32)
                ld = [[mk.sb(f"ld{n}{i}", [128, H8, 128], F32) for n in range(5)] for i in range(2)]
                gsmq = [mk.sb(f"gsm{i}", [128, 16], F32) for i in range(2)]
                smq = [mk.sb(f"sm{i}", [128, 6, 8], F32) for i in range(2)]
                rhsD = mk.sb("rhsD", [128, H8, 128], F32)
                E2 = mk.sb("E2", [128, H8, 128], F32)
                Amat = mk.sb("Amat", [128, H8, 128], F32)
                Atq = [mk.sb(f"At{i}", [128, H8, 128], F32) for i in range(2)]
                Pq = [mk.sb(f"Pq{i}", [128, H8, 128], F32) for i in range(2)]
                Ptq = [mk.sb(f"Ptq{i}", [128, H8, 128], F32) for i in range(2)]
                Xq0 = [mk.sb(f"Xq0{i}", [128, H8, 128], F32) for i in range(2)]
                Xq1 = mk.sb("Xq1", [128, H8, 128], F32)
                vbq = [mk.sb(f"vb{i}", [128, H8, 128], F32) for i in range(2)]
                kbgq = [mk.sb(f"kbg{i}", [128, H8, 128], F32) for i in range(2)]
                kdecq = [mk.sb(f"kdec{i}", [128, H8, 128], F32) for i in range(2)]
                nwT = mk.sb("nwT", [128, H8, 128], F32)
                vn = mk.sb("vn", [128, H8, 128], F32)
                osb = mk.sb("osb", [128, H8, 128], F32)
                sqo = mk.sb("sqo", [128, H8, 128], F32)
                mixdn = [mk.sb(f"mixdn{i}", [128, 1024], BF16) for i in range(2)]
                PA_ = mk.ps("PA_", [128, 1024], F32)
                PB_ = mk.ps("PB_", [128, 1024], F32)
                PC_ = mk.ps("PC_", [128, 1024], F32)
                PD_ = mk.ps("PD_", [128, 1024], F32)
                V(lambda e: e.tensor_single_scalar(out=Uincl[:], in_=dif[:], scalar=0.0, op=ALU.is_ge), ["dif"], ["Uincl"])
                V(lambda e: e.tensor_single_scalar(out=offd[:], in_=dif[:], scalar=0.0, op=ALU.not_equal), ["dif"], ["offd"])
                for a in range(4):
                    V(lambda e, a=a: e.tensor_single_scalar(out=M2rep[:, a, :], in_=dif[:], scalar=0.0, op=ALU.is_gt), ["dif"], [f"M2rep{a}"])
                    V(lambda e, a=a: e.tensor_single_scalar(out=M2rep[:, a, :], in_=M2rep[:, a, :], scalar=1.0e4, op=ALU.mult), [f"M2rep{a}"], [f"M2rep{a}"])
                M2k = [f"M2rep{a}" for a in range(4)]
                DM("sp", lambda e: e.dma_start(out=gdn[:], in_=dng_d.ap().partition_broadcast(128)), w=["gdn"])
                G(lambda e: e.memset(Sst[:], 0.0), w=["Sst"])
                F = H8 * 128

                def bc_col(t, Ft, col0):
                    return AP(t, Ft, col0, [[1, H8], [0, 128]])

                def bc_mat(t):
                    return AP(t, 128, 0, [[0, H8], [1, 128]])

                def flat(t):
                    return t[:].rearrange("p h c -> p (h c)")

                def ph(ps, h):
                    return ps[:, h * 128:(h + 1) * 128]

                def ps3(ps):
                    return ps[:].rearrange("p (h c) -> p h c", h=H8)

                def pk(name):
                    return [name + "0", name + "1"]

                def solve(c):
                    b = c % 2
                    qTc, kTc, ktok, vtok, zc = ld[b]
                    sm = smq[b]
                    gsm = gsmq[b]
                    At = Atq[b]
                    kbg = kbgq[b]
                    vb = vbq[b]
                    kdec = kdecq[b]
                    Xq = [Xq0[b], Xq1]
                    SM = f"sm{b}_"
                    sl = slice(c * 128, (c + 1) * 128)
                    DM("sp", lambda e: e.dma_start(out=qTc[:], in_=qT_d.ap().rearrange("h d s -> d h s")[:, :, sl]), [], [f"qTc{b}"])
                    DM("sp", lambda e: e.dma_start(out=kTc[:], in_=kT_d.ap().rearrange("h d s -> d h s")[:, :, sl]), [], [f"kTc{b}"])
                    DM("sp", lambda e: e.dma_start(out=ktok[:], in_=k_d.ap()[sl]), [], [f"ktok{b}"])
                    DM("sp", lambda e: e.dma_start(out=vtok[:], in_=v_d.ap()[sl]), [], [f"vtok{b}"])
                    DM("sp", lambda e: e.dma_start(out=flat(zc), in_=z_d.ap()[sl, :]), [], [f"zc{b}"])

                    def mm1(e):
                        e.matmul(PC_[:, 0:8], lhsT=Uincl[:], rhs=bg_sb[:, c, 8:16], start=True, stop=True)
                        return e.matmul(PC_[:, 8:16], lhsT=onesf[:], rhs=bg_sb[:, c, 8:16], start=True, stop=True)
                    P(mm1, ["Uincl", "onesf", "bg_g"], ["PC_0"])
                    V(lambda e: e.tensor_copy(out=gsm[:], in_=PC_[:, 0:16]), [], ["PC_0", f"gsm{b}"])
                    A(lambda e: e.activation(out=sm[:, 0, :], in_=gsm[:, 0:8], func=AF.Exp), [f"gsm{b}"], [SM + "0"])
                    A(lambda e: e.activation(out=sm[:, 1, :], in_=gsm[:, 8:16], func=AF.Exp), [f"gsm{b}"], [SM + "1"])
                    V(lambda e: e.tensor_tensor(out=sm[:, 2, :], in0=gsm[:, 8:16], in1=gsm[:, 0:8], op=ALU.subtract), [f"gsm{b}"], [SM + "2"])
                    A(lambda e: e.activation(out=sm[:, 2, :], in_=sm[:, 2, :], func=AF.Exp), [SM + "2"], [SM + "2"])
                    V(lambda e: e.tensor_tensor(out=sm[:, 3, :], in0=bg_sb[:, c, 0:8], in1=sm[:, 0, :], op=ALU.mult), ["bg_b", SM + "0"], [SM + "3"])
                    V(lambda e: e.tensor_single_scalar(out=sm[:, 4, :], in_=bg_sb[:, c, 0:8], scalar=-1.0, op=ALU.mult), ["bg_b"], [SM + "4"])
                    V(lambda e: e.tensor_tensor(out=rhsD[:], in0=bc_mat(identf), in1=bc_col(gsm, 16, 0), op=ALU.mult), ["identf", f"gsm{b}"], ["rhsD"])
                    yield
                    for hf in range(2):
                        def mm3(e, hf=hf):
                            e.matmul(PA_[:, hf * 512:(hf + 1) * 512], lhsT=onesf[:], rhs=flat(rhsD)[:, hf * 512:(hf + 1) * 512], start=True, stop=False)
                            return e.matmul(PA_[:, hf * 512:(hf + 1) * 512], lhsT=identf[:], rhs=M2rep[:].rearrange("p a c -> p (a c)"), start=False, stop=True)
                        P(mm3, ["onesf", "identf", "rhsD"] + M2k, [f"PA_{hf}"])
                    for h in range(H8):
                        A(lambda e, h=h: e.activation(out=E2[:, h, :], in_=ph(PA_, h), func=AF.Exp, bias=gsm[:, h:h + 1], scale=-1.0), [f"gsm{b}"], [f"PA_{h // 4}", f"E2_{h}"])
                    E2k = [f"E2_{h}" for h in range(H8)]
                    for hf in range(2):
                        def mm4(e, hf=hf):
                            for h in range(hf * 4, hf * 4 + 4):
                                r = e.matmul(ph(PB_, h), lhsT=qTc[:, h, :], rhs=kTc[:, h, :], start=True, stop=True)
                            return r
                        P(mm4, [f"qTc{b}", f"kTc{b}"], [f"PB_{hf}"])

                        def mm5(e, hf=hf):
                            for h in range(hf * 4, hf * 4 + 4):
                                r = e.matmul(ph(PC_, h), lhsT=kTc[:, h, :], rhs=kTc[:, h, :], start=True, stop=True)
                            return r
                        P(mm5, [f"kTc{b}"], [f"PC_{hf}"])
                    yield
                    V(lambda e: e.tensor_tensor(out=Amat[:], in0=ps3(PB_), in1=E2[:], op=ALU.mult), E2k, pk("PB_") + ["Amat"])
                    V(lambda e: e.tensor_tensor(out=Pq[0][:], in0=ps3(PC_), in1=bc_mat(offd), op=ALU.mult), ["offd"], pk("PC_") + ["Pq0"])
                    V(lambda e: e.tensor_tensor(out=Pq[0][:], in0=Pq[0][:], in1=E2[:], op=ALU.mult), E2k + ["Pq0"], ["Pq0"])
                    V(lambda e: e.tensor_tensor(out=Pq[0][:], in0=Pq[0][:], in1=AP(sm, 48, 32, [[1, H8], [0, 128]]), op=ALU.mult), [SM + "4", "Pq0"], ["Pq0"])
                    for hf in range(2):
                        def tr1(e, hf=hf):
                            for h in range(hf * 4, hf * 4 + 4):
                                r = e.transpose(ph(PA_, h), Amat[:, h, :], identf[:])
                            return r
                        P(tr1, ["Amat", "identf"], [f"PA_{hf}"])

                        def tr2(e, hf=hf):
                            for h in range(hf * 4, hf * 4 + 4):
                                r = e.transpose(ph(PB_, h), Pq[0][:, h, :], identf[:])
                            return r
                        P(tr2, ["Pq0", "identf"], [f"PB_{hf}"])
                    yield
                    A(lambda e: e.copy(out=flat(At), in_=PA_[:]), [], pk("PA_") + [f"At{b}"])
                    V(lambda e: e.tensor_copy(out=flat(Ptq[0]), in_=PB_[:]), [], pk("PB_") + ["Ptq0"])
                    V(lambda e: e.tensor_tensor(out=Xq[0][:], in0=Ptq[0][:], in1=bc_mat(identf), op=ALU.add), ["Ptq0", "identf"], [f"Xq0{b}"])
                    xkey = [f"Xq0{b}", "Xq1"]
                    G(lambda e: e.tensor_tensor(out=vb[:], in0=vtok[:], in1=AP(bg_sb, NT * 16, c * 16, [[1, H8], [0, 128]]), op=ALU.mult), [f"vtok{b}", "bg_b"], [f"vb{b}"])
                    G(lambda e: e.tensor_tensor(out=kbg[:], in0=ktok[:], in1=AP(sm, 48, 24, [[1, H8], [0, 128]]), op=ALU.mult), [f"ktok{b}", SM + "3"], [f"kbg{b}"])
                    G(lambda e: e.tensor_tensor(out=kdec[:], in0=ktok[:], in1=AP(sm, 48, 16, [[1, H8], [0, 128]]), op=ALU.mult), [f"ktok{b}", SM + "2"], [f"kdec{b}"])
                    cur = 0
                    for kk in range(1, 7):
                        nx = 1 - cur
                        for hf in range(2):
                            def d1(e, hf=hf, cur=cur):
                                for h in range(hf * 4, hf * 4 + 4):
                                    r = e.matmul(ph(PA_, h), lhsT=Ptq[cur][:, h, :], rhs=Pq[cur][:, h, :], start=True, stop=True)
                                return r
                            P(d1, [f"Ptq{cur}", f"Pq{cur}"], [f"PA_{hf}"])
                        if kk < 6:
                            for hf in range(2):
                                def d2(e, hf=hf, cur=cur):
                                    for h in range(hf * 4, hf * 4 + 4):
                                        r = e.matmul(ph(PB_, h), lhsT=Pq[cur][:, h, :], rhs=Ptq[cur][:, h, :], start=True, stop=True)
                                    return r
                                P(d2, [f"Ptq{cur}", f"Pq{cur}"], [f"PB_{hf}"])
                        A(lambda e, nx=nx: e.copy(out=flat(Pq[nx]), in_=PA_[:]), [], pk("PA_") + [f"Pq{nx}"])
                        if kk < 6:
                            V(lambda e, nx=nx: e.tensor_copy(out=flat(Ptq[nx]), in_=PB_[:]), [], pk("PB_") + [f"Ptq{nx}"])
                        yield
                        for hf in range(2):
                            def d3(e, hf=hf, cur=cur, nx=nx):
                                for h in range(hf * 4, hf * 4 + 4):
                                    r = e.matmul(ph(PC_, h), lhsT=Pq[nx][:, h, :], rhs=Xq[cur][:, h, :], start=True, stop=True)
                                return r
                            P(d3, [f"Pq{nx}", xkey[cur]], [f"PC_{hf}"])
                        V(lambda e, cur=cur, nx=nx: e.tensor_tensor(out=flat(Xq[nx]), in0=PC_[:], in1=flat(Xq[cur]), op=ALU.add), [xkey[cur]], pk("PC_") + [xkey[nx]])
                        cur = nx
                        yield
                    assert cur == 0

                def apply(c):
                    b = c % 2
                    qTc, kTc, ktok, vtok, zc = ld[b]
                    sm = smq[b]
                    At = Atq[b]
                    kbg = kbgq[b]
                    vb = vbq[b]
                    kdec = kdecq[b]
                    X = Xq0[b]
                    Xk = f"Xq0{b}"
                    SM = f"sm{b}_"
                    sl = slice(c * 128, (c + 1) * 128)
                    for hf in range(2):
                        def w1(e, hf=hf):
                            for h in range(hf * 4, hf * 4 + 4):
                                r = e.matmul(ph(PD_, h), lhsT=kbg[:, h, :], rhs=X[:, h, :], start=True, stop=True)
                            return r
                        P(w1, [f"kbg{b}", Xk], [f"PD_{hf}"])
                    A(lambda e: e.mul(out=flat(nwT), in_=PD_[:], mul=-1.0), [], pk("PD_") + ["nwT"])
                    yield
                    for hf in range(2):
                        def w2(e, hf=hf):
                            for h in range(hf * 4, hf * 4 + 4):
                                e.matmul(ph(PD_, h), lhsT=X[:, h, :], rhs=vb[:, h, :], start=True, stop=False)
                                r = e.matmul(ph(PD_, h), lhsT=nwT[:, h, :], rhs=Sst[:, h, :], start=False, stop=True)
                            return r
                        P(w2, [f"vb{b}", Xk, "nwT", "Sst"], [f"PD_{hf}"])
                    V(lambda e: e.tensor_copy(out=flat(vn), in_=PD_[:]), [], pk("PD_") + ["vn"])
                    yield
                    for hf in range(2):
                        def o1(e, hf=hf):
                            for h in range(hf * 4, hf * 4 + 4):
                                r = e.matmul(ph(PD_, h), lhsT=qTc[:, h, :], rhs=Sst[:, h, :], start=True, stop=True)
                            return r
                        P(o1, [f"qTc{b}", "Sst"], [f"PD_{hf}"])
                    V(lambda e: e.tensor_tensor(out=osb[:], in0=ps3(PD_), in1=AP(sm, 48, 0, [[1, H8], [0, 128]]), op=ALU.mult), [SM + "0"], pk("PD_") + ["osb"])
                    yield
                    for hf in range(2):
                        def o2(e, hf=hf):
                            for h in range(hf * 4, hf * 4 + 4):
                                r = e.matmul(ph(PD_, h), lhsT=At[:, h, :], rhs=vn[:, h, :], start=True, stop=True)
                            return r
                        P(o2, [f"At{b}", "vn"], [f"PD_{hf}"])
                    V(lambda e: e.tensor_tensor(out=flat(osb), in0=PD_[:], in1=flat(osb), op=ALU.add), ["osb"], pk("PD_") + ["osb"])
                    yield
                    for hf in range(2):
                        def s1(e, hf=hf):
                            for h in range(hf * 4, hf * 4 + 4):
                                r = e.matmul(ph(PD_, h), lhsT=kdec[:, h, :], rhs=vn[:, h, :], start=True, stop=True)
                            return r
                        P(s1, [f"kdec{b}", "vn"], [f"PD_{hf}"])
                    V(lambda e: e.tensor_tensor(out=Sst[:], in0=Sst[:], in1=AP(sm, 48, 8, [[1, H8], [0, 128]]), op=ALU.mult), ["Sst", SM + "1"], ["Sst"])
                    V(lambda e: e.tensor_tensor(out=flat(Sst), in0=PD_[:], in1=flat(Sst), op=ALU.add), ["Sst"], pk("PD_") + ["Sst"])
                    yield
                    G(lambda e: e.tensor_tensor(out=sqo[:], in0=osb[:], in1=osb[:], op=ALU.mult), ["osb"], ["sqo"])
                    V(lambda e: e.reduce_sum(out=sm[:, 5, :], in_=sqo[:], axis=AX.X), ["sqo"], [SM + "5"])
                    A(lambda e: e.activation(out=sm[:, 5, :], in_=sm[:, 5, :], func=AF.Sqrt, bias=EPS, scale=1.0 / 128), [SM + "5"], [SM + "5"])
                    V(lambda e: e.reciprocal(out=sm[:, 5, :], in_=sm[:, 5, :]), [SM + "5"], [SM + "5"])
                    G(lambda e: e.tensor_tensor(out=sqo[:], in0=osb[:], in1=AP(sm, 48, 40, [[1, H8], [0, 128]]), op=ALU.mult), ["osb", SM + "5", "sqo"], ["sqo"])
                    G(lambda e: e.tensor_tensor(out=sqo[:], in0=sqo[:], in1=bc_mat(gdn), op=ALU.mult), ["sqo", "gdn"], ["sqo"])
                    G(lambda e: e.tensor_tensor(out=mixdn[b][:], in0=flat(sqo), in1=flat(zc), op=ALU.mult), ["sqo", f"zc{b}"], [f"mixdn{b}"])
                    DM("sp", lambda e: e.dma_start(out=mix_d.ap()[sl, 0:1024], in_=mixdn[b][:]), [f"mixdn{b}"], [f"mix_dn{c}"])
                    yield

                def interleave(gens):
                    gens = [g for g in gens if g is not None]
                    while gens:
                        for g in list(gens):
                            try:
                                next(g)
                            except StopIteration:
                                gens.remove(g)

                interleave([solve(0)])
                for c in range(NT):
                    interleave([solve(c + 1) if c + 1 < NT else None, apply(c)])
                if dbg and upto == 4:
                    mk.barrier()
                    dd = dbgout("mix", [S, D], BF16)
                    DM("sp", lambda e: e.dma_start(out=dd.ap(), in_=mix_d.ap()), [], ["dbgmix"])
                mk.flush()

        if upto >= 5:
            p57 = ExitStack()
            es.enter_context(p57)
            mk.pes = p57
            logits = mk.sb("logits", [128, NT, 64], F32)
            dest_i = mk.sb("dest_i", [128, NT, 8], I32)
            BEi4 = mk.sb("BEi4", [128, NBLK, 4], I32)
            BEi2 = mk.sb("BEi2", [128, NBLK, 2], I32)
            with ExitStack() as pes:
                mk.pes = pes
                wout = mk.sb("wout", [128, 16, D], BF16)
                wr = mk.sb("wr", [128, 16, 64], BF16)
                g1b = mk.sb("g1b", [128, D], F32)
                G2b = mk.sb("G2b", [128, D], F32)
                S2b = mk.sb("S2b", [128, D], F32)
                xt = [mk.sb(f"xt5{i}", [128, D], F32) for i in range(2)]
                mixt = [mk.sb(f"mixt{i}", [128, D], BF16) for i in range(2)]
                mixT = [mk.sb(f"mixT{i}", [128, 16, 128], BF16) for i in range(2)]
                h2T = mk.sb("h2T", [128, 16, 128], BF16)
                tmpf = mk.sb("tmpf5", [128, D], F32)
                tmp2 = mk.sb("tmp25", [128, D], F32)
                x1t = mk.sb("x1t", [128, D], F32)
                h2t = [mk.sb(f"h2t{i}", [128, D], BF16) for i in range(2)]
                junk = mk.sb("junk5", [128, D], BF16)
                st = mk.sb("st5", [128, NT, 2], F32)
                PT = [mk.ps(f"PT5{i}", [128, 1024], BF16) for i in range(2)]
                PO = mk.ps("PO5", [128, D], F32)
                PR = mk.ps("PR5", [128, 64], F32)
                PT2 = [mk.ps("PT52", [128, 1024], BF16), None]
                PT2[1] = PT2[0]
                for n in range(4):
                    DM("pool", lambda e, n=n: e.dma_start(out=wout[:, :, n * 512:(n + 1) * 512], in_=wout_d.ap().rearrange("(k p) n -> p k n", p=128)[:, :, n * 512:(n + 1) * 512]), w=[f"wout{n}"])
                DM("pool", lambda e: e.dma_start(out=wr[:], in_=wr_d.ap().rearrange("(k p) n -> p k n", p=128)), w=["wr"])
                DM("sp", lambda e: e.dma_start(out=g1b[:], in_=ada_d.ap()[2:3, :].partition_broadcast(128)), w=["g1b"])
                DM("sp", lambda e: e.dma_start(out=S2b[:], in_=ada_d.ap()[3:4, :].partition_broadcast(128)), w=["S2b"])
                DM("sp", lambda e: e.dma_start(out=G2b[:], in_=ada_d.ap()[4:5, :].partition_broadcast(128)), w=["G2b"])
                G(lambda e: e.memset(junk[:], 0.0), w=["junk5"])
                DM("sp", lambda e: e.dma_start(out=h2_d.ap()[S:S + 128, :], in_=junk[:]), ["junk5"], ["h2pad"])
                woutk = [f"wout{n}" for n in range(4)]

                def front(t):
                    b = t % 2
                    sl = slice(t * 128, (t + 1) * 128)
                    DM("sp", lambda e: e.dma_start(out=mixt[b][:], in_=mix_d.ap()[sl, :]), w=[f"mixt{b}"])
                    DM("sp", lambda e: e.dma_start(out=xt[b][:], in_=x_d.ap()[sl, :]), w=[f"xt5{b}"])
                    for hh in range(2):
                        def tr(e, hh=hh):
                            for k in range(8):
                                kk = hh * 8 + k
                                r = e.transpose(PT[hh][:, k * 128:(k + 1) * 128], mixt[b][:, kk * 128:(kk + 1) * 128], identb[:])
                            return r
                        P(tr, [f"mixt{b}", "identb"], [f"PT5{hh}"])
                        if hh == 0:
                            V(lambda e, hh=hh: e.tensor_copy(out=mixT[b][:, hh * 8:(hh + 1) * 8, :], in_=PT[hh][:].rearrange("p (k c) -> p k c", k=8)), [], [f"PT5{hh}", f"mixT{b}_{hh}"])
                        else:
                            A(lambda e, hh=hh: e.copy(out=mixT[b][:, hh * 8:(hh + 1) * 8, :], in_=PT[hh][:].rearrange("p (k c) -> p k c", k=8)), [], [f"PT5{hh}", f"mixT{b}_{hh}"])
                    yield
                    for n in range(4):
                        def mm(e, n=n):
                            for k in range(16):
                                r = e.matmul(PO[:, n * 512:(n + 1) * 512], lhsT=mixT[b][:, k, :], rhs=wout[:, k, n * 512:(n + 1) * 512], start=(k == 0), stop=(k == 15))
                            return r
                        P(mm, [f"mixT{b}_0", f"mixT{b}_1"] + woutk, [f"PO{n}"])
                        V(lambda e, n=n: e.tensor_tensor(out=tmpf[:, n * 512:(n + 1) * 512], in0=PO[:, n * 512:(n + 1) * 512], in1=g1b[:, n * 512:(n + 1) * 512], op=ALU.mult), ["g1b"], [f"PO{n}", f"tmpf5_{n}"])
                        yield

                def back(t):
                    b = t % 2
                    sl = slice(t * 128, (t + 1) * 128)
                    tk = [f"tmpf5_{n}" for n in range(4)]
                    V(lambda e: e.tensor_tensor(out=x1t[:], in0=tmpf[:], in1=xt[b][:], op=ALU.add), tk + [f"xt5{b}"], ["x1t"])
                    DM("sp", lambda e: e.dma_start(out=x1_d.ap()[sl, :], in_=x1t[:]), ["x1t"], [f"x1_d{t}"])
                    A(lambda e: e.activation(out=junk[:], in_=x1t[:], func=AF.Square, accum_out=st[:, t, 0:1]), ["x1t"], ["junk5", f"st5{t}"])
                    A(lambda e: e.activation(out=st[:, t, 1:2], in_=st[:, t, 0:1], func=AF.Sqrt, bias=EPS, scale=1.0 / D), [f"st5{t}"], [f"st5{t}"])
                    V(lambda e: e.reciprocal(out=st[:, t, 1:2], in_=st[:, t, 1:2]), [f"st5{t}"], [f"st5{t}"])
                    yield
                    V(lambda e: e.scalar_tensor_tensor(out=tmp2[:], in0=x1t[:], scalar=st[:, t, 1:2], in1=G2b[:], op0=ALU.mult, op1=ALU.mult), ["x1t", f"st5{t}", "G2b"], ["tmp2"])
                    V(lambda e: e.tensor_tensor(out=h2t[b][:], in0=tmp2[:], in1=S2b[:], op=ALU.add), ["tmp2", "S2b"], [f"h2t{b}"])
                    DM("sp", lambda e: e.dma_start(out=h2_d.ap()[sl, :], in_=h2t[b][:]), [f"h2t{b}"], [f"h2_d{t}"])
                    yield
                    for hh in range(2):
                        def tr2(e, hh=hh):
                            for k in range(8):
                                kk = hh * 8 + k
                                r = e.transpose(PT2[hh][:, k * 128:(k + 1) * 128], h2t[b][:, kk * 128:(kk + 1) * 128], identb[:])
                            return r
                        P(tr2, [f"h2t{b}", "identb"], ["PT52"])
                        if hh == 0:
                            V(lambda e, hh=hh: e.tensor_copy(out=h2T[:, hh * 8:(hh + 1) * 8, :], in_=PT2[hh][:].rearrange("p (k c) -> p k c", k=8)), [], ["PT52", f"h2T{hh}"])
                        else:
                            A(lambda e, hh=hh: e.copy(out=h2T[:, hh * 8:(hh + 1) * 8, :], in_=PT2[hh][:].rearrange("p (k c) -> p k c", k=8)), [], ["PT52", f"h2T{hh}"])
                    yield

                    def mmr(e):
                        for k in range(16):
                            r = e.matmul(PR[:], lhsT=h2T[:, k, :], rhs=wr[:, k, :], start=(k == 0), stop=(k == 15))
                        return r
                    P(mmr, ["h2T0", "h2T1", "wr"], ["PR5"])
                    V(lambda e: e.tensor_copy(out=logits[:, t, :], in_=PR[:]), [], ["PR5", f"logits{t}"])
                    yield

                def interleave5(gens):
                    gens = [g for g in gens if g is not None]
                    while gens:
                        for g in list(gens):
                            try:
                                next(g)
                            except StopIteration:
                                gens.remove(g)

                interleave5([front(0)])
                for t in range(NT):
                    interleave5([front(t + 1) if t + 1 < NT else None, back(t)])
                mk.flush()
            with ExitStack() as pes:
                mk.pes = pes
                NG = NT * 8
                scores = mk.sb("scores", [128, NT, 64], F32)
                biased = mk.sb("biased", [128, NT, 64], F32)
                tmpA = mk.sb("tmpA", [128, NT, 64], F32)
                mb = mk.sb("mb", [128, NT, 64], F32)
                sel = mk.sb("sel", [128, NT, 64], F32)
                wn = mk.sb("wn", [128, NT, 64], F32)
                pos = mk.sb("pos", [128, NT, 64], F32)
                rb = mk.sb("rb", [128, 64], F32)
                m1 = mk.sb("m1", [128, NG], F32)
                m2 = mk.sb("m2", [128, NG], F32)
                t8 = mk.sb("t8", [128, NT, 8], F32)
                v8 = mk.sb("v8", [128, NT, 8], F32)
                den = mk.sb("den", [128, NT], F32)
                selsum = mk.sb("selsum", [128, 64], F32)
                Ustr = mk.sb("Ustr", [128, 128], F32)
                cA = mk.sb("cA", [128, 64], F32)
                cB = mk.sb("cB", [128, 64], F32)
                nbf = mk.sb("nbf", [128, 64], F32)
                nbi = mk.sb("nbi", [128, 64], I32)
                rowbase = mk.sb("rowbase", [128, 64], F32)
                d6 = mk.sb("d6", [128, NT, 8], F32)
                meta = mk.sb("meta", [128, NT, 8, 2], F32)
                junk64 = mk.sb("junk64", [128, 64], F32)
                tok_i = mk.sb("tok_i", [128, NT], I32)
                tokf = mk.sb("tokf", [128, NT], F32)
                bidx_i = mk.sb("bidx_i", [128, NBLK], I32)
                bidxf = mk.sb("bidxf", [128, NBLK], F32)
                cmp = mk.sb("cmp", [128, NBLK, 64], F32)
                BEf = mk.sb("BEf", [128, NBLK], F32)
                BE4f = mk.sb("BE4f", [128, NBLK, 4], F32)
                rminit = mk.sb("rminit", [128, NROWS // 128, 2], F32)
                Ppos = mk.ps("Ppos", [128, 64], F32)
                DM("sp", lambda e: e.dma_start(out=rb[:], in_=rb_d.ap().partition_broadcast(128)), w=["rb"])
                G(lambda e: e.memset(rminit[:], 0.0), w=["rminit"])
                G(lambda e: e.memset(rminit[:, :, 0:1], float(S)), ["rminit"], ["rminit"])
                DM("sp", lambda e: e.dma_start(out=rm_d.ap().rearrange("(p a) c -> p a c", p=128), in_=rminit[:]), ["rminit"], ["rm_init"])
                G(lambda e: e.memset(d6[:], 0.0), w=["d6"])
                G(lambda e: e.memset(meta[:], 0.0), w=["meta"])
                G(lambda e: e.iota(tok_i[:], [[128, NT]], base=0, channel_multiplier=1), w=["tok_i"])
                G(lambda e: e.iota(bidx_i[:], [[1, NBLK]], base=0, channel_multiplier=0), w=["bidx_i"])
                V(lambda e: e.tensor_copy(out=tokf[:], in_=tok_i[:]), ["tok_i"], ["tokf"])
                V(lambda e: e.tensor_copy(out=bidxf[:], in_=bidx_i[:]), ["bidx_i"], ["bidxf"])
                V(lambda e: e.tensor_single_scalar(out=Ustr[:], in_=dif[:], scalar=0.0, op=ALU.is_gt), ["dif"], ["Ustr"])
                lk = [f"logits{t}" for t in range(NT)]
                fl = lambda t_: t_[:].rearrange("p t e -> p (t e)")
                g3 = lambda t_: t_[:].rearrange("p t (g e) -> p (t g) e", e=8)
                A(lambda e: e.activation(out=fl(scores), in_=fl(logits), func=AF.Sigmoid), lk, ["scores"])
                V(lambda e: e.tensor_tensor(out=biased[:], in0=scores[:], in1=AP(rb, 64, 0, [[0, NT], [1, 64]]), op=ALU.add), ["scores", "rb"], ["biased"])
                V(lambda e: e.reduce_max(out=m1[:], in_=g3(biased), axis=AX.X), ["biased"], ["m1"])
                V(lambda e: e.tensor_tensor(out=g3(tmpA), in0=g3(biased), in1=AP(m1, NG, 0, [[1, NG], [0, 8]]), op=ALU.is_equal), ["biased", "m1"], ["tmpA"])
                V(lambda e: e.scalar_tensor_tensor(out=fl(tmpA), in0=fl(tmpA), scalar=-1.0e9, in1=fl(biased), op0=ALU.mult, op1=ALU.add), ["tmpA", "biased"], ["tmpA"])
                V(lambda e: e.reduce_max(out=m2[:], in_=g3(tmpA), axis=AX.X), ["tmpA"], ["m2"])
                V(lambda e: e.tensor_tensor(out=m1[:], in0=m1[:], in1=m2[:], op=ALU.add), ["m1", "m2"], ["m1"])
                for t in range(NT):
                    V(lambda e, t=t: e.max(out=t8[:, t, :], in_=m1[:, t * 8:(t + 1) * 8]), ["m1"], [f"t8_{t}"])
                t8k = [f"t8_{t}" for t in range(NT)]
                V(lambda e: e.tensor_tensor(out=m2[:].rearrange("p (t g) -> p t g", g=8), in0=m1[:].rearrange("p (t g) -> p t g", g=8), in1=AP(t8, NT * 8, 3, [[8, NT], [0, 8]]), op=ALU.is_ge), ["m1", "m2"] + t8k, ["m2"])
                V(lambda e: e.tensor_scalar(out=m2[:], in0=m2[:], scalar1=1.0e9, scalar2=-1.0e9, op0=ALU.mult, op1=ALU.add), ["m2"], ["m2"])
                V(lambda e: e.tensor_tensor(out=g3(mb), in0=g3(biased), in1=AP(m2, NG, 0, [[1, NG], [0, 8]]), op=ALU.add), ["biased", "m2"], ["mb"])
                for t in range(NT):
                    V(lambda e, t=t: e.max(out=v8[:, t, :], in_=mb[:, t, :]), ["mb"], [f"v8_{t}"])
                v8k = [f"v8_{t}" for t in range(NT)]
                V(lambda e: e.tensor_tensor(out=sel[:], in0=mb[:], in1=AP(v8, NT * 8, 5, [[8, NT], [0, 64]]), op=ALU.is_ge), ["mb"] + v8k, ["sel"])
                V(lambda e: e.tensor_tensor(out=wn[:], in0=sel[:], in1=scores[:], op=ALU.mult), ["sel", "scores"], ["wn"])
                V(lambda e: e.reduce_sum(out=den[:], in_=wn[:], axis=AX.X), ["wn"], ["den"])
                V(lambda e: e.reciprocal(out=den[:], in_=den[:]), ["den"], ["den"])
                V(lambda e: e.tensor_single_scalar(out=den[:], in_=den[:], scalar=2.5, op=ALU.mult), ["den"], ["den"])
                V(lambda e: e.tensor_tensor(out=wn[:], in0=wn[:], in1=AP(den, NT, 0, [[1, NT], [0, 64]]), op=ALU.mult), ["wn", "den"], ["wn"])
                for t in range(NT):
                    def mp(e, t=t):
                        r = e.matmul(Ppos[:], lhsT=Ustr[:], rhs=sel[:, t, :], start=True, stop=(t == 0))
                        if t > 0:
                            r = e.matmul(Ppos[:], lhsT=onesf[:], rhs=selsum[:], start=False, stop=True)
                        return r
                    P(mp, ["Ustr", "onesf", "sel", "selsum"], ["Ppos"])
                    V(lambda e, t=t: e.tensor_copy(out=pos[:, t, :], in_=Ppos[:]), [], ["Ppos", f"pos{t}"])
                    if t == 0:
                        V(lambda e: e.tensor_copy(out=selsum[:], in_=sel[:, 0, :]), ["sel"], ["selsum"])
                    else:
                        V(lambda e, t=t: e.tensor_tensor(out=selsum[:], in0=selsum[:], in1=sel[:, t, :], op=ALU.add), ["sel", "selsum"], ["selsum"])
                P(lambda e: e.matmul(Ppos[:], lhsT=onesf[:], rhs=selsum[:], start=True, stop=True), ["onesf", "selsum"], ["Ppos"])
                V(lambda e: e.tensor_scalar(out=nbf[:], in0=Ppos[:], scalar1=float(BS - 1), scalar2=1.0 / BS, op0=ALU.add, op1=ALU.mult), [], ["Ppos", "nbf"])
                V(lambda e: e.tensor_single_scalar(out=nbf[:], in_=nbf[:], scalar=-0.5 + 1.0 / (2 * BS), op=ALU.add), ["nbf"], ["nbf"])
                V(lambda e: e.tensor_copy(out=nbi[:], in_=nbf[:]), ["nbf"], ["nbi"])
                V(lambda e: e.tensor_copy(out=nbf[:], in_=nbi[:]), ["nbi"], ["nbf"])
                V(lambda e: e.tensor_copy(out=cA[:], in_=nbf[:]), ["nbf"], ["cA"])
                ca, cb, can, cbn = cA, cB, "cA", "cB"
                for sft in (1, 2, 4, 8, 16, 32):
                    V(lambda e, ca=ca, cb=cb, sft=sft: e.tensor_copy(out=cb[:, 0:sft], in_=ca[:, 0:sft]), [can], [cbn])
                    V(lambda e, ca=ca, cb=cb, sft=sft: e.tensor_tensor(out=cb[:, sft:64], in0=ca[:, sft:64], in1=ca[:, 0:64 - sft], op=ALU.add), [can, cbn], [cbn])
                    ca, cb, can, cbn = cb, ca, cbn, can
                bsincl, bsk = ca, can
                V(lambda e: e.tensor_tensor(out=rowbase[:], in0=bsincl[:], in1=nbf[:], op=ALU.subtract), [bsk, "nbf"], ["rowbase"])
                V(lambda e: e.tensor_single_scalar(out=rowbase[:], in_=rowbase[:], scalar=float(BS), op=ALU.mult), ["rowbase"], ["rowbase"])
                posk = [f"pos{t}" for t in range(NT)]
                V(lambda e: e.tensor_tensor(out=pos[:], in0=pos[:], in1=AP(rowbase, 64, 0, [[0, NT], [1, 64]]), op=ALU.add), posk + ["rowbase"], ["posall"])
                for t in range(NT):
                    for k in range(6):
                        V(lambda e, t=t, k=k: e.scalar_tensor_tensor(out=junk64[:], in0=mb[:, t, :], scalar=v8[:, t, k:k + 1], in1=pos[:, t, :], op0=ALU.is_equal, op1=ALU.mult, accum_out=d6[:, t, k:k + 1]),
                          ["mb", "posall", "d6"] + v8k, ["junk64", f"d6_{t}_{k}"])
                        V(lambda e, t=t, k=k: e.scalar_tensor_tensor(out=junk64[:], in0=mb[:, t, :], scalar=v8[:, t, k:k + 1], in1=wn[:, t, :], op0=ALU.is_equal, op1=ALU.mult, accum_out=meta[:, t, k, 1:2]),
                          ["mb", "wn", "meta"] + v8k, ["junk64", f"meta_{t}_{k}"])
                d6k = [f"d6_{t}_{k}" for t in range(NT) for k in range(6)]
                mtk = [f"meta_{t}_{k}" for t in range(NT) for k in range(6)]
                V(lambda e: e.tensor_copy(out=dest_i[:], in_=d6[:]), d6k + ["d6"], ["dest_i"])
                V(lambda e: e.tensor_copy(out=meta[:, :, :, 0], in_=AP(tokf, NT, 0, [[1, NT], [0, 8]])), ["tokf", "meta"] + mtk, ["meta_tok"])
                for t in range(NT):
                    for k in range(6):
                        DM("pool", lambda e, t=t, k=k: e.indirect_dma_start(out=rm_d.ap(), out_offset=bass.IndirectOffsetOnAxis(ap=dest_i[:, t, k:k + 1], axis=0), in_=meta[:, t, k, :], in_offset=None),
                           ["dest_i", "meta_tok", "rm_init"] + mtk, [f"rm_{t}_{k}"])
                V(lambda e: e.tensor_tensor(out=cmp[:], in0=AP(bidxf, NBLK, 0, [[1, NBLK], [0, 64]]), in1=AP(bsincl, 64, 0, [[0, NBLK], [1, 64]]), op=ALU.is_ge), ["bidxf", bsk], ["cmp"])
                V(lambda e: e.reduce_sum(out=BEf[:], in_=cmp[:], axis=AX.X), ["cmp"], ["BEf"])
                V(lambda e: e.tensor_single_scalar(out=bidxf[:], in_=BEf[:], scalar=64.0, op=ALU.is_ge), ["BEf", "cmp"], ["bidxf"])
                V(lambda e: e.scalar_tensor_tensor(out=BEf[:], in0=bidxf[:], scalar=1000.0, in1=BEf[:], op0=ALU.mult, op1=ALU.add), ["bidxf", "BEf"], ["BEf"])
                V(lambda e: e.tensor_scalar(out=BEf[:], in0=BEf[:], scalar1=128.0, scalar2=tokf[:, 0:1], op0=ALU.mult, op1=ALU.add), ["BEf", "tokf"], ["BEf"])
                for j in range(4):
                    V(lambda e, j=j: e.tensor_scalar(out=BE4f[:, :, j], in0=BEf[:], scalar1=4.0, scalar2=float(j), op0=ALU.mult, op1=ALU.add), ["BEf"], [f"BE4f{j}"])
                V(lambda e: e.tensor_copy(out=BEi4[:], in_=BE4f[:]), [f"BE4f{j}" for j in range(4)], ["BEi4"])
                for j in range(2):
                    V(lambda e, j=j: e.tensor_scalar(out=BE4f[:, :, j], in0=BEf[:], scalar1=2.0, scalar2=float(j), op0=ALU.mult, op1=ALU.add), ["BEf", "BEi4"], [f"BE4f{j}"])
                V(lambda e: e.tensor_copy(out=BEi2[:], in_=BE4f[:, :, 0:2]), ["BE4f0", "BE4f1"], ["BEi2"])
                if dbg and upto == 5:
                    mk.barrier()
                    for nm, src, shp, dty in (("x1", x1_d, [S, D], F32), ("h2", h2_d, [S + 128, D], BF16), ("rm", rm_d, [NROWS, 2], F32)):
                        dd = dbgout(nm, shp, dty)
                        DM("sp", lambda e, dd=dd, src=src: e.dma_start(out=dd.ap(), in_=src.ap()), [], ["dbg_" + nm])
                    for nm, src, shp, dty in (("logits", logits, [128, NT, 64], F32), ("sel", sel, [128, NT, 64], F32), ("wn", wn, [128, NT, 64], F32), ("dest", dest_i, [128, NT, 8], I32), ("BEi4", BEi4, [128, NBLK, 4], I32)):
                        dd = dbgout(nm, shp, dty)
                        DM("sp", lambda e, dd=dd, src=src: e.dma_start(out=dd.ap(), in_=src[:]), [], ["dbg_" + nm])
                mk.flush()

        if upto >= 6:
            with ExitStack() as pes:
                mk.pes = pes
                NST = 4
                NTOT = NBLK + 8
                stg = [mk.sb(f"stg{i}", [128, 4096], F32) for i in range(NST)]
                wgu = [mk.sb(f"wgu{i}", [128, 16, 1024], BF16) for i in range(2)]
                wd = [mk.sb(f"wd{i}", [128, 4, 2048], BF16) for i in range(2)]
                rmt = [mk.sb(f"rmt{i}", [128, 2, 2], F32) for i in range(2)]
                toki = [mk.sb(f"toki{i}", [128, 2], I32) for i in range(2)]
                hg = [mk.sb(f"hg{i}", [128, D], BF16) for i in range(4)]
                hgT = mk.sb("hgT", [128, 16, BS], BF16)
                gsb = [mk.sb(f"gsb{i}", [128, BS], F32) for i in range(2)]
                actT = mk.sb("actT", [128, 4, BS], BF16)
                ysb = [mk.sb(f"ysb{i}", [128, 1024], BF16) for i in range(2)]
                PT = [mk.ps(f"PT6{i}", [128, 1024], BF16) for i in range(2)]
                PG = [mk.ps(f"PG6{i}", [128, 512], F32) for i in range(2)]
                PU = [mk.ps(f"PU6{i}", [128, 512], F32) for i in range(2)]
                PY = [mk.ps(f"PY6{i}", [128, 512], F32) for i in range(2)]
                state = {"sti": 0, "yi": 0, "ys": 0}

                order = []
                for i in range(NBLK // 2):
                    order += [i, NBLK - 1 - i]
                order += list(range(NBLK, NTOT))
                SHP = NBLK % 2

                def wb_of(pos):
                    return pos % 2 if pos < NBLK else SHP

                def rows(pos):
                    blk = order[pos]
                    mb_ = pos % 2
                    if blk < NBLK:
                        DM("sp", lambda e: e.dma_start(out=rmt[mb_][:], in_=rm_d.ap()[blk * BS:(blk + 1) * BS, :].rearrange("(s p) c -> p s c", p=128)), [], [f"rmt{mb_}"])
                        V(lambda e: e.tensor_copy(out=toki[mb_][:], in_=rmt[mb_][:, :, 0]), [f"rmt{mb_}"], [f"toki{mb_}"])
                    else:
                        G(lambda e: e.memset(rmt[mb_][:], 1.0), [], [f"rmt{mb_}"])
                    for sbi in range(2):
                        hi = mb_ * 2 + sbi
                        if blk < NBLK:
                            DM("pool", lambda e, sbi=sbi, hi=hi: e.indirect_dma_start(out=hg[hi][:], out_offset=None, in_=h2_d.ap(), in_offset=bass.IndirectOffsetOnAxis(ap=toki[mb_][:, sbi:sbi + 1], axis=0)),
                               [f"toki{mb_}"], [f"hg{hi}"])
                        else:
                            r0 = (blk - NBLK) * BS + sbi * 128
                            DM("sp", lambda e, hi=hi, r0=r0: e.dma_start(out=hg[hi][:], in_=h2_d.ap()[r0:r0 + 128, :]), [], [f"hg{hi}"])

                def wdma(pos, j):
                    blk = order[pos]
                    wbuf = wb_of(pos)
                    if blk < NBLK:
                        sb_ = state["sti"] % NST
                        state["sti"] += 1
                        if j < 4:
                            DM("pool", lambda e: e.indirect_dma_start(out=stg[sb_][:], out_offset=None, in_=wegu_d.ap(), in_offset=bass.IndirectOffsetOnAxis(ap=BEi4[:, blk, j:j + 1], axis=0), bounds_check=state["bc"], oob_is_err=False), ["BEi4"], [f"stg{sb_}"])
                        else:
                            DM("pool", lambda e: e.indirect_dma_start(out=stg[sb_][:], out_offset=None, in_=wed_d.ap(), in_offset=bass.IndirectOffsetOnAxis(ap=BEi2[:, blk, j - 4:j - 3], axis=0), bounds_check=state["bc"], oob_is_err=False), ["BEi2"], [f"stg{sb_}"])
                        return sb_
                    if pos == NBLK:
                        if j < 4:
                            DM("pool", lambda e: e.dma_start(out=wgu[wbuf][:, 4 * j:4 * j + 4, :].rearrange("p k n -> p (k n)"), in_=wsgu_d.ap()[:, j * 4096:(j + 1) * 4096]), [], [f"wgu{wbuf}_{j}"])
                        else:
                            DM("pool", lambda e: e.dma_start(out=wd[wbuf][:, 2 * (j - 4):2 * (j - 4) + 2, :].rearrange("p k n -> p (k n)"), in_=wsd_d.ap()[:, (j - 4) * 4096:(j - 3) * 4096]), [], [f"wd{wbuf}_{j - 4}"])
                    return None

                def wcast(pos, j, sb_):
                    if sb_ is None:
                        return
                    wbuf = wb_of(pos)
                    if j < 4:
                        dstw = wgu[wbuf][:, 4 * j:4 * j + 4, :].rearrange("p k n -> p (k n)")
                        wkey = f"wgu{wbuf}_{j}"
                    else:
                        dstw = wd[wbuf][:, 2 * (j - 4):2 * (j - 4) + 2, :].rearrange("p k n -> p (k n)")
                        wkey = f"wd{wbuf}_{j - 4}"
                    if j % 2 == 0:
                        A(lambda e: e.copy(out=dstw, in_=stg[sb_][:]), [f"stg{sb_}"], [wkey])
                    else:
                        V(lambda e: e.tensor_copy(out=dstw, in_=stg[sb_][:]), [f"stg{sb_}"], [wkey])

                def transposes(pos):
                    mb_ = pos % 2
                    for sbi in range(2):
                        hi = mb_ * 2 + sbi
                        for hh in range(2):
                            def tr(e, hi=hi, hh=hh):
                                for k in range(8):
                                    kk = hh * 8 + k
                                    r = e.transpose(PT[hh][:, k * 128:(k + 1) * 128], AP(hg[hi], D, kk, [[16, 128]]), identb[:])
                                return r
                            P(tr, [f"hg{hi}", "identb"], [f"PT6{hh}"])
                            if hh == 0:
                                V(lambda e, sbi=sbi, hh=hh: e.tensor_copy(out=hgT[:, hh * 8:(hh + 1) * 8, sbi * 128:(sbi + 1) * 128], in_=PT[hh][:].rearrange("p (k c) -> p k c", k=8)), [], [f"PT6{hh}", f"hgT{sbi}_{hh}"])
                            else:
                                A(lambda e, sbi=sbi, hh=hh: e.copy(out=hgT[:, hh * 8:(hh + 1) * 8, sbi * 128:(sbi + 1) * 128], in_=PT[hh][:].rearrange("p (k c) -> p k c", k=8)), [], [f"PT6{hh}", f"hgT{sbi}_{hh}"])

                hgTk = [f"hgT{a}_{b2}" for a in range(2) for b2 in range(2)]
                actk = [f"actT{kk}" for kk in range(4)]

                def gateup(pos, kk):
                    wbuf = wb_of(pos)
                    wguk = [f"wgu{wbuf}_{j}" for j in range(4)]
                    pb = kk % 2

                    def mg(e):
                        for k in range(16):
                            r = e.matmul(PG[pb][:, 0:BS], lhsT=AP(wgu[wbuf], 16384, k * 1024 + kk, [[4, 128]]), rhs=hgT[:, k, :], start=(k == 0), stop=(k == 15))
                        return r
                    P(mg, wguk + hgTk, [f"PG6{pb}"])

                    def mu(e):
                        for k in range(16):
                            r = e.matmul(PU[pb][:, 0:BS], lhsT=AP(wgu[wbuf], 16384, k * 1024 + 512 + kk, [[4, 128]]), rhs=hgT[:, k, :], start=(k == 0), stop=(k == 15))
                        return r
                    P(mu, wguk + hgTk, [f"PU6{pb}"])
                    A(lambda e: e.activation(out=gsb[pb][:], in_=PG[pb][:, 0:BS], func=AF.Silu), [], [f"PG6{pb}", f"gsb{pb}"])
                    V(lambda e: e.tensor_tensor(out=actT[:, kk, :], in0=PU[pb][:, 0:BS], in1=gsb[pb][:], op=ALU.mult), [f"gsb{pb}"], [f"PU6{pb}", f"actT{kk}"])

                def down(pos, sbi, half):
                    blk = order[pos]
                    wbuf = wb_of(pos)
                    mb_ = pos % 2
                    wdk = [f"wd{wbuf}_{j}" for j in range(2)]
                    ys = state["ys"] % 2
                    state["ys"] += 1
                    for n2 in range(2):
                        n = half * 2 + n2
                        yb = state["yi"] % 2
                        state["yi"] += 1

                        def md(e, n=n, yb=yb):
                            for kk in range(4):
                                r = e.matmul(PY[yb][:], lhsT=actT[:, kk, sbi * 128:(sbi + 1) * 128], rhs=wd[wbuf][:, kk, n * 512:(n + 1) * 512], start=(kk == 0), stop=(kk == 3))
                            return r
                        P(md, actk + wdk, [f"PY6{yb}"])
                        if n2 == 0:
                            A(lambda e, n2=n2, yb=yb: e.activation(out=ysb[ys][:, n2 * 512:(n2 + 1) * 512], in_=PY[yb][:], func=AF.Copy, scale=rmt[mb_][:, sbi, 1:2]), [f"rmt{mb_}"], [f"PY6{yb}", f"ysb{ys}_{n2}"])
                        else:
                            V(lambda e, n2=n2, yb=yb: e.tensor_scalar(out=ysb[ys][:, n2 * 512:(n2 + 1) * 512], in0=PY[yb][:], scalar1=rmt[mb_][:, sbi, 1:2], scalar2=None, op0=ALU.mult), [f"rmt{mb_}"], [f"PY6{yb}", f"ysb{ys}_{n2}"])
                    r0 = blk * BS + sbi * 128
                    DM("sp", lambda e: e.dma_start(out=Y_d.ap()[r0:r0 + 128, half * 1024:(half + 1) * 1024], in_=ysb[ys][:]), [f"ysb{ys}_0", f"ysb{ys}_1"], [f"Y_d{blk}_{sbi}_{half}"])

                def mkreg(e):
                    r = e.alloc_register("oobbound")
                    e.reg_mov(r, 64 * 128 * 4 - 1)
                    state["bc"] = e.snap(r)
                mk.q["pool"].append(mkreg)
                slot_of = {}

                def issue(b_, j):
                    if b_ < NTOT and b_ <= NBLK and (b_, j) not in slot_of:
                        slot_of[(b_, j)] = wdma(b_, j)

                def cast(b_, j):
                    if b_ < NTOT and b_ <= NBLK:
                        issue(b_, j)
                        wcast(b_, j, slot_of[(b_, j)])

                rows(0)
                for j in range(6):
                    cast(0, j)
                for j in range(4):
                    issue(1, j)
                for blk in range(NTOT):
                    nb = blk + 1
                    have_next = nb < NTOT
                    early = nb + 1 < NBLK
                    if have_next:
                        rows(nb)
                        for j in range(4):
                            issue(nb, j)
                    transposes(blk)
                    if have_next:
                        cast(nb, 0)
                        issue(nb, 4)
                        cast(nb, 1)
                        issue(nb, 5)
                    gateup(blk, 0)
                    if have_next:
                        cast(nb, 2)
                        if early:
                            issue(nb + 1, 0)
                    gateup(blk, 1)
                    if have_next:
                        cast(nb, 3)
                        if early:
                            issue(nb + 1, 1)
                    gateup(blk, 2)
                    if have_next:
                        cast(nb, 4)
                        if early:
                            issue(nb + 1, 2)
                    gateup(blk, 3)
                    if have_next:
                        cast(nb, 5)
                        if early:
                            issue(nb + 1, 3)
                    for sbi in range(2):
                        for half in range(2):
                            down(blk, sbi, half)
                mk.flush()
        if upto >= 7:
            with ExitStack() as pes:
                mk.pes = pes
                g2b = mk.sb("g2b", [128, D], F32)
                fgb = mk.sb("fgb", [128, D], F32)
                acc = [mk.sb(f"acc7{i}", [128, D], F32) for i in range(2)]
                gk = [mk.sb(f"gk{i}", [128, D], BF16) for i in range(8)]
                x1t = [mk.sb(f"x1t7{i}", [128, D], F32) for i in range(2)]
                junk = mk.sb("junk7", [128, D], BF16)
                tsum = mk.sb("tsum", [128, D], F32)
                st = mk.sb("st7", [128, NT, 2], F32)
                DM("sp", lambda e: e.dma_start(out=g2b[:], in_=ada_d.ap()[5:6, :].partition_broadcast(128)), w=["g2b"])
                DM("sp", lambda e: e.dma_start(out=fgb[:], in_=fng_d.ap().partition_broadcast(128)), w=["fgb"])
                gi = 0
                for t in range(NT):
                    b = t % 2
                    sl = slice(t * 128, (t + 1) * 128)
                    DM("sp", lambda e, b=b, sl=sl: e.dma_start(out=x1t[b][:], in_=x1_d.ap()[sl, :]), w=[f"x1t7{b}"])
                    gs = []
                    for k in range(7):
                        g_ = gi % 8
                        gi += 1
                        gs.append(g_)
                        if k == 6:
                            DM("sp", lambda e, g_=g_, t=t: e.dma_start(out=gk[g_][:], in_=Y_d.ap()[NROWS + t * 128:NROWS + (t + 1) * 128, :]), w=[f"gk{g_}"])
                        else:
                            DM("pool", lambda e, g_=g_, t=t, k=k: e.indirect_dma_start(out=gk[g_][:], out_offset=None, in_=Y_d.ap(), in_offset=bass.IndirectOffsetOnAxis(ap=dest_i[:, t, k:k + 1], axis=0)), ["dest_i"], [f"gk{g_}"])
                    G(lambda e, gs=gs: e.tensor_tensor(out=tsum[:], in0=gk[gs[0]][:], in1=gk[gs[1]][:], op=ALU.add), [f"gk{gs[0]}", f"gk{gs[1]}"], ["tsum"])
                    G(lambda e, gs=gs: e.tensor_tensor(out=tsum[:], in0=tsum[:], in1=gk[gs[2]][:], op=ALU.add), [f"gk{gs[2]}", "tsum"], ["tsum"])
                    V(lambda e, b=b, gs=gs: e.tensor_tensor(out=acc[b][:], in0=gk[gs[3]][:], in1=gk[gs[4]][:], op=ALU.add), [f"gk{gs[3]}", f"gk{gs[4]}"], [f"acc7{b}"])
                    for k in (5, 6):
                        V(lambda e, b=b, g_=gs[k]: e.tensor_tensor(out=acc[b][:], in0=acc[b][:], in1=gk[g_][:], op=ALU.add), [f"gk{gs[k]}", f"acc7{b}"], [f"acc7{b}"])
                    V(lambda e, b=b: e.tensor_tensor(out=acc[b][:], in0=acc[b][:], in1=tsum[:], op=ALU.add), ["tsum", f"acc7{b}"], [f"acc7{b}"])
                    V(lambda e, b=b: e.tensor_tensor(out=acc[b][:], in0=acc[b][:], in1=g2b[:], op=ALU.mult), ["g2b", f"acc7{b}"], [f"acc7{b}"])
                    V(lambda e, b=b: e.tensor_tensor(out=acc[b][:], in0=acc[b][:], in1=x1t[b][:], op=ALU.add), [f"x1t7{b}", f"acc7{b}"], [f"acc7{b}"])
                    A(lambda e, t=t, b=b: e.activation(out=junk[:], in_=acc[b][:], func=AF.Square, accum_out=st[:, t, 0:1]), [f"acc7{b}"], ["junk7", f"st7{t}"])
                    A(lambda e, t=t: e.activation(out=st[:, t, 1:2], in_=st[:, t, 0:1], func=AF.Sqrt, bias=EPS, scale=1.0 / D), [f"st7{t}"], [f"st7{t}"])
                    V(lambda e, t=t: e.reciprocal(out=st[:, t, 1:2], in_=st[:, t, 1:2]), [f"st7{t}"], [f"st7{t}"])
                    V(lambda e, t=t, b=b: e.scalar_tensor_tensor(out=acc[b][:], in0=acc[b][:], scalar=st[:, t, 1:2], in1=fgb[:], op0=ALU.mult, op1=ALU.mult), [f"acc7{b}", f"st7{t}", "fgb"], [f"acc7{b}"])
                    DM("act", lambda e, b=b, sl=sl: e.dma_start(out=out_d.ap()[sl, :], in_=acc[b][:]), [f"acc7{b}"], [f"out{t}"])
                mk.flush()
        if upto >= 5:
            p57.close()

        if dbg and upto <= 2:
            pass
        mk.pes = pers
        mk.flush()
    return nc, dbg_d


def _host_inputs(b, inputs, with_experts=True):
    f = lambda a: np.ascontiguousarray(a, dtype=np.float32)
    half = 32
    inv = (10000.0 ** (-np.arange(half, dtype=np.float32) / half)).astype(np.float32)
    invf = np.zeros((64, 2), np.float32)
    invf[:, 0] = np.concatenate([inv, inv])
    invf[:32, 1] = -1.0
    invf[32:, 1] = 1.0
    m = {
        "x": f(inputs["x"][b]),
        "c": f(inputs["c"][b].reshape(16, 128).T),
        "positions": np.ascontiguousarray(inputs["positions"][b].reshape(1, S).astype(np.int32)),
        "w_ada": f(inputs["w_ada"][0]),
        "b_ada": f(inputs["b_ada"][0].reshape(1, -1)),
        "norm1_gain": f(inputs["norm1_gain"][0].reshape(1, -1)),
        "w_in": f(inputs["w_in"][0]),
        "dn_conv_w": f(inputs["dn_conv_w"][0].reshape(24, 128, 4).transpose(1, 0, 2)),
        "dn_a_log": f(inputs["dn_a_log"][0].reshape(1, 8)),
        "dn_dt_bias": f(inputs["dn_dt_bias"][0].reshape(1, 8)),
        "dn_norm_gain": f(inputs["dn_norm_gain"][0].reshape(1, 128)),
        "mla_q_norm_gain": f(inputs["mla_q_norm_gain"][0].reshape(4, 128).T),
        "w_q_up": f(inputs["w_q_up"][0]),
        "mla_kv_norm_gain": f(inputs["mla_kv_norm_gain"][0].reshape(4, 128).T),
        "w_kv_up": f(inputs["w_kv_up"][0]),
        "w_out": f(inputs["w_out"][0]),
        "norm2_gain": f(inputs["norm2_gain"][0].reshape(1, -1)),
        "w_router": f(inputs["w_router"][0]),
        "router_bias": f(inputs["router_bias"][0].reshape(1, 64)),
        "final_norm_gain": f(inputs["final_norm_gain"].reshape(1, -1)),
        "invf": invf,
    }
    if with_experts:
        m["w_exp_gate_up"] = f(inputs["w_exp_gate_up"][0]).reshape(64 * 128 * 4, 4096)
        m["w_exp_down"] = f(inputs["w_exp_down"][0]).reshape(64 * 128 * 2, 4096)
        m["w_sh_gate_up"] = f(inputs["w_sh_gate_up"][0]).reshape(128, 16 * 1024)
        m["w_sh_down"] = f(inputs["w_sh_down"][0]).reshape(128, 4 * 2048)
    return m


def kernel(**inputs):
    nc, _ = build()
    shared = None
    in_maps = []
    for b in range(8):
        m = _host_inputs(b, inputs)
        if shared is None:
            shared = m
        else:
            for k in m:
                if k not in ("x", "c", "positions"):
                    m[k] = shared[k]
        in_maps.append(m)
    res = run_bass_kernel_spmd(nc, in_maps, core_ids=list(range(8)))
    return np.stack([r["out"] for r in res.results], axis=0).astype(np.float32)
```
